# Optimizing a Trainium2 kernel written in Bass

```python
import jax, jax.numpy as jnp
from jax import lax

D_MODEL = 2048
BATCH = 2
SEQ = 4096
DEPTH = 1

HEAD_DIM = 128
NSA_HEADS = 16
NSA_KV_GROUPS = 2
NSA_HPG = NSA_HEADS // NSA_KV_GROUPS
CMP_BLOCK = 32
CMP_STRIDE = 16
CMP_HIDDEN = 256
SEL_BLOCK = 64
SEL_TOPK = 16
WINDOW = 512
Q_BLOCK = 128
SGU_WIDTH = 2048
SGU_GROUPS = 8
SGU_CHUNK = 128
MEM_LEN = 256
MEM_HEADS = 4
N_GROUPS = 4
EXPERTS_PER_GROUP = 16
N_EXPERTS = N_GROUPS * EXPERTS_PER_GROUP
EXPERT_TOPK = 2
EXPERT_FF = 512
MOE_BLOCK = 128
ROPE_THETA = 10000.0
LN_EPS = 1e-5
SEL_BIG = 1e9
DN_ALPHA = (2.0 * DEPTH) ** 0.25
DN_BETA = (8.0 * DEPTH) ** -0.25

Q_WIDTH = NSA_HEADS * HEAD_DIM
KV_WIDTH = 3 * 2 * NSA_KV_GROUPS * HEAD_DIM
NSA_GATE_WIDTH = 3 * NSA_HEADS
SGU_IN_WIDTH = 2 * SGU_WIDTH
MERGE_WIDTH = 2 * D_MODEL
IN_WIDTH = Q_WIDTH + KV_WIDTH + NSA_GATE_WIDTH + SGU_IN_WIDTH + MERGE_WIDTH
MEM_WIDTH = MEM_HEADS * HEAD_DIM

kernel_name = 'hybrid_nsa_sgu_hmoe_block'


def layer_norm(x, g, b):
    xf = x.astype(jnp.float32)
    mu = jnp.mean(xf, -1, keepdims=True)
    var = jnp.mean(jnp.square(xf - mu), -1, keepdims=True)
    return ((xf - mu) * lax.rsqrt(var + LN_EPS) * g + b).astype(x.dtype)


def rope_tables(pos):
    inv = ROPE_THETA ** (-jnp.arange(0, HEAD_DIM, 2, dtype=jnp.float32) / HEAD_DIM)
    ang = pos.astype(jnp.float32)[:, None] * inv[None, :]
    return jnp.cos(ang), jnp.sin(ang)


def apply_rope(t, cos, sin):
    t1, t2 = jnp.split(t, 2, axis=-1)
    c = cos[None, :, None, :]
    s = sin[None, :, None, :]
    return jnp.concatenate([t1 * c - t2 * s, t1 * s + t2 * c], -1).astype(t.dtype)


def masked_softmax(s, mask):
    s = jnp.where(mask, s.astype(jnp.float32), -jnp.inf)
    m = jnp.max(s, -1, keepdims=True)
    m = jnp.where(jnp.isfinite(m), m, 0.0)
    p = jnp.exp(s - m)
    return p / jnp.maximum(jnp.sum(p, -1, keepdims=True), 1e-30)


def compress_blocks(t, pe, w1, w2):
    B, S, G, D = t.shape
    c = t.reshape(B, S // CMP_STRIDE, CMP_STRIDE, G, D)
    blocks = jnp.concatenate([c[:, :-1], c[:, 1:]], axis=2)
    blocks = blocks + pe[None, None, :, None, :]
    n_cmp = blocks.shape[1]
    flat = jnp.moveaxis(blocks, 3, 2).reshape(B, n_cmp, G, CMP_BLOCK * D)
    return jax.nn.gelu(flat @ w1) @ w2


def nsa_attention(q, k_cmp, v_cmp, cmp_end, k_slc, v_slc, k_win, v_win, gates):
    B, S, H, D = q.shape
    G, HPG = NSA_KV_GROUPS, NSA_HPG
    n_cmp = k_cmp.shape[1]
    n_sel = S // SEL_BLOCK
    n_pick = min(SEL_TOPK, n_sel)
    nq = S // Q_BLOCK
    ci = CMP_STRIDE * jnp.arange(n_cmp)
    sj = SEL_BLOCK * jnp.arange(n_sel)
    overlap = jnp.clip(jnp.minimum(ci[:, None] + CMP_BLOCK, sj[None, :] + SEL_BLOCK)
                       - jnp.maximum(ci[:, None], sj[None, :]), 0, None)
    sel_map = overlap.astype(jnp.float32) / CMP_BLOCK

    def to_blocks(t):
        return t.reshape(B, n_sel, SEL_BLOCK, G, D).transpose(0, 3, 1, 2, 4).reshape(B, G, n_sel, SEL_BLOCK * D)

    ks_b, vs_b = to_blocks(k_slc), to_blocks(v_slc)
    pad = ((0, 0), (WINDOW, 0), (0, 0), (0, 0))
    kw_p, vw_p = jnp.pad(k_win, pad), jnp.pad(v_win, pad)
    q_blk = q.reshape(B, nq, Q_BLOCK, G, HPG, D).swapaxes(0, 1)
    g_blk = gates.reshape(B, nq, Q_BLOCK, G, HPG, 3).swapaxes(0, 1)
    sel_ids = jnp.arange(n_sel)
    win_off = jnp.arange(WINDOW + Q_BLOCK)
    b_ix = jnp.arange(B)[:, None, None]
    g_ix = jnp.arange(G)[None, :, None]

    def one_block(args):
        c, qc, gc = args
        t = c * Q_BLOCK + jnp.arange(Q_BLOCK)
        s_c = jnp.einsum('btgnd,bkgd->btgnk', qc, k_cmp)
        m_c = cmp_end[None, :] <= t[:, None]
        p_c = masked_softmax(s_c, m_c[None, :, None, None, :])
        o_c = jnp.einsum('btgnk,bkgd->btgnd', p_c.astype(v_cmp.dtype), v_cmp)
        imp = jnp.einsum('btgnk,kj->btgj', p_c, sel_map)
        cur = t // SEL_BLOCK
        visible = sel_ids[None, :] <= cur[:, None]
        forced = (sel_ids[None, :] == 0) | (sel_ids[None, :] == cur[:, None]) | (sel_ids[None, :] == cur[:, None] - 1)
        score = jnp.where(forced[None, :, None, :], SEL_BIG,
                          jnp.where(visible[None, :, None, :], imp, -SEL_BIG))
        _, idx = lax.top_k(score, n_pick)
        idx_bg = jnp.moveaxis(idx, 2, 1).reshape(B, G, Q_BLOCK * n_pick)
        k_sel = ks_b[b_ix, g_ix, idx_bg].reshape(B, G, Q_BLOCK, n_pick * SEL_BLOCK, D)
        v_sel = vs_b[b_ix, g_ix, idx_bg].reshape(B, G, Q_BLOCK, n_pick * SEL_BLOCK, D)
        s_s = jnp.einsum('btgnd,bgtkd->btgnk', qc, k_sel)
        key_pos = (idx[..., None] * SEL_BLOCK + jnp.arange(SEL_BLOCK)).reshape(B, Q_BLOCK, G, n_pick * SEL_BLOCK)
        m_s = key_pos <= t[None, :, None, None]
        p_s = masked_softmax(s_s, m_s[:, :, :, None, :])
        o_s = jnp.einsum('btgnk,bgtkd->btgnd', p_s.astype(v_sel.dtype), v_sel)
        kwc = lax.dynamic_slice_in_dim(kw_p, c * Q_BLOCK, WINDOW + Q_BLOCK, axis=1)
        vwc = lax.dynamic_slice_in_dim(vw_p, c * Q_BLOCK, WINDOW + Q_BLOCK, axis=1)
        s_w = jnp.einsum('btgnd,bkgd->btgnk', qc, kwc)
        kp = c * Q_BLOCK - WINDOW + win_off
        m_w = (kp[None, :] >= 0) & (kp[None, :] <= t[:, None]) & (t[:, None] - kp[None, :] < WINDOW)
        p_w = masked_softmax(s_w, m_w[None, :, None, None, :])
        o_w = jnp.einsum('btgnk,bkgd->btgnd', p_w.astype(vwc.dtype), vwc)
        return gc[..., 0:1] * o_c + gc[..., 1:2] * o_s + gc[..., 2:3] * o_w

    out = lax.map(one_block, (jnp.arange(nq, dtype=jnp.int32), q_blk, g_blk))
    return out.swapaxes(0, 1).reshape(B, S, H * D)


def spatial_gating(u, v, ln_g, ln_b, w_s, b_s):
    B, S, _ = v.shape
    u = jax.nn.gelu(u)
    v = layer_norm(jax.nn.gelu(v), ln_g, ln_b)
    vc = v.reshape(B, S // SGU_CHUNK, SGU_CHUNK, SGU_GROUPS, SGU_WIDTH // SGU_GROUPS)
    causal = jnp.tril(jnp.ones((SGU_CHUNK, SGU_CHUNK), dtype=bool))
    w_causal = jnp.where(causal[None], w_s, jnp.zeros_like(w_s))
    z = jnp.einsum('gts,bcsgd->bctgd', w_causal, vc) + b_s.T[None, None, :, :, None]
    return u * z.reshape(B, S, SGU_WIDTH)


def memory_cross_attention(x, mem, w_xq, w_xk, w_xv, w_xo):
    B, S, _ = x.shape
    M = mem.shape[1]
    q = (x @ w_xq).reshape(B, S, MEM_HEADS, HEAD_DIM) * HEAD_DIM ** -0.5
    k = (mem @ w_xk).reshape(B, M, MEM_HEADS, HEAD_DIM)
    v = (mem @ w_xv).reshape(B, M, MEM_HEADS, HEAD_DIM)
    p = jax.nn.softmax(jnp.einsum('bshd,bmhd->bhsm', q, k).astype(jnp.float32), axis=-1)
    o = jnp.einsum('bhsm,bmhd->bshd', p.astype(v.dtype), v).reshape(B, S, MEM_WIDTH)
    return o @ w_xo


def hierarchical_moe(x, w_router_grp, b_router_grp, w_router_exp, b_router_exp, w_exp_gate, w_exp_up, w_exp_down):
    B, S, D = x.shape
    N = B * S
    xf = x.reshape(N, D)
    p_grp = jax.nn.softmax((xf @ w_router_grp).astype(jnp.float32) + b_router_grp, axis=-1)
    g_grp, grp = lax.top_k(p_grp, 1)
    logit_exp = ((xf @ w_router_exp).astype(jnp.float32) + b_router_exp).reshape(N, N_GROUPS, EXPERTS_PER_GROUP)
    logit_in = logit_exp[jnp.arange(N), grp[:, 0]]
    g_in, local = lax.top_k(jax.nn.softmax(logit_in, axis=-1), EXPERT_TOPK)
    gate = g_grp * g_in / jnp.sum(g_in, -1, keepdims=True)
    expert = grp * EXPERTS_PER_GROUP + local
    NK = N * EXPERT_TOPK
    e_flat = expert.reshape(NK)
    tok_flat = jnp.repeat(jnp.arange(N, dtype=jnp.int32), EXPERT_TOPK)
    w_flat = gate.reshape(NK)
    order = jnp.argsort(e_flat)
    e_s = e_flat[order]
    counts = jnp.bincount(e_flat, length=N_EXPERTS)
    start = jnp.cumsum(counts) - counts
    pcounts = ((counts + MOE_BLOCK - 1) // MOE_BLOCK) * MOE_BLOCK
    pend = jnp.cumsum(pcounts)
    pstart = pend - pcounts
    dest = pstart[e_s] + (jnp.arange(NK) - start[e_s])
    n_blocks = -(-NK // MOE_BLOCK) + N_EXPERTS
    P = n_blocks * MOE_BLOCK
    row_tok = jnp.full((P,), N, dtype=jnp.int32).at[dest].set(tok_flat[order])
    row_w = jnp.zeros((P,), jnp.float32).at[dest].set(w_flat[order])
    blk_exp = jnp.minimum(jnp.searchsorted(pend, jnp.arange(n_blocks) * MOE_BLOCK, side='right'), N_EXPERTS - 1)
    x_pad = jnp.concatenate([xf, jnp.zeros((1, D), xf.dtype)], axis=0)

    def block_ffn(args):
        tok, wr, e = args
        h = x_pad[tok]
        a = jax.nn.silu(h @ w_exp_gate[e]) * (h @ w_exp_up[e])
        return (a @ w_exp_down[e]) * wr[:, None].astype(h.dtype)

    y_rows = lax.map(block_ffn, (row_tok.reshape(n_blocks, MOE_BLOCK), row_w.reshape(n_blocks, MOE_BLOCK), blk_exp))
    out = jnp.zeros((N + 1, D), x.dtype).at[row_tok].add(y_rows.reshape(P, D))[:N]
    return out.reshape(B, S, D)


def hybrid_layer(x, mem, w_in, cmp_pe_k, cmp_w1_k, cmp_w2_k, cmp_pe_v, cmp_w1_v, cmp_w2_v,
                 sgu_ln_g, sgu_ln_b, sgu_w_s, sgu_b_s, w_branch_a, w_branch_b, w_o, ln1_g, ln1_b,
                 w_xq, w_xk, w_xv, w_xo, ln2_g, ln2_b,
                 w_router_grp, b_router_grp, w_router_exp, b_router_exp,
                 w_exp_gate, w_exp_up, w_exp_down, ln3_g, ln3_b):
    B, S, _ = x.shape
    cuts = [Q_WIDTH, Q_WIDTH + KV_WIDTH, Q_WIDTH + KV_WIDTH + NSA_GATE_WIDTH,
            Q_WIDTH + KV_WIDTH + NSA_GATE_WIDTH + SGU_WIDTH,
            Q_WIDTH + KV_WIDTH + NSA_GATE_WIDTH + SGU_IN_WIDTH]
    q, kv, g_nsa, u, v, g_merge = jnp.split(x @ w_in, cuts, axis=-1)
    cos, sin = rope_tables(jnp.arange(S, dtype=jnp.int32))
    q = apply_rope(q.reshape(B, S, NSA_HEADS, HEAD_DIM), cos, sin) * HEAD_DIM ** -0.5
    kv = kv.reshape(B, S, 3, 2, NSA_KV_GROUPS, HEAD_DIM)
    k_cmp = compress_blocks(kv[:, :, 0, 0], cmp_pe_k, cmp_w1_k, cmp_w2_k)
    v_cmp = compress_blocks(kv[:, :, 0, 1], cmp_pe_v, cmp_w1_v, cmp_w2_v)
    cmp_end = CMP_STRIDE * jnp.arange(k_cmp.shape[1], dtype=jnp.int32) + CMP_BLOCK - 1
    ccos, csin = rope_tables(cmp_end)
    k_cmp = apply_rope(k_cmp, ccos, csin)
    k_slc = apply_rope(kv[:, :, 1, 0], cos, sin)
    k_win = apply_rope(kv[:, :, 2, 0], cos, sin)
    o_nsa = nsa_attention(q, k_cmp, v_cmp, cmp_end, k_slc, kv[:, :, 1, 1], k_win, kv[:, :, 2, 1],
                          jax.nn.sigmoid(g_nsa.reshape(B, S, NSA_HEADS, 3)))
    o_sgu = spatial_gating(u, v, sgu_ln_g, sgu_ln_b, sgu_w_s, sgu_b_s)
    g_a, g_b = jnp.split(jax.nn.sigmoid(g_merge), 2, axis=-1)
    merged = g_a * (o_nsa @ w_branch_a) + g_b * (o_sgu @ w_branch_b)
    x = layer_norm(DN_ALPHA * x + merged @ w_o, ln1_g, ln1_b)
    x = layer_norm(DN_ALPHA * x + memory_cross_attention(x, mem, w_xq, w_xk, w_xv, w_xo), ln2_g, ln2_b)
    moe = hierarchical_moe(x, w_router_grp, b_router_grp, w_router_exp, b_router_exp, w_exp_gate, w_exp_up, w_exp_down)
    return layer_norm(DN_ALPHA * x + moe, ln3_g, ln3_b)


def setup_inputs(seed: int = 0) -> dict:
    key = jax.random.key(seed)
    ks = jax.random.split(key, 40)
    L, D = DEPTH, D_MODEL

    def nrm(k, shape, scale):
        return jax.random.normal(k, shape, jnp.float32) * scale

    return {
        'x': nrm(ks[0], (BATCH, SEQ, D), 1.0),
        'mem': nrm(ks[1], (BATCH, MEM_LEN, D), 1.0),
        'w_in': nrm(ks[2], (L, D, IN_WIDTH), D ** -0.5),
        'cmp_pe_k': nrm(ks[3], (L, CMP_BLOCK, HEAD_DIM), 0.1),
        'cmp_w1_k': nrm(ks[4], (L, CMP_BLOCK * HEAD_DIM, CMP_HIDDEN), (CMP_BLOCK * HEAD_DIM) ** -0.5),
        'cmp_w2_k': nrm(ks[5], (L, CMP_HIDDEN, HEAD_DIM), CMP_HIDDEN ** -0.5),
        'cmp_pe_v': nrm(ks[6], (L, CMP_BLOCK, HEAD_DIM), 0.1),
        'cmp_w1_v': nrm(ks[7], (L, CMP_BLOCK * HEAD_DIM, CMP_HIDDEN), (CMP_BLOCK * HEAD_DIM) ** -0.5),
        'cmp_w2_v': nrm(ks[8], (L, CMP_HIDDEN, HEAD_DIM), CMP_HIDDEN ** -0.5),
        'sgu_ln_g': 1.0 + nrm(ks[9], (L, SGU_WIDTH), 0.02),
        'sgu_ln_b': nrm(ks[10], (L, SGU_WIDTH), 0.02),
        'sgu_w_s': nrm(ks[11], (L, SGU_GROUPS, SGU_CHUNK, SGU_CHUNK), SGU_CHUNK ** -0.5),
        'sgu_b_s': 1.0 + nrm(ks[12], (L, SGU_GROUPS, SGU_CHUNK), 0.1),
        'w_branch_a': nrm(ks[13], (L, Q_WIDTH, D), Q_WIDTH ** -0.5),
        'w_branch_b': nrm(ks[14], (L, SGU_WIDTH, D), SGU_WIDTH ** -0.5),
        'w_o': nrm(ks[15], (L, D, D), DN_BETA * D ** -0.5),
        'ln1_g': 1.0 + nrm(ks[16], (L, D), 0.02),
        'ln1_b': nrm(ks[17], (L, D), 0.02),
        'w_xq': nrm(ks[18], (L, D, MEM_WIDTH), D ** -0.5),
        'w_xk': nrm(ks[19], (L, D, MEM_WIDTH), D ** -0.5),
        'w_xv': nrm(ks[20], (L, D, MEM_WIDTH), D ** -0.5),
        'w_xo': nrm(ks[21], (L, MEM_WIDTH, D), DN_BETA * MEM_WIDTH ** -0.5),
        'ln2_g': 1.0 + nrm(ks[22], (L, D), 0.02),
        'ln2_b': nrm(ks[23], (L, D), 0.02),
        'w_router_grp': nrm(ks[24], (L, D, N_GROUPS), D ** -0.5),
        'b_router_grp': nrm(ks[25], (L, N_GROUPS), 0.01),
        'w_router_exp': nrm(ks[26], (L, D, N_EXPERTS), D ** -0.5),
        'b_router_exp': nrm(ks[27], (L, N_EXPERTS), 0.01),
        'w_exp_gate': nrm(ks[28], (L, N_EXPERTS, D, EXPERT_FF), D ** -0.5),
        'w_exp_up': nrm(ks[29], (L, N_EXPERTS, D, EXPERT_FF), D ** -0.5),
        'w_exp_down': nrm(ks[30], (L, N_EXPERTS, EXPERT_FF, D), DN_BETA * EXPERT_FF ** -0.5),
        'ln3_g': 1.0 + nrm(ks[31], (L, D), 0.02),
        'ln3_b': nrm(ks[32], (L, D), 0.02),
    }


def reference(x, mem, w_in, cmp_pe_k, cmp_w1_k, cmp_w2_k, cmp_pe_v, cmp_w1_v, cmp_w2_v,
              sgu_ln_g, sgu_ln_b, sgu_w_s, sgu_b_s, w_branch_a, w_branch_b, w_o, ln1_g, ln1_b,
              w_xq, w_xk, w_xv, w_xo, ln2_g, ln2_b,
              w_router_grp, b_router_grp, w_router_exp, b_router_exp,
              w_exp_gate, w_exp_up, w_exp_down, ln3_g, ln3_b):
    layer_params = (w_in, cmp_pe_k, cmp_w1_k, cmp_w2_k, cmp_pe_v, cmp_w1_v, cmp_w2_v,
                    sgu_ln_g, sgu_ln_b, sgu_w_s, sgu_b_s, w_branch_a, w_branch_b, w_o, ln1_g, ln1_b,
                    w_xq, w_xk, w_xv, w_xo, ln2_g, ln2_b,
                    w_router_grp, b_router_grp, w_router_exp, b_router_exp,
                    w_exp_gate, w_exp_up, w_exp_down, ln3_g, ln3_b)
    h = x
    for l in range(DEPTH):
        h = hybrid_layer(h, mem, *[p[l] for p in layer_params])
    return h
```

```python
import numpy as np
import concourse.bass as bass
import concourse.mybir as mybir
from concourse.bass_utils import run_bass_kernel_spmd

F32 = mybir.dt.float32
BF16 = mybir.dt.bfloat16
I32 = mybir.dt.int32
AF = mybir.ActivationFunctionType
ALU = mybir.AluOpType
AX = mybir.AxisListType

class Buf:
    __slots__ = ("t", "w", "r", "name")

    def __init__(self, t, name=""):
        self.t = t
        self.w = None
        self.r = {}
        self.name = name

    def __getitem__(self, k):
        return self.t[k]


class _Eng:
    def __init__(self, name, eng, sem):
        self.name = name
        self.eng = eng
        self.sem = sem
        self.tick = 0
        self.seen = {}
        self.pool = []
        self.pool_i = 0


class FW:
    def __init__(self, nc, n_dma_sems=6):
        self.nc = nc
        self.E = {}
        for name, eng in (("pe", nc.tensor), ("act", nc.scalar), ("dve", nc.vector),
                          ("pool", nc.gpsimd), ("sp", nc.sync)):
            e = _Eng(name, eng, nc.alloc_semaphore("s_" + name))
            self.E[name] = e
        for q in ("sp", "pool", "act"):
            e = self.E[q]
            for i in range(n_dma_sems):
                e.pool.append([nc.alloc_semaphore(f"d_{q}{i}"), 0])
        self.nsb = 0
        self.n_inst = 0

    def sb(self, shape, dtype, name=None, side=None):
        self.nsb += 1
        name = (name or "sb") + f"_{self.nsb}"
        return Buf(self.nc.alloc_sbuf_tensor(name, list(shape), dtype, side=side), name)

    def ps(self, shape, dtype=F32, name=None):
        self.nsb += 1
        name = name or f"ps{self.nsb}"
        return Buf(self.nc.alloc_psum_tensor(name, list(shape), dtype), name)

    def dram(self, name, shape, dtype, kind="Internal"):
        return Buf(self.nc.dram_tensor(name, list(shape), dtype, kind=kind), name)

    def _deps(self, reads, writes):
        evs = []
        for b in reads:
            if b.w is not None:
                evs.append(b.w)
        for b in writes:
            if b.w is not None:
                evs.append(b.w)
            evs.extend(b.r.values())
        return evs

    def _wait(self, e, evs):
        need = {}
        for (sem, val) in evs:
            k = id(sem)
            if e.seen.get(k, 0) >= val:
                continue
            if k not in need or need[k][1] < val:
                need[k] = (sem, val)
        for k, (sem, val) in need.items():
            if e.name == "pe" and sem is e.sem:
                continue
            e.eng.wait_ge(sem, val)
            e.seen[k] = val

    def _record(self, ev, reads, writes):
        k = id(ev[0])
        for b in reads:
            b.r[k] = ev
        for b in writes:
            b.w = ev
            b.r = {}

    def op(self, ename, fn, reads=(), writes=(), inc=True):
        e = self.E[ename]
        self._wait(e, self._deps(reads, writes))
        ins = fn(e.eng)
        self.n_inst += 1
        if inc:
            e.tick += 1
            ins.then_inc(e.sem, 1)
            self._record((e.sem, e.tick), reads, writes)
        else:
            self._record((e.sem, e.tick + 1), reads, writes)

    def barrier(self):
        evs = []
        for en in self.E.values():
            for s in en.pool:
                if s[1] > 0:
                    evs.append((s[0], s[1]))
            if en.tick > 0:
                evs.append((en.sem, en.tick))
        for en in self.E.values():
            for (sem, val) in evs:
                if en.seen.get(id(sem), 0) < val:
                    en.eng.wait_ge(sem, val)
                    en.seen[id(sem)] = val

    def dma(self, q, out_ap, in_ap, reads=(), writes=(), **kw):
        e = self.E[q]
        slot = e.pool[e.pool_i % len(e.pool)]
        e.pool_i += 1
        evs = self._deps(reads, writes)
        if slot[1] > 0:
            evs.append((slot[0], slot[1]))
        self._wait(e, evs)
        slot[1] += 16
        e.eng.dma_start(out=out_ap, in_=in_ap, **kw).then_inc(slot[0], 16)
        self.n_inst += 1
        self._record((slot[0], slot[1]), reads, writes)

    def finish(self, bufs):
        e = self.E["sp"]
        evs = []
        for b in bufs:
            if b.w is not None:
                evs.append(b.w)
        for en in self.E.values():
            for s in en.pool:
                if s[1] > 0:
                    evs.append((s[0], s[1]))
            if en.tick > 0 and en is not e:
                evs.append((en.sem, en.tick))
        self._wait(e, evs)


S = 4096
D = 2048
NB = 8
OFF_Q, OFF_KV, OFF_G, OFF_U, OFF_V, OFF_GA, OFF_GB = 0, 2048, 3584, 3632, 5680, 7728, 9776
ALPHA = 2.0 ** 0.25
EPS = 1e-5
CAP = 64


def _consts(j):
    c = {}
    c["ident"] = np.eye(128, dtype=np.float32)
    c["perm"] = np.roll(np.eye(128, dtype=np.float32), 64, axis=0)
    inv = (10000.0 ** (-np.arange(0, 128, 2, dtype=np.float32) / 128)).astype(np.float32)
    inv2 = np.concatenate([inv, inv])
    sgn = np.concatenate([-np.ones(64, np.float32), np.ones(64, np.float32)])

    def tab(pos, scale=1.0):
        ang = pos.astype(np.float32)[None, :] * inv2[:, None]
        return ((np.cos(ang) * scale).astype(np.float32),
                (np.sin(ang) * sgn[:, None] * scale).astype(np.float32))

    c["cosK"], c["sinK"] = tab(np.arange(S))
    own_pos = np.concatenate([128 * (4 * i + j) + np.arange(128) for i in range(NB)])
    c["cosQ"], c["sinQ"] = tab(own_pos, 128.0 ** -0.5)
    c["cosC"], c["sinC"] = tab(16 * np.arange(256) + 31)
    p = np.arange(128)[:, None]
    tl = np.arange(128)[None, :]
    caus = (p <= tl).astype(np.float32)
    dm = np.zeros((128, 4, 128), np.float32)
    for r in range(4):
        dm[:, r, :] = 1.0 if r < j else (caus if r == j else 0.0)
    c["dmask"] = dm
    wm = np.zeros((128, 8, 128), np.float32)
    for r in range(8):
        rel = r - 4 - j
        if rel == 0:
            wm[:, r, :] = caus
        elif rel in (-1, -2, -3):
            wm[:, r, :] = 1.0
        elif rel == -4:
            wm[:, r, :] = 1.0 - caus
    c["wmask"] = wm
    cm = np.zeros((128, NB, 2, 128), np.float32)
    sA = np.zeros((128, NB, 64), np.float32)
    sB = np.zeros((128, NB, 64), np.float32)
    jj = np.arange(64)[None, :]
    for i in range(NB):
        cb = 4 * i + j
        t = 128 * cb + np.arange(128)
        for ch in range(2):
            n = ch * 128 + np.arange(128)
            cm[:, i, ch, :] = ((16 * n[:, None] + 31 <= t[None, :]) & (n[:, None] < 255)).astype(np.float32)
        cur = (t // 64)[:, None]
        vis = jj <= cur
        f0 = jj == 0
        f1 = jj == cur
        f2 = jj == cur - 1
        forced = f0 | f1 | f2
        sA[:, i, :] = (vis & ~forced).astype(np.float32)
        b = np.where(vis, 0.0, -1e9)
        b = np.where(f2, 1e9, b)
        b = np.where(f1, 2e9, b)
        b = np.where(f0, 3e9, b)
        sB[:, i, :] = b
    c["cmask"], c["selA"], c["selB"] = cm, sA, sB
    n = np.arange(256)[:, None]
    ov = np.clip(np.minimum(16 * n + 32, 64 * jj + 64) - np.maximum(16 * n, 64 * jj), 0, None) / 32.0
    ov[255] = 0.0
    c["selmap"] = np.ascontiguousarray(ov.reshape(2, 128, 64).transpose(1, 0, 2)).astype(np.float32)
    ex = np.zeros((64, 32, 128), np.float32)
    for kt in range(32):
        ex[2 * kt, kt, :64] = 1.0
        ex[2 * kt + 1, kt, 64:] = 1.0
    c["exall"] = ex
    c["tri"] = caus
    c["ustrict"] = (p < tl).astype(np.float32)
    c["iota_c"] = np.tile(np.arange(64, dtype=np.float32)[None, :], (128, 1))
    c["iota_pb"] = np.tile(np.arange(64, dtype=np.float32)[:, None], (1, 64))
    return c


CONST_SHAPES = {k: v.shape for k, v in _consts(0).items()}


def build(dbg=()):
    nc = bass.Bass("TRN2", target_bir_lowering=False)
    fw = FW(nc)
    dbg_out = {}

    def din(name, shape):
        return fw.dram(name, shape, F32, kind="ExternalInput")

    x_d = din("x", [S, D])
    xo_d = din("x_own", [NB * 128, D])
    w_in = din("w_in", [D, 11824])
    wq_rot = din("wq_rot", [D, 2048])
    wk_rot = din("wk_rot", [D, 512])
    pe_kT = din("pe_kT", [128, 32])
    pe_vT = din("pe_vT", [128, 32])
    w1k_d = din("cmp_w1_k", [4096, 256])
    w1v_d = din("cmp_w1_v", [4096, 256])
    w2k_d = din("cmp_w2_k", [256, 128])
    w2kr_d = din("cmp_w2_k_rot", [256, 128])
    w2v_d = din("cmp_w2_v", [256, 128])
    sgu_wsT = din("sgu_wsT", [8, 128, 128])
    sgu_g_fm = din("sgu_g_fm", [128, 16])
    sgu_b_fm = din("sgu_b_fm", [128, 16])
    sgu_bs = din("sgu_bs", [1, 1024])
    w_br_a = din("w_branch_a", [D, D])
    w_br_b = din("w_branch_b", [D, D])
    w_o_d = din("w_o", [D, D])
    ln1_g = din("ln1_g", [1, D])
    ln1_b = din("ln1_b", [1, D])
    mem_d = din("mem", [256, D])
    w_xq = din("w_xq", [D, 512])
    w_xk = din("w_xk", [D, 512])
    w_xv = din("w_xv", [D, 512])
    w_xo = din("w_xo", [512, D])
    ln2_g = din("ln2_g", [1, D])
    ln2_b = din("ln2_b", [1, D])
    w_rt = din("w_router", [D, 68])
    b_rt = din("b_router", [1, 68])
    w_eg = din("w_exp_gate", [64, D, 512])
    w_eu = din("w_exp_up", [64, D, 512])
    w_ed = din("w_exp_down", [64, 512, D])
    ln3_g = din("ln3_g", [1, D])
    ln3_b = din("ln3_b", [1, D])
    C = {k: din("c_" + k, list(s)) for k, s in CONST_SHAPES.items()}
    out_d = fw.dram("out", [NB * 128, D], F32, kind="ExternalOutput")

    kT_d = fw.dram("kT_scr", [4, 128, S], BF16)
    v_d = fw.dram("v_scr", [S, 512], BF16)

    def r3(buf):
        return buf.t.ap().rearrange("(kc p) n -> p kc n", p=128)

    PS = fw.nc.alloc_psum_tensor("psum", [128, 8, 512], F32)
    banks = [Buf(PS, f"bank{i}") for i in range(8)]

    def bk(i):
        return PS[:, i, :]

    def bkbf(i):
        return PS[:, i, :].bitcast(BF16)

    rr = [0]

    def nextbank():
        i = rr[0] % 8
        rr[0] += 1
        return i

    epsc = fw.sb([128, 1], F32, "epsc")
    fw.op("dve", lambda e: e.memset(epsc[:], EPS), writes=[epsc])
    ident = fw.sb([128, 128], BF16, "ident")
    fw.dma("pool", ident[:], C["ident"][:], reads=[C["ident"]], writes=[ident])

    def mm(ps_ap, lhsT, rhs, start, stop, R, W, inc=None, sgc=False):
        if inc is None:
            inc = stop
        fw.op("pe", lambda e: e.matmul(ps_ap, lhsT, rhs, start=start, stop=stop, skip_group_check=sgc),
              reads=R, writes=W, inc=inc)

    def load_xT(src_d, row0, xb, dstT, col0, evac):
        fw.dma("pool", xb[:], src_d[row0:row0 + 128, :], reads=[src_d], writes=[xb])
        for half in range(2):
            b = nextbank()
            for k8 in range(8):
                kc = half * 8 + k8
                fw.op("pe", lambda e: e.transpose(bkbf(b)[:, k8 * 128:(k8 + 1) * 128],
                                                  xb[:, kc * 128:(kc + 1) * 128], ident[:]),
                      reads=[xb, ident], writes=[banks[b]], inc=(k8 == 7))
            src = bkbf(b).rearrange("p (k t) -> p k t", k=8)
            dst = dstT[:, half * 8:(half + 1) * 8, col0:col0 + 128]
            if evac == "act":
                fw.op("act", lambda e: e.copy(dst, src), reads=[banks[b]], writes=[dstT])
            else:
                fw.op("dve", lambda e: e.tensor_copy(dst, src), reads=[banks[b]], writes=[dstT])

    mark0 = nc.sbuf_base
    cmpraw = fw.sb([128, 4, S], BF16, "cmpraw")
    mark_dbg = nc.sbuf_base
    wkv = fw.sb([128, 16, 1536], BF16, "wkv")
    wkr = fw.sb([128, 16, 512], BF16, "wkr")
    w_in_r = r3(w_in)
    for c3 in range(3):
        fw.dma("pool", wkv[:, :, c3 * 512:(c3 + 1) * 512], w_in_r[:, :, OFF_KV + c3 * 512:OFF_KV + (c3 + 1) * 512],
               reads=[w_in], writes=[wkv])
    fw.dma("pool", wkr[:], r3(wk_rot), reads=[wk_rot], writes=[wkr])
    xbs = [fw.sb([128, D], BF16, f"xb{i}") for i in range(3)]
    xTs = [fw.sb([128, 16, 512], BF16, f"xT{i}") for i in range(2)]
    cos_t = [fw.sb([128, 512], F32, f"cos{i}") for i in range(2)]
    sin_t = [fw.sb([128, 512], F32, f"sin{i}") for i in range(2)]
    kst = [fw.sb([128, 4, 512], BF16, f"kst{i}") for i in range(2)]
    vst = [fw.sb([128, 4, 512], BF16, f"vst{i}") for i in range(2)]
    ropa = [fw.sb([128, 512], F32, f"ropa{i}") for i in range(2)]
    ropb = [fw.sb([128, 512], F32, f"ropb{i}") for i in range(2)]
    kT_r = kT_d.t.ap().rearrange("k d t -> d k t")

    def rope(ps_t, ps_r, cosb, sinb, cos_ap, sin_ap, out_ap, outbuf, n, idx):
        a, b2 = ropa[idx % 2], ropb[idx % 2]
        fw.op("dve", lambda e: e.tensor_tensor(a[:, 0:n], bk(ps_t)[:, 0:n], cos_ap, ALU.mult),
              reads=[banks[ps_t], cosb], writes=[a])
        fw.op("dve", lambda e: e.tensor_tensor(b2[:, 0:n], bk(ps_r)[:, 0:n], sin_ap, ALU.mult),
              reads=[banks[ps_r], sinb], writes=[b2])
        fw.op("pool", lambda e: e.tensor_tensor(out_ap, a[:, 0:n], b2[:, 0:n], ALU.add),
              reads=[a, b2], writes=[outbuf])

    ridx = [0]
    for tile in range(8):
        xT = xTs[tile % 2]
        ct, st = cos_t[tile % 2], sin_t[tile % 2]
        fw.dma("sp", ct[:], C["cosK"][:, tile * 512:(tile + 1) * 512], reads=[C["cosK"]], writes=[ct])
        fw.dma("sp", st[:], C["sinK"][:, tile * 512:(tile + 1) * 512], reads=[C["sinK"]], writes=[st])
        for blk in range(4):
            g = tile * 4 + blk
            load_xT(x_d, g * 128, xbs[g % 3], xT, blk * 128, "act" if blk % 2 == 0 else "dve")
        for f in range(4):
            b = nextbank()
            for kc in range(16):
                mm(bk(b), wkv[:, kc, f * 128:(f + 1) * 128], xT[:, kc, :], kc == 0, kc == 15, [wkv, xT], [banks[b]])
            fw.op("act", lambda e: e.copy(cmpraw[:, f, tile * 512:(tile + 1) * 512], bk(b)),
                  reads=[banks[b]], writes=[cmpraw])
        ks = kst[tile % 2]
        for kk in range(4):
            col = (512 if kk < 2 else 1024) + (kk % 2) * 128
            b1 = nextbank()
            for kc in range(16):
                mm(bk(b1), wkv[:, kc, col:col + 128], xT[:, kc, :], kc == 0, kc == 15, [wkv, xT], [banks[b1]])
            b2 = nextbank()
            for kc in range(16):
                mm(bk(b2), wkr[:, kc, kk * 128:(kk + 1) * 128], xT[:, kc, :], kc == 0, kc == 15, [wkr, xT], [banks[b2]])
            rope(b1, b2, ct, st, ct[:], st[:], ks[:, kk, :], ks, 512, ridx[0])
            ridx[0] += 1
        fw.dma("sp", kT_r[:, :, tile * 512:(tile + 1) * 512], ks[:], reads=[ks], writes=[kT_d])
        vs = vst[tile % 2]
        for blk in range(4):
            b = nextbank()
            for half, col in enumerate((768, 1280)):
                for kc in range(16):
                    mm(bk(b)[:, half * 256:(half + 1) * 256], xT[:, kc, blk * 128:(blk + 1) * 128],
                       wkv[:, kc, col:col + 256], kc == 0, kc == 15, [wkv, xT], [banks[b]],
                       inc=(kc == 15 and half == 1))
            fw.op("dve", lambda e: e.tensor_copy(vs[:, blk, :], bk(b)), reads=[banks[b]], writes=[vs])
        fw.dma("sp", v_d.t.ap()[tile * 512:(tile + 1) * 512, :].rearrange("(b p) f -> p b f", p=128), vs[:],
               reads=[vs], writes=[v_d])

    if "A" in dbg:
        dbg_out["cmpraw"] = fw.dram("dbg_cmpraw", [128, 4, S], BF16, kind="ExternalOutput")
        fw.dma("sp", dbg_out["cmpraw"].t.ap(), cmpraw[:], reads=[cmpraw], writes=[dbg_out["cmpraw"]])
        dbg_out["kT"] = fw.dram("dbg_kT", [4, 128, S], BF16, kind="ExternalOutput")
        dbg_out["v"] = fw.dram("dbg_v", [S, 512], BF16, kind="ExternalOutput")
        fw.barrier()
        nc.sbuf_base = mark_dbg
        tmpk = fw.sb([128, 4, S], BF16, "tmpk")
        fw.dma("sp", tmpk[:], kT_r, reads=[kT_d], writes=[tmpk])
        fw.dma("sp", dbg_out["kT"].t.ap().rearrange("k d t -> d k t"), tmpk[:], reads=[tmpk], writes=[dbg_out["kT"]])
        tmpv = fw.sb([128, 32, 512], BF16, "tmpv")
        fw.dma("sp", tmpv[:], v_d.t.ap().rearrange("(b p) f -> p b f", p=128), reads=[v_d], writes=[tmpv])
        fw.dma("sp", dbg_out["v"].t.ap().rearrange("(b p) f -> p b f", p=128), tmpv[:], reads=[tmpv], writes=[dbg_out["v"]])
        fw.finish(list(dbg_out.values()))
        return nc, fw


    fw.barrier()
    nc.sbuf_base = mark_dbg
    qT = fw.sb([128, NB, 16, 128], BF16, "qT")
    gates = fw.sb([128, NB, 48], F32, "gates")
    kcmpT = fw.sb([128, 2, 256], BF16, "kcmpT")
    vcmp = fw.sb([128, 2, 2, 129], BF16, "vcmp")
    mark2 = nc.sbuf_base
    xTo = fw.sb([128, 16, NB * 128], BF16, "xTo")
    xbo = [fw.sb([128, D], BF16, f"xbo{i}") for i in range(2)]
    for blk in range(NB):
        load_xT(xo_d, blk * 128, xbo[blk % 2], xTo, blk * 128, "act" if blk % 2 == 0 else "dve")
    wg = fw.sb([128, 16, 48], BF16, "wg")
    fw.dma("pool", wg[:], w_in_r[:, :, OFF_G:OFF_G + 48], reads=[w_in], writes=[wg])
    for blk in range(NB):
        b = nextbank()
        for kc in range(16):
            mm(bk(b)[:, 0:48], xTo[:, kc, blk * 128:(blk + 1) * 128], wg[:, kc, :], kc == 0, kc == 15, [xTo, wg], [banks[b]])
        fw.op("act", lambda e: e.activation(gates[:, blk, :], bk(b)[:, 0:48], AF.Sigmoid), reads=[banks[b]], writes=[gates])
    cosq = fw.sb([128, NB * 128], F32, "cosq")
    sinq = fw.sb([128, NB * 128], F32, "sinq")
    fw.dma("sp", cosq[:], C["cosQ"][:], reads=[C["cosQ"]], writes=[cosq])
    fw.dma("sp", sinq[:], C["sinQ"][:], reads=[C["sinQ"]], writes=[sinq])
    wqb = [fw.sb([128, 16, 512], BF16, f"wq{i}") for i in range(2)]
    wqrb = [fw.sb([128, 16, 512], BF16, f"wqr{i}") for i in range(2)]
    ropa2 = [fw.sb([128, 512], F32, f"ropa2{i}") for i in range(2)]
    ropb2 = [fw.sb([128, 512], F32, f"ropb2{i}") for i in range(2)]
    wqr_r = r3(wq_rot)
    ri = 0
    for hg in range(4):
        wq, wqr = wqb[hg % 2], wqrb[hg % 2]
        fw.dma("pool", wq[:], w_in_r[:, :, OFF_Q + hg * 512:OFF_Q + (hg + 1) * 512], reads=[w_in], writes=[wq])
        fw.dma("pool", wqr[:], wqr_r[:, :, hg * 512:(hg + 1) * 512], reads=[wq_rot], writes=[wqr])
        for half in range(2):
            for hh in range(4):
                b1 = nextbank()
                for kc in range(16):
                    mm(bk(b1), wq[:, kc, hh * 128:(hh + 1) * 128], xTo[:, kc, half * 512:(half + 1) * 512], kc == 0, kc == 15, [wq, xTo], [banks[b1]])
                b2 = nextbank()
                for kc in range(16):
                    mm(bk(b2), wqr[:, kc, hh * 128:(hh + 1) * 128], xTo[:, kc, half * 512:(half + 1) * 512], kc == 0, kc == 15, [wqr, xTo], [banks[b2]])
                a, bb = ropa2[ri % 2], ropb2[ri % 2]
                ri += 1
                fw.op("dve", lambda e: e.tensor_tensor(a[:], bk(b1), cosq[:, half * 512:(half + 1) * 512], ALU.mult), reads=[banks[b1], cosq], writes=[a])
                fw.op("dve", lambda e: e.tensor_tensor(bb[:], bk(b2), sinq[:, half * 512:(half + 1) * 512], ALU.mult), reads=[banks[b2], sinq], writes=[bb])
                fw.op("pool", lambda e: e.tensor_tensor(qT[:, half * 4:(half + 1) * 4, hg * 4 + hh, :],
                                                        a[:].rearrange("p (b t) -> p b t", b=4),
                                                        bb[:].rearrange("p (b t) -> p b t", b=4), ALU.add),
                      reads=[a, bb], writes=[qT])

    fw.barrier()
    nc.sbuf_base = mark2
    w1 = [fw.sb([128, 32, 256], BF16, f"w1{i}") for i in range(2)]
    peT = [fw.sb([128, 32], BF16, f"peT{i}") for i in range(2)]
    for kv, (wd, pd) in enumerate(((w1k_d, pe_kT), (w1v_d, pe_vT))):
        fw.dma("pool", w1[kv][:], wd.t.ap().rearrange("(j d) h -> d j h", d=128), reads=[wd], writes=[w1[kv]])
        fw.dma("pool", peT[kv][:], pd[:], reads=[pd], writes=[peT[kv]])
    w2s = []
    for wd in (w2k_d, w2kr_d, w2v_d):
        t = fw.sb([128, 2, 128], BF16, "w2")
        fw.dma("pool", t[:], wd.t.ap().rearrange("(hc h) d -> h hc d", h=128), reads=[wd], writes=[t])
        w2s.append(t)
    w2k, w2kr, w2v = w2s
    cosc = fw.sb([128, 256], F32, "cosc")
    sinc = fw.sb([128, 256], F32, "sinc")
    fw.dma("sp", cosc[:], C["cosC"][:], reads=[C["cosC"]], writes=[cosc])
    fw.dma("sp", sinc[:], C["sinC"][:], reads=[C["sinC"]], writes=[sinc])
    biasS = fw.sb([128, 4], F32, "biasS")
    for kv in range(2):
        for hc in range(2):
            b = nextbank()
            for jx in range(32):
                mm(bk(b)[:, 0:1], w1[kv][:, jx, hc * 128:(hc + 1) * 128], peT[kv][:, jx:jx + 1], jx == 0, jx == 31, [w1[kv], peT[kv]], [banks[b]])
            fw.op("act", lambda e: e.copy(biasS[:, kv * 2 + hc:kv * 2 + hc + 1], bk(b)[:, 0:1]), reads=[banks[b]], writes=[biasS])
    fw.op("dve", lambda e: e.memset(kcmpT[:], 0.0), writes=[kcmpT])
    fw.op("dve", lambda e: e.memset(vcmp[:], 0.0), writes=[vcmp])
    fw.op("dve", lambda e: e.memset(vcmp[:, :, :, 128:129], 1.0), writes=[vcmp])
    hidTs = [fw.sb([128, 2, 256], BF16, f"hidT{i}") for i in range(2)]
    ropc = [fw.sb([128, 256], F32, f"ropc{i}") for i in range(2)]
    for kv in range(2):
        for g in range(2):
            hidT = hidTs[(kv * 2 + g) % 2]
            for hc in range(2):
                b = nextbank()
                for jx in range(32):
                    mm(bk(b)[:, 0:255], w1[kv][:, jx, hc * 128:(hc + 1) * 128], cmpraw[:, kv * 2 + g, jx:jx + 16 * 254 + 1:16],
                       jx == 0, jx == 31, [w1[kv], cmpraw], [banks[b]])
                fw.op("act", lambda e: e.activation(hidT[:, hc, 0:255], bk(b)[:, 0:255], AF.Gelu_apprx_tanh,
                                                    bias=biasS[:, kv * 2 + hc:kv * 2 + hc + 1]),
                      reads=[banks[b], biasS], writes=[hidT])
            if kv == 0:
                b1 = nextbank()
                for hc in range(2):
                    mm(bk(b1)[:, 0:255], w2k[:, hc, :], hidT[:, hc, 0:255], hc == 0, hc == 1, [w2k, hidT], [banks[b1]])
                b2 = nextbank()
                for hc in range(2):
                    mm(bk(b2)[:, 0:255], w2kr[:, hc, :], hidT[:, hc, 0:255], hc == 0, hc == 1, [w2kr, hidT], [banks[b2]])
                a, bb = ropc[0], ropc[1]
                fw.op("dve", lambda e: e.tensor_tensor(a[:, 0:255], bk(b1)[:, 0:255], cosc[:, 0:255], ALU.mult), reads=[banks[b1], cosc], writes=[a])
                fw.op("dve", lambda e: e.tensor_tensor(bb[:, 0:255], bk(b2)[:, 0:255], sinc[:, 0:255], ALU.mult), reads=[banks[b2], sinc], writes=[bb])
                fw.op("dve", lambda e: e.tensor_tensor(kcmpT[:, g, 0:255], a[:, 0:255], bb[:, 0:255], ALU.add), reads=[a, bb], writes=[kcmpT])
            else:
                for ch in range(2):
                    nn = 128 if ch == 0 else 127
                    b = nextbank()
                    for hc in range(2):
                        mm(bk(b)[0:nn, 0:128], hidT[:, hc, ch * 128:ch * 128 + nn], w2v[:, hc, :], hc == 0, hc == 1, [hidT, w2v], [banks[b]])
                    fw.op("act", lambda e: e.copy(vcmp[0:nn, ch, g, 0:128], bk(b)[0:nn, 0:128]), reads=[banks[b]], writes=[vcmp])

    if "B" in dbg:
        for nm, buf, shp, dt_ in (("qT", qT, [128, NB, 16, 128], BF16), ("gates", gates, [128, NB, 48], F32),
                                  ("kcmpT", kcmpT, [128, 2, 256], BF16), ("vcmp", vcmp, [128, 2, 2, 129], BF16)):
            dbg_out[nm] = fw.dram("dbg_" + nm, shp, dt_, kind="ExternalOutput")
            fw.dma("sp", dbg_out[nm].t.ap(), buf[:], reads=[buf], writes=[dbg_out[nm]])
        fw.finish(list(dbg_out.values()))
        return nc, fw


    fw.barrier()
    nc.sbuf_base = mark2
    o_nsaT = fw.sb([128, 16, NB * 128], BF16, "o_nsaT", side="right")

    def cload(name, shape, dt_, q="pool"):
        t = fw.sb(shape, dt_, name)
        fw.dma(q, t[:], C[name].t.ap(), reads=[C[name]], writes=[t])
        return t

    exall = cload("exall", [64, 32, 128], BF16)
    dmask = cload("dmask", [128, 4, 128], BF16)
    wmask = cload("wmask", [128, 8, 128], BF16)
    cmask = cload("cmask", [128, NB, 2, 128], BF16)
    selmap = cload("selmap", [128, 2, 64], BF16)
    selA = cload("selA", [128, NB, 64], F32, "sp")
    selB = cload("selB", [128, NB, 64], F32, "sp")
    ksl = [fw.sb([128, S], BF16, f"ksl{i}") for i in range(2)]
    vsl = [fw.sb([128, 32, 129], BF16, f"vsl{i}") for i in range(2)]
    kwn = [fw.sb([128, 1024], BF16, f"kwn{i}") for i in range(2)]
    vwn = [fw.sb([128, 8, 129], BF16, f"vwn{i}") for i in range(2)]
    for t in vsl + vwn:
        fw.op("dve", lambda e: e.memset(t[:, :, 128:129], 1.0), writes=[t])
    eTs = [fw.sb([128, 512], BF16, f"eT{i}") for i in range(4)]
    pTs = [fw.sb([128, 4, 128], BF16, f"pT{i}") for i in range(8)]
    mks = [fw.sb([128, 128], BF16, f"mk{i}") for i in range(2)]
    o_out = fw.sb([128, D], F32, "o_out")
    o_bf = fw.sb([128, D], BF16, "o_bf")
    rinv = fw.sb([128, 8], F32, "rinv")
    wgt = fw.sb([128, 8], F32, "wgt")
    imp = fw.sb([128, 64], F32, "imp")
    score = fw.sb([128, 64], F32, "score")
    tmpm = fw.sb([128, 64], F32, "tmpm")
    mx = fw.sb([128, 16], F32, "mx")
    sel_bf = fw.sb([128, 64], BF16, "sel_bf")
    selT = fw.sb([64, 128], BF16, "selT")
    MB = 4
    OB = (5, 6, 7)
    obufs = [banks[5], banks[6], banks[7]]
    only_br = None
    for d_ in dbg:
        if d_.startswith("C") and len(d_) == 2:
            only_br = int(d_[1])

    def oacc(h, lo=0, hi=129):
        return PS[:, 5 + h // 3, (h % 3) * 129 + lo:(h % 3) * 129 + hi]

    cnt = {"e": 0, "p": 0, "s": 0, "m": 0}

    NS = 4

    def scores(kT_ap, qblk, Rk, hf):
        b = cnt["s"] % NS
        cnt["s"] += 1
        mm(bk(b), kT_ap, qblk[:, hf * 4:(hf + 1) * 4, :], True, True, Rk + [qT], [banks[b]])
        eT = eTs[cnt["e"] % len(eTs)]
        cnt["e"] += 1
        fw.op("act", lambda e: e.activation(eT[:], bk(b), AF.Exp), reads=[banks[b]], writes=[eT])
        return eT

    def masked(eT, mask_ap, Rm):
        pT = pTs[cnt["p"] % len(pTs)]
        cnt["p"] += 1
        fw.op("dve", lambda e: e.tensor_tensor(pT[:], eT[:].rearrange("p (h t) -> p h t", h=4),
                                               mask_ap.unsqueeze(1).to_broadcast([128, 4, 128]), ALU.mult),
              reads=[eT] + Rm, writes=[pT])
        return pT

    def pv(pT, v_ap, Rv, first, last, hf):
        for h4 in range(4):
            h = hf * 4 + h4
            mm(oacc(h), pT[:, h4, :], v_ap, first and h % 3 == 0, last, [pT] + Rv, [obufs[h // 3]],
               inc=(h4 == 3), sgc=True)

    ocp = [fw.sb([128, 3, 387], F32, f"ocp{i}") for i in range(2)]
    pimp_sb = fw.sb([128, 512], F32, "pimp_sb")
    maskT = fw.sb([128, 32, 128], BF16, "maskT")
    ptmp = [fw.sb([128, 128], F32, f"ptmp{i}") for i in range(2)]
    ocnt = [0]

    def grab():
        oc = ocp[ocnt[0] % 2]
        ocnt[0] += 1
        for bnk in range(3):
            nh = 3 if bnk < 2 else 2
            if bnk == 1:
                fw.op("act", lambda e: e.copy(oc[:, bnk, 0:nh * 129], PS[:, 5 + bnk, 0:nh * 129]), reads=[obufs[bnk]], writes=[oc])
            else:
                fw.op("dve", lambda e: e.tensor_copy(oc[:, bnk, 0:nh * 129], PS[:, 5 + bnk, 0:nh * 129]), reads=[obufs[bnk]], writes=[oc])
        return oc

    def fin_ops(i, g, br, oc):
        ops = []
        ocv = oc[:].rearrange("p b (h c) -> p b h c", c=129)

        def och(h, lo, hi):
            return oc[:, h // 3, (h % 3) * 129 + lo:(h % 3) * 129 + hi]

        def f_rs():
            for bnk in range(3):
                nh = 3 if bnk < 2 else 2
                fw.op("dve", lambda e: e.tensor_scalar(rinv[:, bnk * 3:bnk * 3 + nh], ocv[:, bnk, 0:nh, 128], 1e-30, None, ALU.max),
                      reads=[oc], writes=[rinv])
            fw.op("dve", lambda e: e.reciprocal(rinv[:], rinv[:]), reads=[rinv], writes=[rinv])
            if only_br is None:
                gv = gates[:, i, g * 24 + br:g * 24 + 24:3]
                fw.op("dve", lambda e: e.tensor_tensor(wgt[:], rinv[:], gv, ALU.mult), reads=[rinv, gates], writes=[wgt])
            elif only_br == br:
                fw.op("dve", lambda e: e.tensor_copy(wgt[:], rinv[:]), reads=[rinv], writes=[wgt])
            else:
                fw.op("dve", lambda e: e.memset(wgt[:], 0.0), writes=[wgt])
        ops.append(f_rs)
        for h in range(8):
            def f_h(h=h):
                dst = o_out[:, (g * 8 + h) * 128:(g * 8 + h + 1) * 128]
                if br == 0:
                    fw.op("dve", lambda e: e.tensor_scalar(dst, och(h, 0, 128), wgt[:, h:h + 1], None, ALU.mult),
                          reads=[oc, wgt], writes=[o_out])
                else:
                    fw.op("dve", lambda e: e.scalar_tensor_tensor(dst, och(h, 0, 128), wgt[:, h:h + 1], dst, ALU.mult, ALU.add),
                          reads=[oc, wgt, o_out], writes=[o_out])
            ops.append(f_h)
        return ops

    def topk_ops(i):
        ops = []

        def f0():
            fw.op("act", lambda e: e.copy(pimp_sb[:], bk(MB)), reads=[banks[MB]], writes=[pimp_sb])
        ops.append(f0)
        for h in range(8):
            def f(h=h):
                src = pimp_sb[:, h * 64:(h + 1) * 64]
                if h == 0:
                    fw.op("dve", lambda e: e.tensor_scalar(imp[:], src, rinv[:, 0:1], None, ALU.mult), reads=[pimp_sb, rinv], writes=[imp])
                else:
                    fw.op("dve", lambda e: e.scalar_tensor_tensor(imp[:], src, rinv[:, h:h + 1], imp[:], ALU.mult, ALU.add),
                          reads=[pimp_sb, rinv, imp], writes=[imp])
            ops.append(f)

        def f1():
            fw.op("dve", lambda e: e.tensor_tensor(score[:], imp[:], selA[:, i, :], ALU.mult), reads=[imp, selA], writes=[score])
            fw.op("dve", lambda e: e.tensor_tensor(score[:], score[:], selB[:, i, :], ALU.add), reads=[score, selB], writes=[score])
            fw.op("dve", lambda e: e.max(out=mx[:, 0:8], in_=score[:]), reads=[score], writes=[mx])
        ops.append(f1)

        def f2():
            fw.op("dve", lambda e: e.match_replace(out=tmpm[:], in_to_replace=mx[:, 0:8], in_values=score[:], imm_value=-1e30),
                  reads=[score, mx], writes=[tmpm])
            fw.op("dve", lambda e: e.max(out=mx[:, 8:16], in_=tmpm[:]), reads=[tmpm], writes=[mx])
            fw.op("dve", lambda e: e.tensor_scalar(sel_bf[:], score[:], mx[:, 15:16], None, ALU.is_ge), reads=[score, mx], writes=[sel_bf])
        ops.append(f2)

        def f3():
            fw.op("pe", lambda e: e.transpose(bkbf(MB)[0:64, 0:128], sel_bf[:], ident[:]), reads=[sel_bf, ident], writes=[banks[MB]])
            fw.op("act", lambda e: e.copy(selT[:], bkbf(MB)[0:64, 0:128]), reads=[banks[MB]], writes=[selT])
        ops.append(f3)
        return ops

    pending = []

    def drain(n):
        for _ in range(min(n, len(pending))):
            pending.pop(0)()

    SKEW = 3

    def run_tiles(fronts, backs, per_tile=2):
        live = []
        for k in range(len(fronts)):
            live.append(fronts[k]())
            if k >= SKEW:
                backs[k - SKEW](live[k - SKEW])
            drain(per_tile)
        for k in range(max(len(fronts) - SKEW, 0), len(fronts)):
            backs[k](live[k])

    kT_all = kT_d.t.ap()
    v_all = v_d.t.ap()

    def kv_loads(i, g):
        it = i * 2 + g
        nsl = 4 * i + 4
        w0 = max(4 * i - 4, 0)
        nw = 4 * i + 4 - w0
        ks, vs, kw, vw = ksl[it % 2], vsl[it % 2], kwn[it % 2], vwn[it % 2]
        fw.dma("sp", ks[:, 0:nsl * 128], kT_all[g, :, 0:nsl * 128], reads=[kT_d], writes=[ks])
        fw.dma("sp", vs[:, 0:nsl, 0:128], v_all[0:nsl * 128, g * 128:(g + 1) * 128].rearrange("(t p) d -> p t d", p=128),
               reads=[v_d], writes=[vs])
        fw.dma("sp", kw[:, 0:nw * 128], kT_all[2 + g, :, w0 * 128:(w0 + nw) * 128], reads=[kT_d], writes=[kw])
        fw.dma("sp", vw[:, 0:nw, 0:128],
               v_all[w0 * 128:(w0 + nw) * 128, 256 + g * 128:256 + (g + 1) * 128].rearrange("(t p) d -> p t d", p=128),
               reads=[v_d], writes=[vw])

    def block_out(i):
        def f():
            if "C" in dbg or only_br is not None:
                if "o" not in dbg_out:
                    dbg_out["o"] = fw.dram("dbg_o", [NB, 128, D], F32, kind="ExternalOutput")
                fw.dma("sp", dbg_out["o"].t.ap()[i], o_out[:], reads=[o_out], writes=[dbg_out["o"]])
            fw.op("act", lambda e: e.copy(o_bf[:], o_out[:]), reads=[o_out], writes=[o_bf])
            for half in range(2):
                b = MB
                for k8 in range(8):
                    kc = half * 8 + k8
                    fw.op("pe", lambda e: e.transpose(bkbf(b)[:, k8 * 128:(k8 + 1) * 128], o_bf[:, kc * 128:(kc + 1) * 128], ident[:]),
                          reads=[o_bf, ident], writes=[banks[b]], inc=(k8 == 7))
                fw.op("act", lambda e: e.copy(o_nsaT[:, half * 8:(half + 1) * 8, i * 128:(i + 1) * 128],
                                              bkbf(b).rearrange("p (k t) -> p k t", k=8)),
                      reads=[banks[b]], writes=[o_nsaT])
        return f

    units = []

    def add_iter(i, g):
        it = i * 2 + g
        nsl = 4 * i + 4
        w0 = max(4 * i - 4, 0)
        nw = 4 * i + 4 - w0
        ks, vs, kw, vw = ksl[it % 2], vsl[it % 2], kwn[it % 2], vwn[it % 2]
        qblk = qT[:, i, g * 8:(g + 1) * 8, :]
        nch = 1 if i < 4 else 2
        pcs = {}
        first = len(units)

        def c_front(ch, hf):
            eT = scores(kcmpT[:, g, ch * 128:(ch + 1) * 128], qblk, [kcmpT], hf)
            p_ = masked(eT, cmask[:, i, ch, :], [cmask])
            pcs[(ch, hf)] = p_
            return p_

        def c_post():
            drain(len(pending))
            for h in range(8):
                for ch in range(nch):
                    mm(bk(MB)[:, h * 64:(h + 1) * 64], pcs[(ch, h // 4)][:, h % 4, :], selmap[:, ch, :], ch == 0 and h == 0, ch == nch - 1,
                       [pcs[(ch, h // 4)], selmap], [banks[MB]], inc=(h == 7 and ch == nch - 1) or None, sgc=True)
            drain(len(pending))
            oc = grab()
            pending.extend(fin_ops(i, g, 0, oc))
            pending.extend(topk_ops(i))
            for q4 in range(i + 1):
                def f_mask(q4=q4):
                    for k4 in range(4):
                        mm(bk(MB)[:, k4 * 128:(k4 + 1) * 128], exall[:, q4 * 4 + k4, :], selT[:], True, True, [exall, selT], [banks[MB]],
                           inc=(k4 == 3))
                    src = bk(MB).rearrange("p (k t) -> p k t", k=4)
                    if q4 == i:
                        fw.op("dve", lambda e: e.tensor_tensor(maskT[:, q4 * 4:(q4 + 1) * 4, :], src, dmask[:], ALU.mult),
                              reads=[banks[MB], dmask], writes=[maskT])
                    else:
                        fw.op("act", lambda e: e.copy(maskT[:, q4 * 4:(q4 + 1) * 4, :], src), reads=[banks[MB]], writes=[maskT])
                pending.append(f_mask)

        cu = [(ch, hf) for ch in range(nch) for hf in range(2)]
        for n_, (ch, hf) in enumerate(cu):
            units.append(dict(pre=None, front=(lambda ch=ch, hf=hf: c_front(ch, hf)),
                              back=(lambda p_, ch=ch, hf=hf: pv(p_, vcmp[:, ch, g, :], [vcmp], ch == 0, ch == nch - 1, hf)),
                              post=c_post if n_ == len(cu) - 1 else None))

        def w_front(r_, hf):
            eT = scores(kw[:, r_ * 128:(r_ + 1) * 128], qblk, [kw], hf)
            return masked(eT, wmask[:, w0 + r_ - (4 * i - 4), :], [wmask])

        def w_post():
            drain(len(pending))
            oc = grab()
            pending.extend(fin_ops(i, g, 2, oc))

        wu = [(r_, hf) for r_ in range(nw) for hf in range(2)]
        for n_, (r_, hf) in enumerate(wu):
            units.append(dict(pre=None, front=(lambda r_=r_, hf=hf: w_front(r_, hf)),
                              back=(lambda p_, r_=r_, hf=hf: pv(p_, vw[:, r_, :], [vw], r_ == 0, r_ == nw - 1, hf)),
                              post=w_post if n_ == len(wu) - 1 else None))

        def s_front(kt, hf):
            eT = scores(ks[:, kt * 128:(kt + 1) * 128], qblk, [ks], hf)
            return masked(eT, maskT[:, kt, :], [maskT])

        def s_post():
            drain(len(pending))
            oc = grab()
            pending.extend(fin_ops(i, g, 1, oc))
            if g == 1:
                pending.append(block_out(i))

        su = [(kt, hf) for kt in range(nsl) for hf in range(2)]
        for n_, (kt, hf) in enumerate(su):
            units.append(dict(pre=(lambda: drain(len(pending))) if n_ == 0 else None,
                              front=(lambda kt=kt, hf=hf: s_front(kt, hf)),
                              back=(lambda p_, kt=kt, hf=hf: pv(p_, vs[:, kt, :], [vs], kt == 0, kt == nsl - 1, hf)),
                              post=s_post if n_ == len(su) - 1 else None))
        if it + 1 < 2 * NB:
            ni, ng = (it + 1) // 2, (it + 1) % 2
            old_pre = units[first + 6]["pre"]

            def pre6(old_pre=old_pre, ni=ni, ng=ng):
                if old_pre is not None:
                    old_pre()
                kv_loads(ni, ng)
            units[first + 6]["pre"] = pre6

    for i in range(NB):
        for g in range(2):
            add_iter(i, g)
    kv_loads(0, 0)
    live = {}

    def do_back(k):
        units[k]["back"](live.pop(k))
        if units[k]["post"] is not None:
            units[k]["post"]()

    for k, u in enumerate(units):
        if u["pre"] is not None:
            u["pre"]()
        live[k] = u["front"]()
        if k >= SKEW:
            do_back(k - SKEW)
        drain(2)
    for k in range(len(units) - SKEW, len(units)):
        do_back(k)
    drain(len(pending))

    if "C" in dbg or only_br is not None:
        fw.finish(list(dbg_out.values()))
        return nc, fw


    BASE = 16512 + 1024

    def at(kib, shape, dt_, name):
        assert (kib * 1024) % 32 == 0, (name, kib)
        fw.nsb += 1
        return Buf(nc.alloc_sbuf_tensor_at(f"{name}_{fw.nsb}", list(shape), dt_, offset=BASE + int(kib * 1024)), name)

    fw.barrier()
    xTo = at(0, [128, 16, NB * 128], BF16, "xTo2")
    uT = at(32, [128, 16, NB * 128], BF16, "uT")
    vtm = at(64, [128, NB, D], BF16, "vtm")
    wst = [at(96, [128, 16, 512], BF16, "wst0"), at(112, [128, 16, 512], BF16, "wst1")]
    xbo2 = [at(128, [128, D], BF16, "xbo2a"), at(132, [128, D], BF16, "xbo2b")]
    wsT_f = at(136, [128, 8, 128], F32, "wsT_f")
    tri = at(140, [128, 128], F32, "tri")
    wcT = at(140.5, [128, 8, 128], BF16, "wcT")
    addt = at(142.5, [128, 16, 128], F32, "addt")
    gcol = at(150.5, [128, 16], F32, "gcol")
    bcol = at(150.75, [128, 16], F32, "bcol")
    bsrow = at(151, [1, 8 * 128], BF16, "bsrow")
    onesr = at(153, [128, 128], BF16, "onesr")
    stats = at(153.5, [128, 4, 6], F32, "stats")
    mv = at(154, [128, 2], F32, "mv")
    rstd = at(154.25, [128, 1], F32, "rstd")
    tmpz = [at(155, [128, 128], F32, "tmpz0"), at(155.5, [128, 128], F32, "tmpz1")]
    for blk in range(NB):
        load_xT(xo_d, blk * 128, xbo2[blk % 2], xTo, blk * 128, "act" if blk % 2 == 0 else "dve")
    fw.dma("sp", wsT_f[:], sgu_wsT.t.ap().rearrange("g s t -> s g t"), reads=[sgu_wsT], writes=[wsT_f])
    fw.dma("sp", tri[:], C["tri"][:], reads=[C["tri"]], writes=[tri])
    fw.dma("sp", gcol[:], sgu_g_fm[:], reads=[sgu_g_fm], writes=[gcol])
    fw.dma("sp", bcol[:], sgu_b_fm[:], reads=[sgu_b_fm], writes=[bcol])
    fw.dma("pool", bsrow[:], sgu_bs[:], reads=[sgu_bs], writes=[bsrow])
    fw.op("dve", lambda e: e.memset(onesr[:], 1.0), writes=[onesr])
    fw.op("dve", lambda e: e.tensor_tensor(wcT[:], wsT_f[:], tri[:].unsqueeze(1).to_broadcast([128, 8, 128]), ALU.mult),
          reads=[wsT_f, tri], writes=[wcT])
    for g8 in range(8):
        b = nextbank()
        mm(bk(b)[:, 0:128], onesr[:], wcT[:, g8, :], True, True, [onesr, wcT], [banks[b]])
        b2 = nextbank()
        mm(bk(b2)[:, 0:128], onesr[0:1, :], bsrow[0:1, g8 * 128:(g8 + 1) * 128], True, True, [onesr, bsrow], [banks[b2]])
        for f2 in range(2):
            fc = g8 * 2 + f2
            fw.op("dve", lambda e: e.tensor_scalar(addt[:, fc, :], bk(b)[:, 0:128], bcol[:, fc:fc + 1], None, ALU.mult),
                  reads=[banks[b], bcol], writes=[addt])
            fw.op("dve", lambda e: e.tensor_tensor(addt[:, fc, :], addt[:, fc, :], bk(b2)[:, 0:128], ALU.add),
                  reads=[banks[b2], addt], writes=[addt])
    for cg in range(4):
        w = wst[cg % 2]
        fw.dma("pool", w[:], w_in_r[:, :, OFF_V + cg * 512:OFF_V + (cg + 1) * 512], reads=[w_in], writes=[w])
        for blk in range(NB):
            b = nextbank()
            for kc in range(16):
                mm(bk(b), xTo[:, kc, blk * 128:(blk + 1) * 128], w[:, kc, :], kc == 0, kc == 15, [xTo, w], [banks[b]])
            fw.op("act", lambda e: e.activation(vtm[:, blk, cg * 512:(cg + 1) * 512], bk(b), AF.Gelu_apprx_tanh),
                  reads=[banks[b]], writes=[vtm])
    for blk in range(NB):
        for c4 in range(4):
            fw.op("dve", lambda e: e.bn_stats(stats[:, c4, :], vtm[:, blk, c4 * 512:(c4 + 1) * 512]), reads=[vtm], writes=[stats])
        fw.op("dve", lambda e: e.bn_aggr(mv[:], stats[:]), reads=[stats], writes=[mv])
        fw.op("act", lambda e: e.activation(rstd[:], mv[:, 1:2], AF.Sqrt, bias=epsc[:]), reads=[mv, epsc], writes=[rstd])
        fw.op("dve", lambda e: e.reciprocal(rstd[:], rstd[:]), reads=[rstd], writes=[rstd])
        fw.op("dve", lambda e: e.tensor_scalar(vtm[:, blk, :], vtm[:, blk, :], mv[:, 0:1], rstd[:, 0:1], ALU.subtract, ALU.mult),
              reads=[vtm, mv, rstd], writes=[vtm])
    for cg in range(4):
        w = wst[cg % 2]
        fw.dma("pool", w[:], w_in_r[:, :, OFF_U + cg * 512:OFF_U + (cg + 1) * 512], reads=[w_in], writes=[w])
        for half in range(2):
            for f4 in range(4):
                b = nextbank()
                for kc in range(16):
                    mm(bk(b), w[:, kc, f4 * 128:(f4 + 1) * 128], xTo[:, kc, half * 512:(half + 1) * 512], kc == 0, kc == 15, [w, xTo], [banks[b]])
                fw.op("act", lambda e: e.activation(uT[:, cg * 4 + f4, half * 512:(half + 1) * 512], bk(b), AF.Gelu_apprx_tanh),
                      reads=[banks[b]], writes=[uT])
    zi = 0
    for blk in range(NB):
        for q4 in range(4):
            b = nextbank()
            for f4 in range(4):
                fc = q4 * 4 + f4
                mm(bk(b)[:, f4 * 128:(f4 + 1) * 128], vtm[:, blk, fc * 128:(fc + 1) * 128], wcT[:, fc // 2, :], True, True,
                   [vtm, wcT], [banks[b]], inc=(f4 == 3))
            for f4 in range(4):
                fc = q4 * 4 + f4
                tz = tmpz[zi % 2]
                zi += 1
                fw.op("dve", lambda e: e.scalar_tensor_tensor(tz[:], bk(b)[:, f4 * 128:(f4 + 1) * 128], gcol[:, fc:fc + 1], addt[:, fc, :],
                                                              ALU.mult, ALU.add), reads=[banks[b], gcol, addt], writes=[tz])
                fw.op("pool", lambda e: e.tensor_tensor(uT[:, fc, blk * 128:(blk + 1) * 128], uT[:, fc, blk * 128:(blk + 1) * 128], tz[:], ALU.mult),
                      reads=[uT, tz], writes=[uT])
    if "D" in dbg:
        dbg_out["osguT"] = fw.dram("dbg_osguT", [128, 16, NB * 128], BF16, kind="ExternalOutput")
        fw.dma("sp", dbg_out["osguT"].t.ap(), uT[:], reads=[uT], writes=[dbg_out["osguT"]])
        fw.finish(list(dbg_out.values()))
        return nc, fw

    fw.barrier()
    mergedT = at(70, [128, 16, NB * 128], BF16, "mergedT")
    wm_ = [[at(102 + 8 * (2 * k + i2), [128, 16, 256], BF16, f"wm{k}{i2}") for i2 in range(2)] for k in range(2)]
    sg = [at(134, [128, 512], F32, "sg0"), at(136, [128, 512], F32, "sg1")]
    m1 = [at(138, [128, 512], F32, "m10"), at(140, [128, 512], F32, "m11")]
    wa_r, wb_r = r3(w_br_a), r3(w_br_b)
    ei = 0
    for ps_ in range(2):
        srcT = o_nsaT if ps_ == 0 else uT
        wr = wa_r if ps_ == 0 else wb_r
        goff = OFF_GA if ps_ == 0 else OFF_GB
        for cg in range(8):
            wA, wG = wm_[0][cg % 2], wm_[1][cg % 2]
            fw.dma("pool", wA[:], wr[:, :, cg * 256:(cg + 1) * 256], reads=[w_br_a, w_br_b], writes=[wA])
            fw.dma("pool", wG[:], w_in_r[:, :, goff + cg * 256:goff + (cg + 1) * 256], reads=[w_in], writes=[wG])
            for fc in range(2):
                for half in range(2):
                    ba = nextbank()
                    for kc in range(16):
                        mm(bk(ba), wA[:, kc, fc * 128:(fc + 1) * 128], srcT[:, kc, half * 512:(half + 1) * 512], kc == 0, kc == 15, [wA, srcT], [banks[ba]])
                    bg = nextbank()
                    for kc in range(16):
                        mm(bk(bg), wG[:, kc, fc * 128:(fc + 1) * 128], xTo[:, kc, half * 512:(half + 1) * 512], kc == 0, kc == 15, [wG, xTo], [banks[bg]])
                    s_, m_ = sg[ei % 2], m1[ei % 2]
                    ei += 1
                    dst = mergedT[:, cg * 2 + fc, half * 512:(half + 1) * 512]
                    fw.op("act", lambda e: e.activation(s_[:], bk(bg), AF.Sigmoid), reads=[banks[bg]], writes=[s_])
                    if ps_ == 0:
                        fw.op("dve", lambda e: e.tensor_tensor(dst, s_[:], bk(ba), ALU.mult), reads=[s_, banks[ba]], writes=[mergedT])
                    else:
                        fw.op("dve", lambda e: e.tensor_tensor(m_[:], s_[:], bk(ba), ALU.mult), reads=[s_, banks[ba]], writes=[m_])
                        fw.op("pool", lambda e: e.tensor_tensor(dst, dst, m_[:], ALU.add), reads=[m_, mergedT], writes=[mergedT])
    fw.barrier()
    x1 = at(102, [128, NB, D], F32, "x1")
    xT1 = at(166, [128, 16, NB * 128], BF16, "xT1")
    wo_ = [at(32, [128, 16, 512], BF16, "wo0"), at(48, [128, 16, 512], BF16, "wo1")]
    xst = [at(64 + 2 * i3, [128, 512], F32, f"xst{i3}") for i3 in range(3)]
    lnt = [at(0, [128, D], F32, "lnt0"), at(8, [128, D], F32, "lnt1")]
    lng = at(16, [128, D], F32, "lng")
    lnb = at(24, [128, D], F32, "lnb")
    stats2 = at(198, [128, 4, 6], F32, "stats2")
    mv2 = at(198.125, [128, 2], F32, "mv2")
    rstd2 = at(198.25, [128, 1], F32, "rstd2")
    xbf = [at(198.5, [128, D], BF16, "xbf0"), at(202.5, [128, D], BF16, "xbf1")]
    wo_r = r3(w_o_d)

    def layer_norm(acc, g_d, b_d, xT_out, out_dram=None):
        fw.dma("sp", lng[:], g_d.t.ap().to_broadcast([128, D]), reads=[g_d], writes=[lng])
        fw.dma("sp", lnb[:], b_d.t.ap().to_broadcast([128, D]), reads=[b_d], writes=[lnb])
        for blk in range(NB):
            t_ = lnt[blk % 2]
            for c4 in range(4):
                fw.op("dve", lambda e: e.bn_stats(stats2[:, c4, :], acc[:, blk, c4 * 512:(c4 + 1) * 512]), reads=[acc], writes=[stats2])
            fw.op("dve", lambda e: e.bn_aggr(mv2[:], stats2[:]), reads=[stats2], writes=[mv2])
            fw.op("act", lambda e: e.activation(rstd2[:], mv2[:, 1:2], AF.Sqrt, bias=epsc[:]), reads=[mv2, epsc], writes=[rstd2])
            fw.op("dve", lambda e: e.reciprocal(rstd2[:], rstd2[:]), reads=[rstd2], writes=[rstd2])
            fw.op("dve", lambda e: e.tensor_scalar(t_[:], acc[:, blk, :], mv2[:, 0:1], rstd2[:, 0:1], ALU.subtract, ALU.mult),
                  reads=[acc, mv2, rstd2], writes=[t_])
            fw.op("dve", lambda e: e.tensor_tensor(t_[:], t_[:], lng[:], ALU.mult), reads=[t_, lng], writes=[t_])
            if out_dram is None:
                fw.op("pool", lambda e: e.tensor_tensor(acc[:, blk, :], t_[:], lnb[:], ALU.add), reads=[t_, lnb], writes=[acc])
            else:
                fw.op("pool", lambda e: e.tensor_tensor(t_[:], t_[:], lnb[:], ALU.add), reads=[t_, lnb], writes=[t_])
                fw.dma("sp", out_dram.t.ap()[blk * 128:(blk + 1) * 128, :], t_[:], reads=[t_], writes=[out_dram])
            if xT_out is not None:
                xb_ = xbf[blk % 2]
                fw.op("act", lambda e: e.copy(xb_[:], acc[:, blk, :]), reads=[acc], writes=[xb_])
                for half in range(2):
                    b = nextbank()
                    for k8 in range(8):
                        kc = half * 8 + k8
                        fw.op("pe", lambda e: e.transpose(bkbf(b)[:, k8 * 128:(k8 + 1) * 128], xb_[:, kc * 128:(kc + 1) * 128], ident[:]),
                              reads=[xb_, ident], writes=[banks[b]], inc=(k8 == 7))
                    fw.op("act", lambda e: e.copy(xT_out[:, half * 8:(half + 1) * 8, blk * 128:(blk + 1) * 128],
                                                  bkbf(b).rearrange("p (k t) -> p k t", k=8)), reads=[banks[b]], writes=[xT_out])

    xi = 0
    for cg in range(4):
        w = wo_[cg % 2]
        fw.dma("pool", w[:], wo_r[:, :, cg * 512:(cg + 1) * 512], reads=[w_o_d], writes=[w])
        for blk in range(NB):
            xs_ = xst[xi % 3]
            xi += 1
            fw.dma("sp", xs_[:], xo_d[blk * 128:(blk + 1) * 128, cg * 512:(cg + 1) * 512], reads=[xo_d], writes=[xs_])
            b = nextbank()
            for kc in range(16):
                mm(bk(b), mergedT[:, kc, blk * 128:(blk + 1) * 128], w[:, kc, :], kc == 0, kc == 15, [mergedT, w], [banks[b]])
            fw.op("dve", lambda e: e.scalar_tensor_tensor(x1[:, blk, cg * 512:(cg + 1) * 512], xs_[:], ALPHA, bk(b), ALU.mult, ALU.add),
                  reads=[xs_, banks[b]], writes=[x1])
    layer_norm(x1, ln1_g, ln1_b, xT1)
    if "E" in dbg:
        dbg_out["x1"] = fw.dram("dbg_x1", [128, NB, D], F32, kind="ExternalOutput")
        fw.dma("sp", dbg_out["x1"].t.ap(), x1[:], reads=[x1], writes=[dbg_out["x1"]])
        fw.finish(list(dbg_out.values()))
        return nc, fw


    fw.barrier()
    wxk = at(32, [128, 16, 512], BF16, "wxk")
    wxv = at(48, [128, 16, 512], BF16, "wxv")
    memb = [at(64, [128, D], BF16, "memb0"), at(68, [128, D], BF16, "memb1")]
    memT = at(72, [128, 16, 256], BF16, "memT")
    KmT = at(80, [128, 4, 256], BF16, "KmT")
    Vm = at(82, [128, 2, 4, 129], BF16, "Vm")
    fw.dma("pool", wxk[:], r3(w_xk), reads=[w_xk], writes=[wxk])
    fw.dma("pool", wxv[:], r3(w_xv), reads=[w_xv], writes=[wxv])
    for mb in range(2):
        load_xT(mem_d, mb * 128, memb[mb], memT, mb * 128, "act" if mb == 0 else "dve")
    fw.op("dve", lambda e: e.memset(Vm[:, :, :, 128:129], 1.0), writes=[Vm])
    for h in range(4):
        b = nextbank()
        for kc in range(16):
            mm(bk(b)[:, 0:256], wxk[:, kc, h * 128:(h + 1) * 128], memT[:, kc, :], kc == 0, kc == 15, [wxk, memT], [banks[b]])
        fw.op("act", lambda e: e.copy(KmT[:, h, :], bk(b)[:, 0:256]), reads=[banks[b]], writes=[KmT])
    for mch in range(2):
        b = nextbank()
        for kc in range(16):
            mm(bk(b), memT[:, kc, mch * 128:(mch + 1) * 128], wxv[:, kc, :], kc == 0, kc == 15, [memT, wxv], [banks[b]])
        fw.op("dve", lambda e: e.tensor_copy(Vm[:, mch, :, 0:128], bk(b).rearrange("p (h d) -> p h d", h=4)), reads=[banks[b]], writes=[Vm])
    fw.barrier()
    wxq = at(32, [128, 16, 512], BF16, "wxq")
    wxo = at(48, [128, 4, D], BF16, "wxo")
    qxT = at(64, [128, 4, NB * 128], BF16, "qxT")
    eTx = [at(72, [128, 2, 128], BF16, "eTx0"), at(72.5, [128, 2, 128], BF16, "eTx1")]
    oxb = at(75, [128, 512], BF16, "oxb")
    oxT = at(76, [128, 4, 128], BF16, "oxT")
    rinvx = at(77, [128, 4], F32, "rinvx")
    fw.dma("pool", wxq[:], r3(w_xq), reads=[w_xq], writes=[wxq])
    fw.dma("pool", wxo[:], r3(w_xo), reads=[w_xo], writes=[wxo])
    for h in range(4):
        for half in range(2):
            b = nextbank()
            for kc in range(16):
                mm(bk(b), wxq[:, kc, h * 128:(h + 1) * 128], xT1[:, kc, half * 512:(half + 1) * 512], kc == 0, kc == 15, [wxq, xT1], [banks[b]])
            fw.op("act", lambda e: e.mul(qxT[:, h, half * 512:(half + 1) * 512], bk(b), 128.0 ** -0.5), reads=[banks[b]], writes=[qxT])
    exi = 0
    for blk in range(NB):
        bA, bB = nextbank(), nextbank()

        def ox_ps(h, lo=0, hi=129):
            return PS[:, bA if h < 3 else bB, (h % 3) * 129 + lo:(h % 3) * 129 + hi]

        for h in range(4):
            b = nextbank()
            for mch in range(2):
                mm(bk(b)[:, mch * 128:(mch + 1) * 128], KmT[:, h, mch * 128:(mch + 1) * 128], qxT[:, h, blk * 128:(blk + 1) * 128],
                   True, True, [KmT, qxT], [banks[b]], inc=(mch == 1))
            et = eTx[exi % 2]
            exi += 1
            fw.op("act", lambda e: e.activation(et[:], bk(b)[:, 0:256].rearrange("p (c t) -> p c t", c=2), AF.Exp), reads=[banks[b]], writes=[et])
            for mch in range(2):
                mm(ox_ps(h), et[:, mch, :], Vm[:, mch, h, :], mch == 0, mch == 1, [et, Vm], [banks[bA if h < 3 else bB]])
        vA = PS[:, bA, 0:387].rearrange("p (h c) -> p h c", c=129)
        fw.op("dve", lambda e: e.reciprocal(rinvx[:, 0:3], vA[:, :, 128]), reads=[banks[bA]], writes=[rinvx])
        fw.op("dve", lambda e: e.reciprocal(rinvx[:, 3:4], PS[:, bB, 128:129]), reads=[banks[bB]], writes=[rinvx])
        for h in range(4):
            fw.op("dve", lambda e: e.tensor_scalar(oxb[:, h * 128:(h + 1) * 128], ox_ps(h, 0, 128), rinvx[:, h:h + 1], None, ALU.mult),
                  reads=[banks[bA if h < 3 else bB], rinvx], writes=[oxb])
        b = nextbank()
        for h in range(4):
            fw.op("pe", lambda e: e.transpose(bkbf(b)[:, h * 128:(h + 1) * 128], oxb[:, h * 128:(h + 1) * 128], ident[:]),
                  reads=[oxb, ident], writes=[banks[b]], inc=(h == 3))
        fw.op("act", lambda e: e.copy(oxT[:], bkbf(b)[:, 0:512].rearrange("p (k t) -> p k t", k=4)), reads=[banks[b]], writes=[oxT])
        for cg in range(4):
            b = nextbank()
            for kc in range(4):
                mm(bk(b), oxT[:, kc, :], wxo[:, kc, cg * 512:(cg + 1) * 512], kc == 0, kc == 3, [oxT, wxo], [banks[b]])
            sl = x1[:, blk, cg * 512:(cg + 1) * 512]
            fw.op("dve", lambda e: e.scalar_tensor_tensor(sl, sl, ALPHA, bk(b), ALU.mult, ALU.add), reads=[x1, banks[b]], writes=[x1])
    layer_norm(x1, ln2_g, ln2_b, None)
    if "F" in dbg:
        dbg_out["x2"] = fw.dram("dbg_x2", [128, NB, D], F32, kind="ExternalOutput")
        fw.dma("sp", dbg_out["x2"].t.ap(), x1[:], reads=[x1], writes=[dbg_out["x2"]])
        fw.finish(list(dbg_out.values()))
        return nc, fw

    fw.barrier()
    acc = x1
    x2b = at(166, [128, NB, D], BF16, "x2b")
    posm_f = at(96, [128, NB, 64], F32, "posm_f")
    GWb = at(98, [128, NB, 64], BF16, "GWb")
    posmT = at(99, [64, NB * 128], BF16, "posmT")
    iota_c = at(101, [128, 64], F32, "iota_c")
    iota_pb = at(101.25, [64, 64], F32, "iota_pb")
    iota_p1 = at(101.5, [64, 1], F32, "iota_p1")
    x2hT = at(32, [128, 16, 128], BF16, "x2hT")
    x2lT = at(36, [128, 16, 128], BF16, "x2lT")
    wr = at(40, [128, 16, 68], F32, "wr")
    wrh = at(51, [128, 16, 68], BF16, "wrh")
    wrl = at(54, [128, 16, 68], BF16, "wrl")
    wtmp = at(57, [128, 16, 68], F32, "wtmp")
    x2l = at(62, [128, D], BF16, "x2l")
    bias_b = at(66, [128, 68], F32, "bias_b")
    logit = at(68, [128, NB, 68], F32, "logit")
    asg = at(46, [128, NB, 64], BF16, "asg")
    ustr = at(47, [128, 128], BF16, "ustr")
    ones_b = at(47.25, [128, 128], BF16, "ones_b")
    msk = at(71, [128, NB, 64], F32, "msk")
    msk2 = at(73, [128, NB, 64], F32, "msk2")
    oh1 = at(75, [128, NB, 64], F32, "oh1")
    oh2 = at(77, [128, NB, 64], F32, "oh2")
    d4 = at(79, [128, NB, 4], F32, "d4")
    e4 = at(79.125, [128, NB, 4], F32, "e4")
    gm = at(79.25, [128, NB, 4], F32, "gm")
    pen = at(79.375, [128, NB, 4], F32, "pen")
    sA_ = at(79.5, [128, NB], F32, "sA_")
    sB_ = at(79.53125, [128, NB], F32, "sB_")
    ggrp = at(79.5625, [128, NB], F32, "ggrp")
    m1_ = at(79.59375, [128, NB], F32, "m1_")
    m2_ = at(79.625, [128, NB], F32, "m2_")
    e2_ = at(79.65625, [128, NB], F32, "e2_")
    g1_ = at(79.6875, [128, NB], F32, "g1_")
    g2_ = at(79.71875, [128, NB], F32, "g2_")
    posb = at(50, [128, 64], BF16, "posb")
    fw.dma("sp", wr[:], r3(w_rt), reads=[w_rt], writes=[wr])
    fw.dma("sp", bias_b[:], b_rt.t.ap().to_broadcast([128, 68]), reads=[b_rt], writes=[bias_b])
    fw.dma("sp", iota_c[:], C["iota_c"][:], reads=[C["iota_c"]], writes=[iota_c])
    fw.dma("sp", iota_pb[:], C["iota_pb"][:], reads=[C["iota_pb"]], writes=[iota_pb])
    fw.op("dve", lambda e: e.tensor_copy(iota_p1[:], iota_pb[:, 0:1]), reads=[iota_pb], writes=[iota_p1])
    fw.dma("pool", ustr[:], C["ustrict"][:], reads=[C["ustrict"]], writes=[ustr])
    fw.op("dve", lambda e: e.memset(ones_b[:], 1.0), writes=[ones_b])
    fw.op("dve", lambda e: e.tensor_copy(wrh[:], wr[:]), reads=[wr], writes=[wrh])
    fw.op("dve", lambda e: e.tensor_copy(wtmp[:], wrh[:]), reads=[wrh], writes=[wtmp])
    fw.op("dve", lambda e: e.tensor_tensor(wrl[:], wr[:], wtmp[:], ALU.subtract), reads=[wr, wtmp], writes=[wrl])
    for blk in range(NB):
        fw.op("act", lambda e: e.copy(x2b[:, blk, :], acc[:, blk, :]), reads=[acc], writes=[x2b])
        fw.op("dve", lambda e: e.tensor_tensor(x2l[:], acc[:, blk, :], x2b[:, blk, :], ALU.subtract), reads=[acc, x2b], writes=[x2l])
        for (srcb, src_ap, dstT) in ((x2b, x2b[:, blk, :], x2hT), (x2l, x2l[:], x2lT)):
            for half in range(2):
                b = nextbank()
                for k8 in range(8):
                    kc = half * 8 + k8
                    fw.op("pe", lambda e: e.transpose(bkbf(b)[:, k8 * 128:(k8 + 1) * 128], src_ap[:, kc * 128:(kc + 1) * 128], ident[:]),
                          reads=[srcb, ident], writes=[banks[b]], inc=(k8 == 7))
                if half == 0:
                    fw.op("act", lambda e: e.copy(dstT[:, half * 8:(half + 1) * 8, :], bkbf(b).rearrange("p (k t) -> p k t", k=8)),
                          reads=[banks[b]], writes=[dstT])
                else:
                    fw.op("dve", lambda e: e.tensor_copy(dstT[:, half * 8:(half + 1) * 8, :], bkbf(b).rearrange("p (k t) -> p k t", k=8)),
                          reads=[banks[b]], writes=[dstT])
        b = nextbank()
        trip = [(x2hT, wrh), (x2hT, wrl), (x2lT, wrh)]
        for ti, (xt_, w_) in enumerate(trip):
            for kc in range(16):
                mm(bk(b)[:, 0:68], xt_[:, kc, :], w_[:, kc, :], ti == 0 and kc == 0, ti == 2 and kc == 15, [xt_, w_], [banks[b]])
        fw.op("dve", lambda e: e.tensor_tensor(logit[:, blk, :], bk(b)[:, 0:68], bias_b[:], ALU.add), reads=[banks[b], bias_b], writes=[logit])
        fw.op("act", lambda e: e.mul(acc[:, blk, :], acc[:, blk, :], ALPHA), reads=[acc], writes=[acc])
    if "R" in dbg:
        dbg_out["logit"] = fw.dram("dbg_logit", [128, NB, 68], F32, kind="ExternalOutput")
        fw.dma("sp", dbg_out["logit"].t.ap(), logit[:], reads=[logit], writes=[dbg_out["logit"]])
    dv = lambda fn, R, W: fw.op("dve", fn, reads=R, writes=W)
    L4 = logit[:, :, 0:4]
    LE = logit[:, :, 4:68].rearrange("p b (g x) -> p b g x", g=4)
    bc4 = lambda t: t[:].unsqueeze(2).to_broadcast([128, NB, 4])
    bc64 = lambda t: t[:].unsqueeze(2).to_broadcast([128, NB, 64])
    m4 = lambda t: t[:].rearrange("p b (g x) -> p b g x", g=4)
    dv(lambda e: e.reduce_max(sA_[:], L4, AX.X), [logit], [sA_])
    dv(lambda e: e.tensor_tensor(d4[:], L4, bc4(sA_), ALU.subtract), [logit, sA_], [d4])
    fw.op("act", lambda e: e.activation(e4[:], d4[:], AF.Exp), reads=[d4], writes=[e4])
    dv(lambda e: e.reduce_sum(sB_[:], e4[:], AX.X), [e4], [sB_])
    dv(lambda e: e.reciprocal(ggrp[:], sB_[:]), [sB_], [ggrp])
    dv(lambda e: e.tensor_scalar(gm[:], d4[:], 0.0, None, ALU.is_equal), [d4], [gm])
    dv(lambda e: e.tensor_scalar(pen[:], gm[:], 1e9, -1e9, ALU.mult, ALU.add), [gm], [pen])
    dv(lambda e: e.tensor_tensor(m4(msk), LE, gm[:].unsqueeze(3).to_broadcast([128, NB, 4, 16]), ALU.mult), [logit, gm], [msk])
    dv(lambda e: e.tensor_tensor(m4(msk), m4(msk), pen[:].unsqueeze(3).to_broadcast([128, NB, 4, 16]), ALU.add), [msk, pen], [msk])
    dv(lambda e: e.reduce_max(m1_[:], msk[:], AX.X), [msk], [m1_])
    dv(lambda e: e.tensor_tensor(oh1[:], msk[:], bc64(m1_), ALU.is_equal), [msk, m1_], [oh1])
    dv(lambda e: e.scalar_tensor_tensor(msk2[:], oh1[:], -1e9, msk[:], ALU.mult, ALU.add), [oh1, msk], [msk2])
    dv(lambda e: e.reduce_max(m2_[:], msk2[:], AX.X), [msk2], [m2_])
    dv(lambda e: e.tensor_tensor(oh2[:], msk2[:], bc64(m2_), ALU.is_equal), [msk2, m2_], [oh2])
    dv(lambda e: e.tensor_tensor(sA_[:], m2_[:], m1_[:], ALU.subtract), [m1_, m2_], [sA_])
    fw.op("act", lambda e: e.activation(e2_[:], sA_[:], AF.Exp), reads=[sA_], writes=[e2_])
    dv(lambda e: e.tensor_scalar(sB_[:], e2_[:], 1.0, None, ALU.add), [e2_], [sB_])
    dv(lambda e: e.reciprocal(sB_[:], sB_[:]), [sB_], [sB_])
    dv(lambda e: e.tensor_tensor(g1_[:], sB_[:], ggrp[:], ALU.mult), [sB_, ggrp], [g1_])
    dv(lambda e: e.tensor_tensor(g2_[:], g1_[:], e2_[:], ALU.mult), [g1_, e2_], [g2_])
    dv(lambda e: e.tensor_tensor(msk[:], oh1[:], bc64(g1_), ALU.mult), [oh1, g1_], [msk])
    dv(lambda e: e.tensor_tensor(msk2[:], oh2[:], bc64(g2_), ALU.mult), [oh2, g2_], [msk2])
    dv(lambda e: e.tensor_tensor(GWb[:], msk[:], msk2[:], ALU.add), [msk, msk2], [GWb])
    dv(lambda e: e.tensor_tensor(asg[:], oh1[:], oh2[:], ALU.add), [oh1, oh2], [asg])
    for blk in range(NB):
        b = nextbank()
        mm(bk(b)[:, 0:64], ustr[:], asg[:, blk, :], True, blk == 0, [ustr, asg], [banks[b]])
        for b2 in range(blk):
            mm(bk(b)[:, 0:64], ones_b[:], asg[:, b2, :], False, b2 == blk - 1, [ones_b, asg], [banks[b]])
        fw.op("dve", lambda e: e.scalar_tensor_tensor(posm_f[:, blk, :], bk(b)[:, 0:64], 1.0, asg[:, blk, :], ALU.add, ALU.mult),
              reads=[banks[b], asg], writes=[posm_f])
        fw.op("dve", lambda e: e.tensor_scalar(posm_f[:, blk, :], posm_f[:, blk, :], -1.0, 200.0, ALU.add, ALU.min), reads=[posm_f], writes=[posm_f])
        fw.op("dve", lambda e: e.tensor_copy(posb[:], posm_f[:, blk, :]), reads=[posm_f], writes=[posb])
        b3 = nextbank()
        fw.op("pe", lambda e: e.transpose(bkbf(b3)[0:64, 0:128], posb[:], ident[:]), reads=[posb, ident], writes=[banks[b3]])
        fw.op("act", lambda e: e.copy(posmT[:, blk * 128:(blk + 1) * 128], bkbf(b3)[0:64, 0:128]), reads=[banks[b3]], writes=[posmT])
    if "R" in dbg:
        dbg_out["posm"] = fw.dram("dbg_posm", [128, NB, 64], F32, kind="ExternalOutput")
        fw.dma("sp", dbg_out["posm"].t.ap(), posm_f[:], reads=[posm_f], writes=[dbg_out["posm"]])
        dbg_out["GW"] = fw.dram("dbg_GW", [128, NB, 64], BF16, kind="ExternalOutput")
        fw.dma("sp", dbg_out["GW"].t.ap(), GWb[:], reads=[GWb], writes=[dbg_out["GW"]])
        fw.finish(list(dbg_out.values()))
        return nc, fw
    fw.barrier()
    GE = 2
    Ygrp2 = [at(0, [64, GE, D], BF16, "Ygrp0"), at(8, [64, GE, D], BF16, "Ygrp1")]
    SelTg2 = [at(16, [64, GE, NB * 128], BF16, "SelTg0"), at(20, [64, GE, NB * 128], BF16, "SelTg1")]
    XgT = [at(24, [128, 16, CAP], BF16, "XgT0"), at(26, [128, 16, CAP], BF16, "XgT1")]
    Sel = [at(28, [128, NB, CAP], BF16, "Sel0"), at(29, [128, NB, CAP], BF16, "Sel1")]
    hb = at(30, [64, 512], BF16, "hb")
    hT = at(31, [128, 4, CAP], BF16, "hT")
    gslot = at(31.5, [64, 1], F32, "gslot")
    rowsel = at(31.75, [64, 64], BF16, "rowsel")
    sgt = at(200, [64, 512], F32, "sgt")
    xgtok = at(202, [64, D], BF16, "xgtok")
    wsl = [at(32 + 16 * i4, [128, 16, 512], BF16, f"wsl{i4}") for i4 in range(4)]

    def expert(ex, el, Ygrp, SelTg):
        wg_, wu_, wd_ = wsl[(3 * ex) % 4], wsl[(3 * ex + 1) % 4], wsl[(3 * ex + 2) % 4]
        fw.dma("pool", wg_[:], w_eg.t.ap()[ex].rearrange("(kc p) n -> p kc n", p=128), reads=[w_eg], writes=[wg_])
        fw.dma("pool", wu_[:], w_eu.t.ap()[ex].rearrange("(kc p) n -> p kc n", p=128), reads=[w_eu], writes=[wu_])
        wdv = wd_[:].rearrange("p a n -> p (a n)").rearrange("p (f n) -> p f n", f=4)
        fw.dma("pool", wdv, w_ed.t.ap()[ex].rearrange("(fc p) n -> p fc n", p=128), reads=[w_ed], writes=[wd_])
        sel = Sel[ex % 2]
        for blk in range(NB):
            fw.op("dve", lambda e: e.tensor_scalar(sel[:, blk, :], iota_c[:], posm_f[:, blk, ex:ex + 1], None, ALU.is_equal),
                  reads=[iota_c, posm_f], writes=[sel])
        xg = XgT[ex % 2]
        gb = [nextbank() for _ in range(4)]
        for blk in range(NB):
            for cg in range(4):
                mm(bk(gb[cg])[0:CAP, :], sel[:, blk, :], x2b[:, blk, cg * 512:(cg + 1) * 512], blk == 0, blk == NB - 1,
                   [sel, x2b], [banks[gb[cg]]])
        for cg in range(4):
            if cg % 2 == 0:
                fw.op("act", lambda e: e.copy(xgtok[:, cg * 512:(cg + 1) * 512], bk(gb[cg])[0:CAP, :]), reads=[banks[gb[cg]]], writes=[xgtok])
            else:
                fw.op("dve", lambda e: e.tensor_copy(xgtok[:, cg * 512:(cg + 1) * 512], bk(gb[cg])[0:CAP, :]), reads=[banks[gb[cg]]], writes=[xgtok])
        b = nextbank()
        for kc in range(16):
            fw.op("pe", lambda e: e.transpose(bkbf(b)[:, kc * CAP:(kc + 1) * CAP], xgtok[:, kc * 128:(kc + 1) * 128], ident[0:CAP, 0:CAP]),
                  reads=[xgtok, ident], writes=[banks[b]], inc=(kc == 15))
        fw.op("act", lambda e: e.copy(xg[:], bkbf(b).rearrange("p (k c) -> p k c", k=16)), reads=[banks[b]], writes=[xg])
        b = nextbank()
        for blk in range(NB):
            mm(bk(b)[0:CAP, 0:1], sel[:, blk, :], GWb[:, blk, ex:ex + 1], blk == 0, blk == NB - 1, [sel, GWb], [banks[b]])
        fw.op("dve", lambda e: e.tensor_copy(gslot[:], bk(b)[0:CAP, 0:1]), reads=[banks[b]], writes=[gslot])
        bg, bu = nextbank(), nextbank()
        for kc in range(16):
            mm(bk(bg)[0:CAP, :], xg[:, kc, :], wg_[:, kc, :], kc == 0, kc == 15, [xg, wg_], [banks[bg]])
        for kc in range(16):
            mm(bk(bu)[0:CAP, :], xg[:, kc, :], wu_[:, kc, :], kc == 0, kc == 15, [xg, wu_], [banks[bu]])
        fw.op("act", lambda e: e.activation(sgt[:], bk(bg)[0:CAP, :], AF.Silu), reads=[banks[bg]], writes=[sgt])
        fw.op("dve", lambda e: e.scalar_tensor_tensor(hb[:], bk(bu)[0:CAP, :], gslot[:, 0:1], sgt[:], ALU.mult, ALU.mult),
              reads=[banks[bu], gslot, sgt], writes=[hb])
        b = nextbank()
        for fc in range(4):
            fw.op("pe", lambda e: e.transpose(bkbf(b)[:, fc * CAP:(fc + 1) * CAP], hb[:, fc * 128:(fc + 1) * 128], ident[0:CAP, 0:CAP]),
                  reads=[hb, ident], writes=[banks[b]], inc=(fc == 3))
        fw.op("act", lambda e: e.copy(hT[:], bkbf(b)[:, 0:4 * CAP].rearrange("p (k c) -> p k c", k=4)), reads=[banks[b]], writes=[hT])
        for cg in range(4):
            b = nextbank()
            for fc in range(4):
                mm(bk(b)[0:CAP, :], hT[:, fc, :], wdv[:, fc, cg * 512:(cg + 1) * 512], fc == 0, fc == 3, [hT, wd_], [banks[b]])
            if cg % 2 == 0:
                fw.op("act", lambda e: e.copy(Ygrp[:, el, cg * 512:(cg + 1) * 512], bk(b)[0:CAP, :]), reads=[banks[b]], writes=[Ygrp])
            else:
                fw.op("dve", lambda e: e.tensor_copy(Ygrp[:, el, cg * 512:(cg + 1) * 512], bk(b)[0:CAP, :]), reads=[banks[b]], writes=[Ygrp])
        fw.op("dve", lambda e: e.tensor_scalar(rowsel[:], iota_pb[:], float(ex), None, ALU.is_equal), reads=[iota_pb], writes=[rowsel])
        for hf in range(2):
            b = nextbank()
            mm(bk(b)[0:CAP, :], rowsel[:], posmT[:, hf * 512:(hf + 1) * 512], True, True, [rowsel, posmT], [banks[b]])
            fw.op("dve", lambda e: e.tensor_scalar(SelTg[:, el, hf * 512:(hf + 1) * 512], bk(b)[0:CAP, :], iota_p1[:, 0:1], None, ALU.is_equal),
                  reads=[banks[b], iota_p1], writes=[SelTg])

    def combine(Ygrp, SelTg, blks):
        for blk in blks:
            bs4 = [nextbank() for _ in range(4)]
            for el in range(GE):
                for cg in range(4):
                    mm(bk(bs4[cg]), SelTg[:, el, blk * 128:(blk + 1) * 128], Ygrp[:, el, cg * 512:(cg + 1) * 512], el == 0, el == GE - 1,
                       [SelTg, Ygrp], [banks[bs4[cg]]])
            for cg in range(4):
                sl = acc[:, blk, cg * 512:(cg + 1) * 512]
                fw.op("dve", lambda e: e.tensor_tensor(sl, sl, bk(bs4[cg]), ALU.add), reads=[acc, banks[bs4[cg]]], writes=[acc])

    prevg = None
    for g2 in range(64 // GE):
        Yg, STg = Ygrp2[g2 % 2], SelTg2[g2 % 2]
        expert(GE * g2, 0, Yg, STg)
        if prevg is not None:
            combine(prevg[0], prevg[1], range(0, 4))
        expert(GE * g2 + 1, 1, Yg, STg)
        if prevg is not None:
            combine(prevg[0], prevg[1], range(4, 8))
        prevg = (Yg, STg)
    combine(prevg[0], prevg[1], range(0, 8))
    fw.barrier()
    layer_norm(acc, ln3_g, ln3_b, None, out_dram=out_d)
    fw.finish([out_d])
    return nc, fw


def _prep_inputs(inputs, core):
    b, j = core // 4, core % 4
    x = np.asarray(inputs["x"])
    w_in = np.asarray(inputs["w_in"])[0]
    m = {}
    m["x"] = np.ascontiguousarray(x[b])
    own = np.concatenate([np.arange(128 * (4 * i + j), 128 * (4 * i + j) + 128) for i in range(NB)])
    m["x_own"] = np.ascontiguousarray(x[b][own])
    m["w_in"] = w_in

    def swap_halves(w):
        sh = w.shape
        return np.ascontiguousarray(w.reshape(sh[0], -1, 2, 64)[:, :, ::-1, :].reshape(sh))

    m["wq_rot"] = swap_halves(w_in[:, OFF_Q:OFF_Q + 2048])
    kcols = np.concatenate([w_in[:, OFF_KV + 512:OFF_KV + 768], w_in[:, OFF_KV + 1024:OFF_KV + 1280]], axis=1)
    m["wk_rot"] = swap_halves(kcols)
    m["pe_kT"] = np.ascontiguousarray(np.asarray(inputs["cmp_pe_k"])[0].T)
    m["pe_vT"] = np.ascontiguousarray(np.asarray(inputs["cmp_pe_v"])[0].T)
    m["cmp_w1_k"] = np.asarray(inputs["cmp_w1_k"])[0]
    m["cmp_w1_v"] = np.asarray(inputs["cmp_w1_v"])[0]
    m["cmp_w2_k"] = np.asarray(inputs["cmp_w2_k"])[0]
    m["cmp_w2_k_rot"] = swap_halves(np.asarray(inputs["cmp_w2_k"])[0])
    m["cmp_w2_v"] = np.asarray(inputs["cmp_w2_v"])[0]
    g = lambda k: np.asarray(inputs[k])[0]
    m["sgu_wsT"] = np.ascontiguousarray(g("sgu_w_s").transpose(0, 2, 1))
    m["sgu_g_fm"] = np.ascontiguousarray(g("sgu_ln_g").reshape(16, 128).T)
    m["sgu_b_fm"] = np.ascontiguousarray(g("sgu_ln_b").reshape(16, 128).T)
    m["sgu_bs"] = np.ascontiguousarray(g("sgu_b_s").reshape(1, 1024))
    m["w_branch_a"] = g("w_branch_a")
    m["w_branch_b"] = g("w_branch_b")
    m["w_o"] = g("w_o")
    m["ln1_g"] = g("ln1_g").reshape(1, D)
    m["ln1_b"] = g("ln1_b").reshape(1, D)
    m["mem"] = np.ascontiguousarray(np.asarray(inputs["mem"])[b])
    for k in ("w_xq", "w_xk", "w_xv", "w_xo", "w_exp_gate", "w_exp_up", "w_exp_down"):
        m[k] = g(k)
    for k in ("ln2_g", "ln2_b", "ln3_g", "ln3_b"):
        m[k] = g(k).reshape(1, D)
    m["w_router"] = np.ascontiguousarray(np.concatenate([g("w_router_grp"), g("w_router_exp")], axis=1))
    m["b_router"] = np.concatenate([g("b_router_grp"), g("b_router_exp")]).reshape(1, 68)
    for k, v in _consts(j).items():
        m["c_" + k] = v
    return m


def kernel(**inputs):
    nc, fw = build()
    in_maps = [_prep_inputs(inputs, c) for c in range(8)]
    res = run_bass_kernel_spmd(nc, in_maps, core_ids=list(range(8)))
    out = np.zeros((2, S, D), np.float32)
    for c in range(8):
        b, j = c // 4, c % 4
        o = res.results[c]["out"]
        for i in range(NB):
            blk = 4 * i + j
            out[b, blk * 128:(blk + 1) * 128] = o[i * 128:(i + 1) * 128]
    return out
```

```python
import numpy as np
import concourse.bass as bass
import concourse.mybir as mybir
from concourse.bass_utils import run_bass_kernel_spmd

F32 = mybir.dt.float32
BF16 = mybir.dt.bfloat16
I32 = mybir.dt.int32
AF = mybir.ActivationFunctionType
ALU = mybir.AluOpType
AX = mybir.AxisListType

class Buf:
    __slots__ = ("t", "w", "r", "name")

    def __init__(self, t, name=""):
        self.t = t
        self.w = None
        self.r = {}
        self.name = name

    def __getitem__(self, k):
        return self.t[k]


class _Eng:
    def __init__(self, name, eng, sem):
        self.name = name
        self.eng = eng
        self.sem = sem
        self.tick = 0
        self.seen = {}
        self.pool = []
        self.pool_i = 0


class FW:
    def __init__(self, nc, n_dma_sems=6):
        self.nc = nc
        self.E = {}
        for name, eng in (("pe", nc.tensor), ("act", nc.scalar), ("dve", nc.vector),
                          ("pool", nc.gpsimd), ("sp", nc.sync)):
            e = _Eng(name, eng, nc.alloc_semaphore("s_" + name))
            self.E[name] = e
        for q in ("sp", "pool", "act"):
            e = self.E[q]
            for i in range(n_dma_sems):
                e.pool.append([nc.alloc_semaphore(f"d_{q}{i}"), 0])
        self.nsb = 0
        self.n_inst = 0

    def sb(self, shape, dtype, name=None, side=None):
        self.nsb += 1
        name = (name or "sb") + f"_{self.nsb}"
        return Buf(self.nc.alloc_sbuf_tensor(name, list(shape), dtype, side=side), name)

    def ps(self, shape, dtype=F32, name=None):
        self.nsb += 1
        name = name or f"ps{self.nsb}"
        return Buf(self.nc.alloc_psum_tensor(name, list(shape), dtype), name)

    def dram(self, name, shape, dtype, kind="Internal"):
        return Buf(self.nc.dram_tensor(name, list(shape), dtype, kind=kind), name)

    def _deps(self, reads, writes):
        evs = []
        for b in reads:
            if b.w is not None:
                evs.append(b.w)
        for b in writes:
            if b.w is not None:
                evs.append(b.w)
            evs.extend(b.r.values())
        return evs

    def _wait(self, e, evs):
        need = {}
        for (sem, val) in evs:
            k = id(sem)
            if e.seen.get(k, 0) >= val:
                continue
            if k not in need or need[k][1] < val:
                need[k] = (sem, val)
        for k, (sem, val) in need.items():
            if e.name == "pe" and sem is e.sem:
                continue
            e.eng.wait_ge(sem, val)
            e.seen[k] = val

    def _record(self, ev, reads, writes):
        k = id(ev[0])
        for b in reads:
            b.r[k] = ev
        for b in writes:
            b.w = ev
            b.r = {}

    def op(self, ename, fn, reads=(), writes=(), inc=True):
        e = self.E[ename]
        self._wait(e, self._deps(reads, writes))
        ins = fn(e.eng)
        self.n_inst += 1
        if inc:
            e.tick += 1
            ins.then_inc(e.sem, 1)
            self._record((e.sem, e.tick), reads, writes)
        else:
            self._record((e.sem, e.tick + 1), reads, writes)

    def barrier(self):
        evs = []
        for en in self.E.values():
            for s in en.pool:
                if s[1] > 0:
                    evs.append((s[0], s[1]))
            if en.tick > 0:
                evs.append((en.sem, en.tick))
        for en in self.E.values():
            for (sem, val) in evs:
                if en.seen.get(id(sem), 0) < val:
                    en.eng.wait_ge(sem, val)
                    en.seen[id(sem)] = val

    def dma(self, q, out_ap, in_ap, reads=(), writes=(), **kw):
        e = self.E[q]
        slot = e.pool[e.pool_i % len(e.pool)]
        e.pool_i += 1
        evs = self._deps(reads, writes)
        if slot[1] > 0:
            evs.append((slot[0], slot[1]))
        self._wait(e, evs)
        slot[1] += 16
        e.eng.dma_start(out=out_ap, in_=in_ap, **kw).then_inc(slot[0], 16)
        self.n_inst += 1
        self._record((slot[0], slot[1]), reads, writes)

    def finish(self, bufs):
        e = self.E["sp"]
        evs = []
        for b in bufs:
            if b.w is not None:
                evs.append(b.w)
        for en in self.E.values():
            for s in en.pool:
                if s[1] > 0:
                    evs.append((s[0], s[1]))
            if en.tick > 0 and en is not e:
                evs.append((en.sem, en.tick))
        self._wait(e, evs)


S = 4096
D = 2048
NB = 8
OFF_Q, OFF_KV, OFF_G, OFF_U, OFF_V, OFF_GA, OFF_GB = 0, 2048, 3584, 3632, 5680, 7728, 9776
ALPHA = 2.0 ** 0.25
EPS = 1e-5
CAP = 64


def _consts(j):
    c = {}
    c["ident"] = np.eye(128, dtype=np.float32)
    c["perm"] = np.roll(np.eye(128, dtype=np.float32), 64, axis=0)
    inv = (10000.0 ** (-np.arange(0, 128, 2, dtype=np.float32) / 128)).astype(np.float32)
    inv2 = np.concatenate([inv, inv])
    sgn = np.concatenate([-np.ones(64, np.float32), np.ones(64, np.float32)])

    def tab(pos, scale=1.0):
        ang = pos.astype(np.float32)[None, :] * inv2[:, None]
        return ((np.cos(ang) * scale).astype(np.float32),
                (np.sin(ang) * sgn[:, None] * scale).astype(np.float32))

    c["cosK"], c["sinK"] = tab(np.arange(S))
    own_pos = np.concatenate([128 * (4 * i + j) + np.arange(128) for i in range(NB)])
    c["cosQ"], c["sinQ"] = tab(own_pos, 128.0 ** -0.5)
    c["cosC"], c["sinC"] = tab(16 * np.arange(256) + 31)
    p = np.arange(128)[:, None]
    tl = np.arange(128)[None, :]
    caus = (p <= tl).astype(np.float32)
    dm = np.zeros((128, 4, 128), np.float32)
    for r in range(4):
        dm[:, r, :] = 1.0 if r < j else (caus if r == j else 0.0)
    c["dmask"] = dm
    wm = np.zeros((128, 8, 128), np.float32)
    for r in range(8):
        rel = r - 4 - j
        if rel == 0:
            wm[:, r, :] = caus
        elif rel in (-1, -2, -3):
            wm[:, r, :] = 1.0
        elif rel == -4:
            wm[:, r, :] = 1.0 - caus
    c["wmask"] = wm
    cm = np.zeros((128, NB, 2, 128), np.float32)
    sA = np.zeros((128, NB, 64), np.float32)
    sB = np.zeros((128, NB, 64), np.float32)
    jj = np.arange(64)[None, :]
    for i in range(NB):
        cb = 4 * i + j
        t = 128 * cb + np.arange(128)
        for ch in range(2):
            n = ch * 128 + np.arange(128)
            cm[:, i, ch, :] = ((16 * n[:, None] + 31 <= t[None, :]) & (n[:, None] < 255)).astype(np.float32)
        cur = (t // 64)[:, None]
        vis = jj <= cur
        f0 = jj == 0
        f1 = jj == cur
        f2 = jj == cur - 1
        forced = f0 | f1 | f2
        sA[:, i, :] = (vis & ~forced).astype(np.float32)
        b = np.where(vis, 0.0, -1e9)
        b = np.where(f2, 1e9, b)
        b = np.where(f1, 2e9, b)
        b = np.where(f0, 3e9, b)
        sB[:, i, :] = b
    c["cmask"], c["selA"], c["selB"] = cm, sA, sB
    n = np.arange(256)[:, None]
    ov = np.clip(np.minimum(16 * n + 32, 64 * jj + 64) - np.maximum(16 * n, 64 * jj), 0, None) / 32.0
    ov[255] = 0.0
    c["selmap"] = np.ascontiguousarray(ov.reshape(2, 128, 64).transpose(1, 0, 2)).astype(np.float32)
    ex = np.zeros((64, 32, 128), np.float32)
    for kt in range(32):
        ex[2 * kt, kt, :64] = 1.0
        ex[2 * kt + 1, kt, 64:] = 1.0
    c["exall"] = ex
    c["tri"] = caus
    c["ustrict"] = (p < tl).astype(np.float32)
    c["iota_c"] = np.tile(np.arange(64, dtype=np.float32)[None, :], (128, 1))
    c["iota_pb"] = np.tile(np.arange(64, dtype=np.float32)[:, None], (1, 64))
    return c


CONST_SHAPES = {k: v.shape for k, v in _consts(0).items()}


def build(dbg=()):
    nc = bass.Bass("TRN2", target_bir_lowering=False)
    fw = FW(nc)
    dbg_out = {}

    def din(name, shape):
        return fw.dram(name, shape, F32, kind="ExternalInput")

    x_d = din("x", [S, D])
    xo_d = din("x_own", [NB * 128, D])
    w_in = din("w_in", [D, 11824])
    wq_rot = din("wq_rot", [D, 2048])
    wk_rot = din("wk_rot", [D, 512])
    pe_kT = din("pe_kT", [128, 32])
    pe_vT = din("pe_vT", [128, 32])
    w1k_d = din("cmp_w1_k", [4096, 256])
    w1v_d = din("cmp_w1_v", [4096, 256])
    w2k_d = din("cmp_w2_k", [256, 128])
    w2kr_d = din("cmp_w2_k_rot", [256, 128])
    w2v_d = din("cmp_w2_v", [256, 128])
    sgu_wsT = din("sgu_wsT", [8, 128, 128])
    sgu_g_fm = din("sgu_g_fm", [128, 16])
    sgu_b_fm = din("sgu_b_fm", [128, 16])
    sgu_bs = din("sgu_bs", [1, 1024])
    w_br_a = din("w_branch_a", [D, D])
    w_br_b = din("w_branch_b", [D, D])
    w_o_d = din("w_o", [D, D])
    ln1_g = din("ln1_g", [1, D])
    ln1_b = din("ln1_b", [1, D])
    mem_d = din("mem", [256, D])
    w_xq = din("w_xq", [D, 512])
    w_xk = din("w_xk", [D, 512])
    w_xv = din("w_xv", [D, 512])
    w_xo = din("w_xo", [512, D])
    ln2_g = din("ln2_g", [1, D])
    ln2_b = din("ln2_b", [1, D])
    w_rt = din("w_router", [D, 68])
    b_rt = din("b_router", [1, 68])
    w_eg = din("w_exp_gate", [64, D, 512])
    w_eu = din("w_exp_up", [64, D, 512])
    w_ed = din("w_exp_down", [64, 512, D])
    ln3_g = din("ln3_g", [1, D])
    ln3_b = din("ln3_b", [1, D])
    C = {k: din("c_" + k, list(s)) for k, s in CONST_SHAPES.items()}
    out_d = fw.dram("out", [NB * 128, D], F32, kind="ExternalOutput")

    kT_d = fw.dram("kT_scr", [4, 128, S], BF16)
    v_d = fw.dram("v_scr", [S, 512], BF16)

    def r3(buf):
        return buf.t.ap().rearrange("(kc p) n -> p kc n", p=128)

    PS = fw.nc.alloc_psum_tensor("psum", [128, 8, 512], F32)
    banks = [Buf(PS, f"bank{i}") for i in range(8)]

    def bk(i):
        return PS[:, i, :]

    def bkbf(i):
        return PS[:, i, :].bitcast(BF16)

    rr = [0]

    def nextbank():
        i = rr[0] % 8
        rr[0] += 1
        return i

    epsc = fw.sb([128, 1], F32, "epsc")
    fw.op("dve", lambda e: e.memset(epsc[:], EPS), writes=[epsc])
    ident = fw.sb([128, 128], BF16, "ident")
    fw.dma("pool", ident[:], C["ident"][:], reads=[C["ident"]], writes=[ident])

    def mm(ps_ap, lhsT, rhs, start, stop, R, W, inc=None, sgc=False):
        if inc is None:
            inc = stop
        fw.op("pe", lambda e: e.matmul(ps_ap, lhsT, rhs, start=start, stop=stop, skip_group_check=sgc),
              reads=R, writes=W, inc=inc)

    def load_xT(src_d, row0, xb, dstT, col0, evac):
        fw.dma("pool", xb[:], src_d[row0:row0 + 128, :], reads=[src_d], writes=[xb])
        for half in range(2):
            b = nextbank()
            for k8 in range(8):
                kc = half * 8 + k8
                fw.op("pe", lambda e: e.transpose(bkbf(b)[:, k8 * 128:(k8 + 1) * 128],
                                                  xb[:, kc * 128:(kc + 1) * 128], ident[:]),
                      reads=[xb, ident], writes=[banks[b]], inc=(k8 == 7))
            src = bkbf(b).rearrange("p (k t) -> p k t", k=8)
            dst = dstT[:, half * 8:(half + 1) * 8, col0:col0 + 128]
            if evac == "act":
                fw.op("act", lambda e: e.copy(dst, src), reads=[banks[b]], writes=[dstT])
            else:
                fw.op("dve", lambda e: e.tensor_copy(dst, src), reads=[banks[b]], writes=[dstT])

    mark0 = nc.sbuf_base
    cmpraw = fw.sb([128, 4, S], BF16, "cmpraw")
    mark_dbg = nc.sbuf_base
    wkv = fw.sb([128, 16, 1536], BF16, "wkv")
    wkr = fw.sb([128, 16, 512], BF16, "wkr")
    w_in_r = r3(w_in)
    for c3 in range(3):
        fw.dma("pool", wkv[:, :, c3 * 512:(c3 + 1) * 512], w_in_r[:, :, OFF_KV + c3 * 512:OFF_KV + (c3 + 1) * 512],
               reads=[w_in], writes=[wkv])
    fw.dma("pool", wkr[:], r3(wk_rot), reads=[wk_rot], writes=[wkr])
    xbs = [fw.sb([128, D], BF16, f"xb{i}") for i in range(3)]
    xTs = [fw.sb([128, 16, 512], BF16, f"xT{i}") for i in range(2)]
    cos_t = [fw.sb([128, 512], F32, f"cos{i}") for i in range(2)]
    sin_t = [fw.sb([128, 512], F32, f"sin{i}") for i in range(2)]
    kst = [fw.sb([128, 4, 512], BF16, f"kst{i}") for i in range(2)]
    vst = [fw.sb([128, 4, 512], BF16, f"vst{i}") for i in range(2)]
    ropa = [fw.sb([128, 512], F32, f"ropa{i}") for i in range(2)]
    ropb = [fw.sb([128, 512], F32, f"ropb{i}") for i in range(2)]
    kT_r = kT_d.t.ap().rearrange("k d t -> d k t")

    def rope(ps_t, ps_r, cosb, sinb, cos_ap, sin_ap, out_ap, outbuf, n, idx):
        a, b2 = ropa[idx % 2], ropb[idx % 2]
        fw.op("dve", lambda e: e.tensor_tensor(a[:, 0:n], bk(ps_t)[:, 0:n], cos_ap, ALU.mult),
              reads=[banks[ps_t], cosb], writes=[a])
        fw.op("dve", lambda e: e.tensor_tensor(b2[:, 0:n], bk(ps_r)[:, 0:n], sin_ap, ALU.mult),
              reads=[banks[ps_r], sinb], writes=[b2])
        fw.op("pool", lambda e: e.tensor_tensor(out_ap, a[:, 0:n], b2[:, 0:n], ALU.add),
              reads=[a, b2], writes=[outbuf])

    ridx = [0]
    for tile in range(8):
        xT = xTs[tile % 2]
        ct, st = cos_t[tile % 2], sin_t[tile % 2]
        fw.dma("sp", ct[:], C["cosK"][:, tile * 512:(tile + 1) * 512], reads=[C["cosK"]], writes=[ct])
        fw.dma("sp", st[:], C["sinK"][:, tile * 512:(tile + 1) * 512], reads=[C["sinK"]], writes=[st])
        for blk in range(4):
            g = tile * 4 + blk
            load_xT(x_d, g * 128, xbs[g % 3], xT, blk * 128, "act" if blk % 2 == 0 else "dve")
        for f in range(4):
            b = nextbank()
            for kc in range(16):
                mm(bk(b), wkv[:, kc, f * 128:(f + 1) * 128], xT[:, kc, :], kc == 0, kc == 15, [wkv, xT], [banks[b]])
            fw.op("act", lambda e: e.copy(cmpraw[:, f, tile * 512:(tile + 1) * 512], bk(b)),
                  reads=[banks[b]], writes=[cmpraw])
        ks = kst[tile % 2]
        for kk in range(4):
            col = (512 if kk < 2 else 1024) + (kk % 2) * 128
            b1 = nextbank()
            for kc in range(16):
                mm(bk(b1), wkv[:, kc, col:col + 128], xT[:, kc, :], kc == 0, kc == 15, [wkv, xT], [banks[b1]])
            b2 = nextbank()
            for kc in range(16):
                mm(bk(b2), wkr[:, kc, kk * 128:(kk + 1) * 128], xT[:, kc, :], kc == 0, kc == 15, [wkr, xT], [banks[b2]])
            rope(b1, b2, ct, st, ct[:], st[:], ks[:, kk, :], ks, 512, ridx[0])
            ridx[0] += 1
        fw.dma("sp", kT_r[:, :, tile * 512:(tile + 1) * 512], ks[:], reads=[ks], writes=[kT_d])
        vs = vst[tile % 2]
        for blk in range(4):
            b = nextbank()
            for half, col in enumerate((768, 1280)):
                for kc in range(16):
                    mm(bk(b)[:, half * 256:(half + 1) * 256], xT[:, kc, blk * 128:(blk + 1) * 128],
                       wkv[:, kc, col:col + 256], kc == 0, kc == 15, [wkv, xT], [banks[b]],
                       inc=(kc == 15 and half == 1))
            fw.op("dve", lambda e: e.tensor_copy(vs[:, blk, :], bk(b)), reads=[banks[b]], writes=[vs])
        fw.dma("sp", v_d.t.ap()[tile * 512:(tile + 1) * 512, :].rearrange("(b p) f -> p b f", p=128), vs[:],
               reads=[vs], writes=[v_d])

    if "A" in dbg:
        dbg_out["cmpraw"] = fw.dram("dbg_cmpraw", [128, 4, S], BF16, kind="ExternalOutput")
        fw.dma("sp", dbg_out["cmpraw"].t.ap(), cmpraw[:], reads=[cmpraw], writes=[dbg_out["cmpraw"]])
        dbg_out["kT"] = fw.dram("dbg_kT", [4, 128, S], BF16, kind="ExternalOutput")
        dbg_out["v"] = fw.dram("dbg_v", [S, 512], BF16, kind="ExternalOutput")
        fw.barrier()
        nc.sbuf_base = mark_dbg
        tmpk = fw.sb([128, 4, S], BF16, "tmpk")
        fw.dma("sp", tmpk[:], kT_r, reads=[kT_d], writes=[tmpk])
        fw.dma("sp", dbg_out["kT"].t.ap().rearrange("k d t -> d k t"), tmpk[:], reads=[tmpk], writes=[dbg_out["kT"]])
        tmpv = fw.sb([128, 32, 512], BF16, "tmpv")
        fw.dma("sp", tmpv[:], v_d.t.ap().rearrange("(b p) f -> p b f", p=128), reads=[v_d], writes=[tmpv])
        fw.dma("sp", dbg_out["v"].t.ap().rearrange("(b p) f -> p b f", p=128), tmpv[:], reads=[tmpv], writes=[dbg_out["v"]])
        fw.finish(list(dbg_out.values()))
        return nc, fw


    fw.barrier()
    nc.sbuf_base = mark_dbg
    qT = fw.sb([128, NB, 16, 128], BF16, "qT")
    gates = fw.sb([128, NB, 48], F32, "gates")
    kcmpT = fw.sb([128, 2, 256], BF16, "kcmpT")
    vcmp = fw.sb([128, 2, 2, 129], BF16, "vcmp")
    mark2 = nc.sbuf_base
    xTo = fw.sb([128, 16, NB * 128], BF16, "xTo")
    xbo = [fw.sb([128, D], BF16, f"xbo{i}") for i in range(2)]
    for blk in range(NB):
        load_xT(xo_d, blk * 128, xbo[blk % 2], xTo, blk * 128, "act" if blk % 2 == 0 else "dve")
    wg = fw.sb([128, 16, 48], BF16, "wg")
    fw.dma("pool", wg[:], w_in_r[:, :, OFF_G:OFF_G + 48], reads=[w_in], writes=[wg])
    for blk in range(NB):
        b = nextbank()
        for kc in range(16):
            mm(bk(b)[:, 0:48], xTo[:, kc, blk * 128:(blk + 1) * 128], wg[:, kc, :], kc == 0, kc == 15, [xTo, wg], [banks[b]])
        fw.op("act", lambda e: e.activation(gates[:, blk, :], bk(b)[:, 0:48], AF.Sigmoid), reads=[banks[b]], writes=[gates])
    cosq = fw.sb([128, NB * 128], F32, "cosq")
    sinq = fw.sb([128, NB * 128], F32, "sinq")
    fw.dma("sp", cosq[:], C["cosQ"][:], reads=[C["cosQ"]], writes=[cosq])
    fw.dma("sp", sinq[:], C["sinQ"][:], reads=[C["sinQ"]], writes=[sinq])
    wqb = [fw.sb([128, 16, 512], BF16, f"wq{i}") for i in range(2)]
    wqrb = [fw.sb([128, 16, 512], BF16, f"wqr{i}") for i in range(2)]
    ropa2 = [fw.sb([128, 512], F32, f"ropa2{i}") for i in range(2)]
    ropb2 = [fw.sb([128, 512], F32, f"ropb2{i}") for i in range(2)]
    wqr_r = r3(wq_rot)
    ri = 0
    for hg in range(4):
        wq, wqr = wqb[hg % 2], wqrb[hg % 2]
        fw.dma("pool", wq[:], w_in_r[:, :, OFF_Q + hg * 512:OFF_Q + (hg + 1) * 512], reads=[w_in], writes=[wq])
        fw.dma("pool", wqr[:], wqr_r[:, :, hg * 512:(hg + 1) * 512], reads=[wq_rot], writes=[wqr])
        for half in range(2):
            for hh in range(4):
                b1 = nextbank()
                for kc in range(16):
                    mm(bk(b1), wq[:, kc, hh * 128:(hh + 1) * 128], xTo[:, kc, half * 512:(half + 1) * 512], kc == 0, kc == 15, [wq, xTo], [banks[b1]])
                b2 = nextbank()
                for kc in range(16):
                    mm(bk(b2), wqr[:, kc, hh * 128:(hh + 1) * 128], xTo[:, kc, half * 512:(half + 1) * 512], kc == 0, kc == 15, [wqr, xTo], [banks[b2]])
                a, bb = ropa2[ri % 2], ropb2[ri % 2]
                ri += 1
                fw.op("dve", lambda e: e.tensor_tensor(a[:], bk(b1), cosq[:, half * 512:(half + 1) * 512], ALU.mult), reads=[banks[b1], cosq], writes=[a])
                fw.op("dve", lambda e: e.tensor_tensor(bb[:], bk(b2), sinq[:, half * 512:(half + 1) * 512], ALU.mult), reads=[banks[b2], sinq], writes=[bb])
                fw.op("pool", lambda e: e.tensor_tensor(qT[:, half * 4:(half + 1) * 4, hg * 4 + hh, :],
                                                        a[:].rearrange("p (b t) -> p b t", b=4),
                                                        bb[:].rearrange("p (b t) -> p b t", b=4), ALU.add),
                      reads=[a, bb], writes=[qT])

    fw.barrier()
    nc.sbuf_base = mark2
    w1 = [fw.sb([128, 32, 256], BF16, f"w1{i}") for i in range(2)]
    peT = [fw.sb([128, 32], BF16, f"peT{i}") for i in range(2)]
    for kv, (wd, pd) in enumerate(((w1k_d, pe_kT), (w1v_d, pe_vT))):
        fw.dma("pool", w1[kv][:], wd.t.ap().rearrange("(j d) h -> d j h", d=128), reads=[wd], writes=[w1[kv]])
        fw.dma("pool", peT[kv][:], pd[:], reads=[pd], writes=[peT[kv]])
    w2s = []
    for wd in (w2k_d, w2kr_d, w2v_d):
        t = fw.sb([128, 2, 128], BF16, "w2")
        fw.dma("pool", t[:], wd.t.ap().rearrange("(hc h) d -> h hc d", h=128), reads=[wd], writes=[t])
        w2s.append(t)
    w2k, w2kr, w2v = w2s
    cosc = fw.sb([128, 256], F32, "cosc")
    sinc = fw.sb([128, 256], F32, "sinc")
    fw.dma("sp", cosc[:], C["cosC"][:], reads=[C["cosC"]], writes=[cosc])
    fw.dma("sp", sinc[:], C["sinC"][:], reads=[C["sinC"]], writes=[sinc])
    biasS = fw.sb([128, 4], F32, "biasS")
    for kv in range(2):
        for hc in range(2):
            b = nextbank()
            for jx in range(32):
                mm(bk(b)[:, 0:1], w1[kv][:, jx, hc * 128:(hc + 1) * 128], peT[kv][:, jx:jx + 1], jx == 0, jx == 31, [w1[kv], peT[kv]], [banks[b]])
            fw.op("act", lambda e: e.copy(biasS[:, kv * 2 + hc:kv * 2 + hc + 1], bk(b)[:, 0:1]), reads=[banks[b]], writes=[biasS])
    fw.op("dve", lambda e: e.memset(kcmpT[:], 0.0), writes=[kcmpT])
    fw.op("dve", lambda e: e.memset(vcmp[:], 0.0), writes=[vcmp])
    fw.op("dve", lambda e: e.memset(vcmp[:, :, :, 128:129], 1.0), writes=[vcmp])
    hidTs = [fw.sb([128, 2, 256], BF16, f"hidT{i}") for i in range(2)]
    ropc = [fw.sb([128, 256], F32, f"ropc{i}") for i in range(2)]
    for kv in range(2):
        for g in range(2):
            hidT = hidTs[(kv * 2 + g) % 2]
            for hc in range(2):
                b = nextbank()
                for jx in range(32):
                    mm(bk(b)[:, 0:255], w1[kv][:, jx, hc * 128:(hc + 1) * 128], cmpraw[:, kv * 2 + g, jx:jx + 16 * 254 + 1:16],
                       jx == 0, jx == 31, [w1[kv], cmpraw], [banks[b]])
                fw.op("act", lambda e: e.activation(hidT[:, hc, 0:255], bk(b)[:, 0:255], AF.Gelu_apprx_tanh,
                                                    bias=biasS[:, kv * 2 + hc:kv * 2 + hc + 1]),
                      reads=[banks[b], biasS], writes=[hidT])
            if kv == 0:
                b1 = nextbank()
                for hc in range(2):
                    mm(bk(b1)[:, 0:255], w2k[:, hc, :], hidT[:, hc, 0:255], hc == 0, hc == 1, [w2k, hidT], [banks[b1]])
                b2 = nextbank()
                for hc in range(2):
                    mm(bk(b2)[:, 0:255], w2kr[:, hc, :], hidT[:, hc, 0:255], hc == 0, hc == 1, [w2kr, hidT], [banks[b2]])
                a, bb = ropc[0], ropc[1]
                fw.op("dve", lambda e: e.tensor_tensor(a[:, 0:255], bk(b1)[:, 0:255], cosc[:, 0:255], ALU.mult), reads=[banks[b1], cosc], writes=[a])
                fw.op("dve", lambda e: e.tensor_tensor(bb[:, 0:255], bk(b2)[:, 0:255], sinc[:, 0:255], ALU.mult), reads=[banks[b2], sinc], writes=[bb])
                fw.op("dve", lambda e: e.tensor_tensor(kcmpT[:, g, 0:255], a[:, 0:255], bb[:, 0:255], ALU.add), reads=[a, bb], writes=[kcmpT])
            else:
                for ch in range(2):
                    nn = 128 if ch == 0 else 127
                    b = nextbank()
                    for hc in range(2):
                        mm(bk(b)[0:nn, 0:128], hidT[:, hc, ch * 128:ch * 128 + nn], w2v[:, hc, :], hc == 0, hc == 1, [hidT, w2v], [banks[b]])
                    fw.op("act", lambda e: e.copy(vcmp[0:nn, ch, g, 0:128], bk(b)[0:nn, 0:128]), reads=[banks[b]], writes=[vcmp])

    if "B" in dbg:
        for nm, buf, shp, dt_ in (("qT", qT, [128, NB, 16, 128], BF16), ("gates", gates, [128, NB, 48], F32),
                                  ("kcmpT", kcmpT, [128, 2, 256], BF16), ("vcmp", vcmp, [128, 2, 2, 129], BF16)):
            dbg_out[nm] = fw.dram("dbg_" + nm, shp, dt_, kind="ExternalOutput")
            fw.dma("sp", dbg_out[nm].t.ap(), buf[:], reads=[buf], writes=[dbg_out[nm]])
        fw.finish(list(dbg_out.values()))
        return nc, fw


    fw.barrier()
    nc.sbuf_base = mark2
    o_nsaT = fw.sb([128, 16, NB * 128], BF16, "o_nsaT", side="right")

    def cload(name, shape, dt_, q="pool"):
        t = fw.sb(shape, dt_, name)
        fw.dma(q, t[:], C[name].t.ap(), reads=[C[name]], writes=[t])
        return t

    exall = cload("exall", [64, 32, 128], BF16)
    dmask = cload("dmask", [128, 4, 128], BF16)
    wmask = cload("wmask", [128, 8, 128], BF16)
    cmask = cload("cmask", [128, NB, 2, 128], BF16)
    selmap = cload("selmap", [128, 2, 64], BF16)
    selA = cload("selA", [128, NB, 64], F32, "sp")
    selB = cload("selB", [128, NB, 64], F32, "sp")
    ksl = [fw.sb([128, S], BF16, f"ksl{i}") for i in range(2)]
    vsl = [fw.sb([128, 32, 129], BF16, f"vsl{i}") for i in range(2)]
    kwn = [fw.sb([128, 1024], BF16, f"kwn{i}") for i in range(2)]
    vwn = [fw.sb([128, 8, 129], BF16, f"vwn{i}") for i in range(2)]
    for t in vsl + vwn:
        fw.op("dve", lambda e: e.memset(t[:, :, 128:129], 1.0), writes=[t])
    eTs = [fw.sb([128, 512], BF16, f"eT{i}") for i in range(4)]
    pTs = [fw.sb([128, 4, 128], BF16, f"pT{i}") for i in range(8)]
    mks = [fw.sb([128, 128], BF16, f"mk{i}") for i in range(2)]
    o_out = fw.sb([128, D], F32, "o_out")
    o_bf = fw.sb([128, D], BF16, "o_bf")
    rinv = fw.sb([128, 8], F32, "rinv")
    wgt = fw.sb([128, 8], F32, "wgt")
    imp = fw.sb([128, 64], F32, "imp")
    score = fw.sb([128, 64], F32, "score")
    tmpm = fw.sb([128, 64], F32, "tmpm")
    mx = fw.sb([128, 16], F32, "mx")
    sel_bf = fw.sb([128, 64], BF16, "sel_bf")
    selT = fw.sb([64, 128], BF16, "selT")
    MB = 4
    OB = (5, 6, 7)
    obufs = [banks[5], banks[6], banks[7]]
    only_br = None
    for d_ in dbg:
        if d_.startswith("C") and len(d_) == 2:
            only_br = int(d_[1])

    def oacc(h, lo=0, hi=129):
        return PS[:, 5 + h // 3, (h % 3) * 129 + lo:(h % 3) * 129 + hi]

    cnt = {"e": 0, "p": 0, "s": 0, "m": 0}

    NS = 4

    def scores(kT_ap, qblk, Rk, hf):
        b = cnt["s"] % NS
        cnt["s"] += 1
        mm(bk(b), kT_ap, qblk[:, hf * 4:(hf + 1) * 4, :], True, True, Rk + [qT], [banks[b]])
        eT = eTs[cnt["e"] % len(eTs)]
        cnt["e"] += 1
        fw.op("act", lambda e: e.activation(eT[:], bk(b), AF.Exp), reads=[banks[b]], writes=[eT])
        return eT

    def masked(eT, mask_ap, Rm):
        pT = pTs[cnt["p"] % len(pTs)]
        cnt["p"] += 1
        fw.op("dve", lambda e: e.tensor_tensor(pT[:], eT[:].rearrange("p (h t) -> p h t", h=4),
                                               mask_ap.unsqueeze(1).to_broadcast([128, 4, 128]), ALU.mult),
              reads=[eT] + Rm, writes=[pT])
        return pT

    def pv(pT, v_ap, Rv, first, last, hf):
        for h4 in range(4):
            h = hf * 4 + h4
            mm(oacc(h), pT[:, h4, :], v_ap, first and h % 3 == 0, last, [pT] + Rv, [obufs[h // 3]],
               inc=(h4 == 3), sgc=True)

    ocp = [fw.sb([128, 3, 387], F32, f"ocp{i}") for i in range(2)]
    pimp_sb = fw.sb([128, 512], F32, "pimp_sb")
    maskT = fw.sb([128, 32, 128], BF16, "maskT")
    ptmp = [fw.sb([128, 128], F32, f"ptmp{i}") for i in range(2)]
    ocnt = [0]

    def grab():
        oc = ocp[ocnt[0] % 2]
        ocnt[0] += 1
        for bnk in range(3):
            nh = 3 if bnk < 2 else 2
            if bnk == 1:
                fw.op("act", lambda e: e.copy(oc[:, bnk, 0:nh * 129], PS[:, 5 + bnk, 0:nh * 129]), reads=[obufs[bnk]], writes=[oc])
            else:
                fw.op("dve", lambda e: e.tensor_copy(oc[:, bnk, 0:nh * 129], PS[:, 5 + bnk, 0:nh * 129]), reads=[obufs[bnk]], writes=[oc])
        return oc

    def fin_ops(i, g, br, oc):
        ops = []
        ocv = oc[:].rearrange("p b (h c) -> p b h c", c=129)

        def och(h, lo, hi):
            return oc[:, h // 3, (h % 3) * 129 + lo:(h % 3) * 129 + hi]

        def f_rs():
            for bnk in range(3):
                nh = 3 if bnk < 2 else 2
                fw.op("dve", lambda e: e.tensor_scalar(rinv[:, bnk * 3:bnk * 3 + nh], ocv[:, bnk, 0:nh, 128], 1e-30, None, ALU.max),
                      reads=[oc], writes=[rinv])
            fw.op("dve", lambda e: e.reciprocal(rinv[:], rinv[:]), reads=[rinv], writes=[rinv])
            if only_br is None:
                gv = gates[:, i, g * 24 + br:g * 24 + 24:3]
                fw.op("dve", lambda e: e.tensor_tensor(wgt[:], rinv[:], gv, ALU.mult), reads=[rinv, gates], writes=[wgt])
            elif only_br == br:
                fw.op("dve", lambda e: e.tensor_copy(wgt[:], rinv[:]), reads=[rinv], writes=[wgt])
            else:
                fw.op("dve", lambda e: e.memset(wgt[:], 0.0), writes=[wgt])
        ops.append(f_rs)
        for h in range(8):
            def f_h(h=h):
                dst = o_out[:, (g * 8 + h) * 128:(g * 8 + h + 1) * 128]
                if br == 0:
                    fw.op("dve", lambda e: e.tensor_scalar(dst, och(h, 0, 128), wgt[:, h:h + 1], None, ALU.mult),
                          reads=[oc, wgt], writes=[o_out])
                else:
                    fw.op("dve", lambda e: e.scalar_tensor_tensor(dst, och(h, 0, 128), wgt[:, h:h + 1], dst, ALU.mult, ALU.add),
                          reads=[oc, wgt, o_out], writes=[o_out])
            ops.append(f_h)
        return ops

    def topk_ops(i):
        ops = []

        def f0():
            fw.op("act", lambda e: e.copy(pimp_sb[:], bk(MB)), reads=[banks[MB]], writes=[pimp_sb])
        ops.append(f0)
        for h in range(8):
            def f(h=h):
                src = pimp_sb[:, h * 64:(h + 1) * 64]
                if h == 0:
                    fw.op("dve", lambda e: e.tensor_scalar(imp[:], src, rinv[:, 0:1], None, ALU.mult), reads=[pimp_sb, rinv], writes=[imp])
                else:
                    fw.op("dve", lambda e: e.scalar_tensor_tensor(imp[:], src, rinv[:, h:h + 1], imp[:], ALU.mult, ALU.add),
                          reads=[pimp_sb, rinv, imp], writes=[imp])
            ops.append(f)

        def f1():
            fw.op("dve", lambda e: e.tensor_tensor(score[:], imp[:], selA[:, i, :], ALU.mult), reads=[imp, selA], writes=[score])
            fw.op("dve", lambda e: e.tensor_tensor(score[:], score[:], selB[:, i, :], ALU.add), reads=[score, selB], writes=[score])
            fw.op("dve", lambda e: e.max(out=mx[:, 0:8], in_=score[:]), reads=[score], writes=[mx])
        ops.append(f1)

        def f2():
            fw.op("dve", lambda e: e.match_replace(out=tmpm[:], in_to_replace=mx[:, 0:8], in_values=score[:], imm_value=-1e30),
                  reads=[score, mx], writes=[tmpm])
            fw.op("dve", lambda e: e.max(out=mx[:, 8:16], in_=tmpm[:]), reads=[tmpm], writes=[mx])
            fw.op("dve", lambda e: e.tensor_scalar(sel_bf[:], score[:], mx[:, 15:16], None, ALU.is_ge), reads=[score, mx], writes=[sel_bf])
        ops.append(f2)

        def f3():
            fw.op("pe", lambda e: e.transpose(bkbf(MB)[0:64, 0:128], sel_bf[:], ident[:]), reads=[sel_bf, ident], writes=[banks[MB]])
            fw.op("act", lambda e: e.copy(selT[:], bkbf(MB)[0:64, 0:128]), reads=[banks[MB]], writes=[selT])
        ops.append(f3)
        return ops

    pending = []

    def drain(n):
        for _ in range(min(n, len(pending))):
            pending.pop(0)()

    SKEW = 3

    def run_tiles(fronts, backs, per_tile=2):
        live = []
        for k in range(len(fronts)):
            live.append(fronts[k]())
            if k >= SKEW:
                backs[k - SKEW](live[k - SKEW])
            drain(per_tile)
        for k in range(max(len(fronts) - SKEW, 0), len(fronts)):
            backs[k](live[k])

    kT_all = kT_d.t.ap()
    v_all = v_d.t.ap()

    def kv_loads(i, g):
        it = i * 2 + g
        nsl = 4 * i + 4
        w0 = max(4 * i - 4, 0)
        nw = 4 * i + 4 - w0
        ks, vs, kw, vw = ksl[it % 2], vsl[it % 2], kwn[it % 2], vwn[it % 2]
        fw.dma("sp", ks[:, 0:nsl * 128], kT_all[g, :, 0:nsl * 128], reads=[kT_d], writes=[ks])
        fw.dma("sp", vs[:, 0:nsl, 0:128], v_all[0:nsl * 128, g * 128:(g + 1) * 128].rearrange("(t p) d -> p t d", p=128),
               reads=[v_d], writes=[vs])
        fw.dma("sp", kw[:, 0:nw * 128], kT_all[2 + g, :, w0 * 128:(w0 + nw) * 128], reads=[kT_d], writes=[kw])
        fw.dma("sp", vw[:, 0:nw, 0:128],
               v_all[w0 * 128:(w0 + nw) * 128, 256 + g * 128:256 + (g + 1) * 128].rearrange("(t p) d -> p t d", p=128),
               reads=[v_d], writes=[vw])

    def block_out(i):
        def f():
            if "C" in dbg or only_br is not None:
                if "o" not in dbg_out:
                    dbg_out["o"] = fw.dram("dbg_o", [NB, 128, D], F32, kind="ExternalOutput")
                fw.dma("sp", dbg_out["o"].t.ap()[i], o_out[:], reads=[o_out], writes=[dbg_out["o"]])
            fw.op("act", lambda e: e.copy(o_bf[:], o_out[:]), reads=[o_out], writes=[o_bf])
            for half in range(2):
                b = MB
                for k8 in range(8):
                    kc = half * 8 + k8
                    fw.op("pe", lambda e: e.transpose(bkbf(b)[:, k8 * 128:(k8 + 1) * 128], o_bf[:, kc * 128:(kc + 1) * 128], ident[:]),
                          reads=[o_bf, ident], writes=[banks[b]], inc=(k8 == 7))
                fw.op("act", lambda e: e.copy(o_nsaT[:, half * 8:(half + 1) * 8, i * 128:(i + 1) * 128],
                                              bkbf(b).rearrange("p (k t) -> p k t", k=8)),
                      reads=[banks[b]], writes=[o_nsaT])
        return f

    units = []

    def add_iter(i, g):
        it = i * 2 + g
        nsl = 4 * i + 4
        w0 = max(4 * i - 4, 0)
        nw = 4 * i + 4 - w0
        ks, vs, kw, vw = ksl[it % 2], vsl[it % 2], kwn[it % 2], vwn[it % 2]
        qblk = qT[:, i, g * 8:(g + 1) * 8, :]
        nch = 1 if i < 4 else 2
        pcs = {}
        first = len(units)

        def c_front(ch, hf):
            eT = scores(kcmpT[:, g, ch * 128:(ch + 1) * 128], qblk, [kcmpT], hf)
            p_ = masked(eT, cmask[:, i, ch, :], [cmask])
            pcs[(ch, hf)] = p_
            return p_

        def c_post():
            drain(len(pending))
            for h in range(8):
                for ch in range(nch):
                    mm(bk(MB)[:, h * 64:(h + 1) * 64], pcs[(ch, h // 4)][:, h % 4, :], selmap[:, ch, :], ch == 0 and h == 0, ch == nch - 1,
                       [pcs[(ch, h // 4)], selmap], [banks[MB]], inc=(h == 7 and ch == nch - 1) or None, sgc=True)
            drain(len(pending))
            oc = grab()
            pending.extend(fin_ops(i, g, 0, oc))
            pending.extend(topk_ops(i))
            for q4 in range(i + 1):
                def f_mask(q4=q4):
                    for k4 in range(4):
                        mm(bk(MB)[:, k4 * 128:(k4 + 1) * 128], exall[:, q4 * 4 + k4, :], selT[:], True, True, [exall, selT], [banks[MB]],
                           inc=(k4 == 3))
                    src = bk(MB).rearrange("p (k t) -> p k t", k=4)
                    if q4 == i:
                        fw.op("dve", lambda e: e.tensor_tensor(maskT[:, q4 * 4:(q4 + 1) * 4, :], src, dmask[:], ALU.mult),
                              reads=[banks[MB], dmask], writes=[maskT])
                    else:
                        fw.op("act", lambda e: e.copy(maskT[:, q4 * 4:(q4 + 1) * 4, :], src), reads=[banks[MB]], writes=[maskT])
                pending.append(f_mask)

        cu = [(ch, hf) for ch in range(nch) for hf in range(2)]
        for n_, (ch, hf) in enumerate(cu):
            units.append(dict(pre=None, front=(lambda ch=ch, hf=hf: c_front(ch, hf)),
                              back=(lambda p_, ch=ch, hf=hf: pv(p_, vcmp[:, ch, g, :], [vcmp], ch == 0, ch == nch - 1, hf)),
                              post=c_post if n_ == len(cu) - 1 else None))

        def w_front(r_, hf):
            eT = scores(kw[:, r_ * 128:(r_ + 1) * 128], qblk, [kw], hf)
            return masked(eT, wmask[:, w0 + r_ - (4 * i - 4), :], [wmask])

        def w_post():
            drain(len(pending))
            oc = grab()
            pending.extend(fin_ops(i, g, 2, oc))

        wu = [(r_, hf) for r_ in range(nw) for hf in range(2)]
        for n_, (r_, hf) in enumerate(wu):
            units.append(dict(pre=None, front=(lambda r_=r_, hf=hf: w_front(r_, hf)),
                              back=(lambda p_, r_=r_, hf=hf: pv(p_, vw[:, r_, :], [vw], r_ == 0, r_ == nw - 1, hf)),
                              post=w_post if n_ == len(wu) - 1 else None))

        def s_front(kt, hf):
            eT = scores(ks[:, kt * 128:(kt + 1) * 128], qblk, [ks], hf)
            return masked(eT, maskT[:, kt, :], [maskT])

        def s_post():
            drain(len(pending))
            oc = grab()
            pending.extend(fin_ops(i, g, 1, oc))
            if g == 1:
                pending.append(block_out(i))

        su = [(kt, hf) for kt in range(nsl) for hf in range(2)]
        for n_, (kt, hf) in enumerate(su):
            units.append(dict(pre=(lambda: drain(len(pending))) if n_ == 0 else None,
                              front=(lambda kt=kt, hf=hf: s_front(kt, hf)),
                              back=(lambda p_, kt=kt, hf=hf: pv(p_, vs[:, kt, :], [vs], kt == 0, kt == nsl - 1, hf)),
                              post=s_post if n_ == len(su) - 1 else None))
        if it + 1 < 2 * NB:
            ni, ng = (it + 1) // 2, (it + 1) % 2
            old_pre = units[first + 6]["pre"]

            def pre6(old_pre=old_pre, ni=ni, ng=ng):
                if old_pre is not None:
                    old_pre()
                kv_loads(ni, ng)
            units[first + 6]["pre"] = pre6

    for i in range(NB):
        for g in range(2):
            add_iter(i, g)
    kv_loads(0, 0)
    live = {}

    def do_back(k):
        units[k]["back"](live.pop(k))
        if units[k]["post"] is not None:
            units[k]["post"]()

    for k, u in enumerate(units):
        if u["pre"] is not None:
            u["pre"]()
        live[k] = u["front"]()
        if k >= SKEW:
            do_back(k - SKEW)
        drain(2)
    for k in range(len(units) - SKEW, len(units)):
        do_back(k)
    drain(len(pending))

    if "C" in dbg or only_br is not None:
        fw.finish(list(dbg_out.values()))
        return nc, fw


    BASE = 16512 + 1024

    def at(kib, shape, dt_, name):
        assert (kib * 1024) % 32 == 0, (name, kib)
        fw.nsb += 1
        return Buf(nc.alloc_sbuf_tensor_at(f"{name}_{fw.nsb}", list(shape), dt_, offset=BASE + int(kib * 1024)), name)

    fw.barrier()
    xTo = at(0, [128, 16, NB * 128], BF16, "xTo2")
    uT = at(32, [128, 16, NB * 128], BF16, "uT")
    vtm = at(64, [128, NB, D], BF16, "vtm")
    wst = [at(96, [128, 16, 512], BF16, "wst0"), at(112, [128, 16, 512], BF16, "wst1")]
    xbo2 = [at(128, [128, D], BF16, "xbo2a"), at(132, [128, D], BF16, "xbo2b")]
    wsT_f = at(136, [128, 8, 128], F32, "wsT_f")
    tri = at(140, [128, 128], F32, "tri")
    wcT = at(140.5, [128, 8, 128], BF16, "wcT")
    addt = at(142.5, [128, 16, 128], F32, "addt")
    gcol = at(150.5, [128, 16], F32, "gcol")
    bcol = at(150.75, [128, 16], F32, "bcol")
    bsrow = at(151, [1, 8 * 128], BF16, "bsrow")
    onesr = at(153, [128, 128], BF16, "onesr")
    stats = at(153.5, [128, 4, 6], F32, "stats")
    mv = at(154, [128, 2], F32, "mv")
    rstd = at(154.25, [128, 1], F32, "rstd")
    tmpz = [at(155, [128, 128], F32, "tmpz0"), at(155.5, [128, 128], F32, "tmpz1")]
    for blk in range(NB):
        load_xT(xo_d, blk * 128, xbo2[blk % 2], xTo, blk * 128, "act" if blk % 2 == 0 else "dve")
    fw.dma("sp", wsT_f[:], sgu_wsT.t.ap().rearrange("g s t -> s g t"), reads=[sgu_wsT], writes=[wsT_f])
    fw.dma("sp", tri[:], C["tri"][:], reads=[C["tri"]], writes=[tri])
    fw.dma("sp", gcol[:], sgu_g_fm[:], reads=[sgu_g_fm], writes=[gcol])
    fw.dma("sp", bcol[:], sgu_b_fm[:], reads=[sgu_b_fm], writes=[bcol])
    fw.dma("pool", bsrow[:], sgu_bs[:], reads=[sgu_bs], writes=[bsrow])
    fw.op("dve", lambda e: e.memset(onesr[:], 1.0), writes=[onesr])
    fw.op("dve", lambda e: e.tensor_tensor(wcT[:], wsT_f[:], tri[:].unsqueeze(1).to_broadcast([128, 8, 128]), ALU.mult),
          reads=[wsT_f, tri], writes=[wcT])
    for g8 in range(8):
        b = nextbank()
        mm(bk(b)[:, 0:128], onesr[:], wcT[:, g8, :], True, True, [onesr, wcT], [banks[b]])
        b2 = nextbank()
        mm(bk(b2)[:, 0:128], onesr[0:1, :], bsrow[0:1, g8 * 128:(g8 + 1) * 128], True, True, [onesr, bsrow], [banks[b2]])
        for f2 in range(2):
            fc = g8 * 2 + f2
            fw.op("dve", lambda e: e.tensor_scalar(addt[:, fc, :], bk(b)[:, 0:128], bcol[:, fc:fc + 1], None, ALU.mult),
                  reads=[banks[b], bcol], writes=[addt])
            fw.op("dve", lambda e: e.tensor_tensor(addt[:, fc, :], addt[:, fc, :], bk(b2)[:, 0:128], ALU.add),
                  reads=[banks[b2], addt], writes=[addt])
    for cg in range(4):
        w = wst[cg % 2]
        fw.dma("pool", w[:], w_in_r[:, :, OFF_V + cg * 512:OFF_V + (cg + 1) * 512], reads=[w_in], writes=[w])
        for blk in range(NB):
            b = nextbank()
            for kc in range(16):
                mm(bk(b), xTo[:, kc, blk * 128:(blk + 1) * 128], w[:, kc, :], kc == 0, kc == 15, [xTo, w], [banks[b]])
            fw.op("act", lambda e: e.activation(vtm[:, blk, cg * 512:(cg + 1) * 512], bk(b), AF.Gelu_apprx_tanh),
                  reads=[banks[b]], writes=[vtm])
    for blk in range(NB):
        for c4 in range(4):
            fw.op("dve", lambda e: e.bn_stats(stats[:, c4, :], vtm[:, blk, c4 * 512:(c4 + 1) * 512]), reads=[vtm], writes=[stats])
        fw.op("dve", lambda e: e.bn_aggr(mv[:], stats[:]), reads=[stats], writes=[mv])
        fw.op("act", lambda e: e.activation(rstd[:], mv[:, 1:2], AF.Sqrt, bias=epsc[:]), reads=[mv, epsc], writes=[rstd])
        fw.op("dve", lambda e: e.reciprocal(rstd[:], rstd[:]), reads=[rstd], writes=[rstd])
        fw.op("dve", lambda e: e.tensor_scalar(vtm[:, blk, :], vtm[:, blk, :], mv[:, 0:1], rstd[:, 0:1], ALU.subtract, ALU.mult),
              reads=[vtm, mv, rstd], writes=[vtm])
    for cg in range(4):
        w = wst[cg % 2]
        fw.dma("pool", w[:], w_in_r[:, :, OFF_U + cg * 512:OFF_U + (cg + 1) * 512], reads=[w_in], writes=[w])
        for half in range(2):
            for f4 in range(4):
                b = nextbank()
                for kc in range(16):
                    mm(bk(b), w[:, kc, f4 * 128:(f4 + 1) * 128], xTo[:, kc, half * 512:(half + 1) * 512], kc == 0, kc == 15, [w, xTo], [banks[b]])
                fw.op("act", lambda e: e.activation(uT[:, cg * 4 + f4, half * 512:(half + 1) * 512], bk(b), AF.Gelu_apprx_tanh),
                      reads=[banks[b]], writes=[uT])
    zi = 0
    for blk in range(NB):
        for q4 in range(4):
            b = nextbank()
            for f4 in range(4):
                fc = q4 * 4 + f4
                mm(bk(b)[:, f4 * 128:(f4 + 1) * 128], vtm[:, blk, fc * 128:(fc + 1) * 128], wcT[:, fc // 2, :], True, True,
                   [vtm, wcT], [banks[b]], inc=(f4 == 3))
            for f4 in range(4):
                fc = q4 * 4 + f4
                tz = tmpz[zi % 2]
                zi += 1
                fw.op("dve", lambda e: e.scalar_tensor_tensor(tz[:], bk(b)[:, f4 * 128:(f4 + 1) * 128], gcol[:, fc:fc + 1], addt[:, fc, :],
                                                              ALU.mult, ALU.add), reads=[banks[b], gcol, addt], writes=[tz])
                fw.op("pool", lambda e: e.tensor_tensor(uT[:, fc, blk * 128:(blk + 1) * 128], uT[:, fc, blk * 128:(blk + 1) * 128], tz[:], ALU.mult),
                      reads=[uT, tz], writes=[uT])
    if "D" in dbg:
        dbg_out["osguT"] = fw.dram("dbg_osguT", [128, 16, NB * 128], BF16, kind="ExternalOutput")
        fw.dma("sp", dbg_out["osguT"].t.ap(), uT[:], reads=[uT], writes=[dbg_out["osguT"]])
        fw.finish(list(dbg_out.values()))
        return nc, fw

    fw.barrier()
    mergedT = at(70, [128, 16, NB * 128], BF16, "mergedT")
    wm_ = [[at(102 + 8 * (2 * k + i2), [128, 16, 256], BF16, f"wm{k}{i2}") for i2 in range(2)] for k in range(2)]
    sg = [at(134, [128, 512], F32, "sg0"), at(136, [128, 512], F32, "sg1")]
    m1 = [at(138, [128, 512], F32, "m10"), at(140, [128, 512], F32, "m11")]
    wa_r, wb_r = r3(w_br_a), r3(w_br_b)
    ei = 0
    for ps_ in range(2):
        srcT = o_nsaT if ps_ == 0 else uT
        wr = wa_r if ps_ == 0 else wb_r
        goff = OFF_GA if ps_ == 0 else OFF_GB
        for cg in range(8):
            wA, wG = wm_[0][cg % 2], wm_[1][cg % 2]
            fw.dma("pool", wA[:], wr[:, :, cg * 256:(cg + 1) * 256], reads=[w_br_a, w_br_b], writes=[wA])
            fw.dma("pool", wG[:], w_in_r[:, :, goff + cg * 256:goff + (cg + 1) * 256], reads=[w_in], writes=[wG])
            for fc in range(2):
                for half in range(2):
                    ba = nextbank()
                    for kc in range(16):
                        mm(bk(ba), wA[:, kc, fc * 128:(fc + 1) * 128], srcT[:, kc, half * 512:(half + 1) * 512], kc == 0, kc == 15, [wA, srcT], [banks[ba]])
                    bg = nextbank()
                    for kc in range(16):
                        mm(bk(bg), wG[:, kc, fc * 128:(fc + 1) * 128], xTo[:, kc, half * 512:(half + 1) * 512], kc == 0, kc == 15, [wG, xTo], [banks[bg]])
                    s_, m_ = sg[ei % 2], m1[ei % 2]
                    ei += 1
                    dst = mergedT[:, cg * 2 + fc, half * 512:(half + 1) * 512]
                    fw.op("act", lambda e: e.activation(s_[:], bk(bg), AF.Sigmoid), reads=[banks[bg]], writes=[s_])
                    if ps_ == 0:
                        fw.op("dve", lambda e: e.tensor_tensor(dst, s_[:], bk(ba), ALU.mult), reads=[s_, banks[ba]], writes=[mergedT])
                    else:
                        fw.op("dve", lambda e: e.tensor_tensor(m_[:], s_[:], bk(ba), ALU.mult), reads=[s_, banks[ba]], writes=[m_])
                        fw.op("dve", lambda e: e.tensor_tensor(dst, dst, m_[:], ALU.add), reads=[m_, mergedT], writes=[mergedT])
    fw.barrier()
    x1 = at(102, [128, NB, D], F32, "x1")
    xT1 = at(166, [128, 16, NB * 128], BF16, "xT1")
    wo_ = [at(32, [128, 16, 512], BF16, "wo0"), at(48, [128, 16, 512], BF16, "wo1")]
    xst = [at(64 + 2 * i3, [128, 512], F32, f"xst{i3}") for i3 in range(3)]
    lnt = [at(0, [128, D], F32, "lnt0"), at(8, [128, D], F32, "lnt1")]
    lng = at(16, [128, D], F32, "lng")
    lnb = at(24, [128, D], F32, "lnb")
    stats2 = at(198, [128, 4, 6], F32, "stats2")
    mv2 = at(198.125, [128, 2], F32, "mv2")
    rstd2 = at(198.25, [128, 1], F32, "rstd2")
    xbf = [at(198.5, [128, D], BF16, "xbf0"), at(202.5, [128, D], BF16, "xbf1")]
    wo_r = r3(w_o_d)

    def layer_norm(acc, g_d, b_d, xT_out, out_dram=None):
        fw.dma("sp", lng[:], g_d.t.ap().to_broadcast([128, D]), reads=[g_d], writes=[lng])
        fw.dma("sp", lnb[:], b_d.t.ap().to_broadcast([128, D]), reads=[b_d], writes=[lnb])
        for blk in range(NB):
            t_ = lnt[blk % 2]
            for c4 in range(4):
                fw.op("dve", lambda e: e.bn_stats(stats2[:, c4, :], acc[:, blk, c4 * 512:(c4 + 1) * 512]), reads=[acc], writes=[stats2])
            fw.op("dve", lambda e: e.bn_aggr(mv2[:], stats2[:]), reads=[stats2], writes=[mv2])
            fw.op("act", lambda e: e.activation(rstd2[:], mv2[:, 1:2], AF.Sqrt, bias=epsc[:]), reads=[mv2, epsc], writes=[rstd2])
            fw.op("dve", lambda e: e.reciprocal(rstd2[:], rstd2[:]), reads=[rstd2], writes=[rstd2])
            fw.op("dve", lambda e: e.tensor_scalar(t_[:], acc[:, blk, :], mv2[:, 0:1], rstd2[:, 0:1], ALU.subtract, ALU.mult),
                  reads=[acc, mv2, rstd2], writes=[t_])
            fw.op("dve", lambda e: e.tensor_tensor(t_[:], t_[:], lng[:], ALU.mult), reads=[t_, lng], writes=[t_])
            if out_dram is None:
                fw.op("pool", lambda e: e.tensor_tensor(acc[:, blk, :], t_[:], lnb[:], ALU.add), reads=[t_, lnb], writes=[acc])
            else:
                fw.op("pool", lambda e: e.tensor_tensor(t_[:], t_[:], lnb[:], ALU.add), reads=[t_, lnb], writes=[t_])
                fw.dma("sp", out_dram.t.ap()[blk * 128:(blk + 1) * 128, :], t_[:], reads=[t_], writes=[out_dram])
            if xT_out is not None:
                xb_ = xbf[blk % 2]
                fw.op("act", lambda e: e.copy(xb_[:], acc[:, blk, :]), reads=[acc], writes=[xb_])
                for half in range(2):
                    b = nextbank()
                    for k8 in range(8):
                        kc = half * 8 + k8
                        fw.op("pe", lambda e: e.transpose(bkbf(b)[:, k8 * 128:(k8 + 1) * 128], xb_[:, kc * 128:(kc + 1) * 128], ident[:]),
                              reads=[xb_, ident], writes=[banks[b]], inc=(k8 == 7))
                    fw.op("act", lambda e: e.copy(xT_out[:, half * 8:(half + 1) * 8, blk * 128:(blk + 1) * 128],
                                                  bkbf(b).rearrange("p (k t) -> p k t", k=8)), reads=[banks[b]], writes=[xT_out])

    xi = 0
    for cg in range(4):
        w = wo_[cg % 2]
        fw.dma("pool", w[:], wo_r[:, :, cg * 512:(cg + 1) * 512], reads=[w_o_d], writes=[w])
        for blk in range(NB):
            xs_ = xst[xi % 3]
            xi += 1
            fw.dma("sp", xs_[:], xo_d[blk * 128:(blk + 1) * 128, cg * 512:(cg + 1) * 512], reads=[xo_d], writes=[xs_])
            b = nextbank()
            for kc in range(16):
                mm(bk(b), mergedT[:, kc, blk * 128:(blk + 1) * 128], w[:, kc, :], kc == 0, kc == 15, [mergedT, w], [banks[b]])
            fw.op("dve", lambda e: e.scalar_tensor_tensor(x1[:, blk, cg * 512:(cg + 1) * 512], xs_[:], ALPHA, bk(b), ALU.mult, ALU.add),
                  reads=[xs_, banks[b]], writes=[x1])
    wxk = at(32, [128, 16, 512], BF16, "wxk")
    wxv = at(48, [128, 16, 512], BF16, "wxv")
    fw.dma("pool", wxk[:], r3(w_xk), reads=[w_xk], writes=[wxk, wo_[0]])
    fw.dma("pool", wxv[:], r3(w_xv), reads=[w_xv], writes=[wxv, wo_[1]])
    layer_norm(x1, ln1_g, ln1_b, xT1)
    if "E" in dbg:
        dbg_out["x1"] = fw.dram("dbg_x1", [128, NB, D], F32, kind="ExternalOutput")
        fw.dma("sp", dbg_out["x1"].t.ap(), x1[:], reads=[x1], writes=[dbg_out["x1"]])
        fw.finish(list(dbg_out.values()))
        return nc, fw


    fw.barrier()
    memb = [at(64, [128, D], BF16, "memb0"), at(68, [128, D], BF16, "memb1")]
    memT = at(72, [128, 16, 256], BF16, "memT")
    KmT = at(80, [128, 4, 256], BF16, "KmT")
    Vm = at(82, [128, 2, 4, 129], BF16, "Vm")
    for mb in range(2):
        load_xT(mem_d, mb * 128, memb[mb], memT, mb * 128, "act" if mb == 0 else "dve")
    fw.op("dve", lambda e: e.memset(Vm[:, :, :, 128:129], 1.0), writes=[Vm])
    for h in range(4):
        b = nextbank()
        for kc in range(16):
            mm(bk(b)[:, 0:256], wxk[:, kc, h * 128:(h + 1) * 128], memT[:, kc, :], kc == 0, kc == 15, [wxk, memT], [banks[b]])
        fw.op("act", lambda e: e.copy(KmT[:, h, :], bk(b)[:, 0:256]), reads=[banks[b]], writes=[KmT])
    for mch in range(2):
        b = nextbank()
        for kc in range(16):
            mm(bk(b), memT[:, kc, mch * 128:(mch + 1) * 128], wxv[:, kc, :], kc == 0, kc == 15, [memT, wxv], [banks[b]])
        fw.op("dve", lambda e: e.tensor_copy(Vm[:, mch, :, 0:128], bk(b).rearrange("p (h d) -> p h d", h=4)), reads=[banks[b]], writes=[Vm])
    fw.barrier()
    wxq = at(32, [128, 16, 512], BF16, "wxq")
    wxo = at(48, [128, 4, D], BF16, "wxo")
    qxT = at(64, [128, 4, NB * 128], BF16, "qxT")
    eTx = [at(72, [128, 2, 128], BF16, "eTx0"), at(72.5, [128, 2, 128], BF16, "eTx1")]
    oxb = at(75, [128, 512], BF16, "oxb")
    oxT = at(76, [128, 4, 128], BF16, "oxT")
    rinvx = at(77, [128, 4], F32, "rinvx")
    fw.dma("pool", wxq[:], r3(w_xq), reads=[w_xq], writes=[wxq])
    fw.dma("pool", wxo[:], r3(w_xo), reads=[w_xo], writes=[wxo])
    for h in range(4):
        for half in range(2):
            b = nextbank()
            for kc in range(16):
                mm(bk(b), wxq[:, kc, h * 128:(h + 1) * 128], xT1[:, kc, half * 512:(half + 1) * 512], kc == 0, kc == 15, [wxq, xT1], [banks[b]])
            fw.op("act", lambda e: e.mul(qxT[:, h, half * 512:(half + 1) * 512], bk(b), 128.0 ** -0.5), reads=[banks[b]], writes=[qxT])
    exi = 0
    for blk in range(NB):
        bA, bB = nextbank(), nextbank()

        def ox_ps(h, lo=0, hi=129):
            return PS[:, bA if h < 3 else bB, (h % 3) * 129 + lo:(h % 3) * 129 + hi]

        for h in range(4):
            b = nextbank()
            for mch in range(2):
                mm(bk(b)[:, mch * 128:(mch + 1) * 128], KmT[:, h, mch * 128:(mch + 1) * 128], qxT[:, h, blk * 128:(blk + 1) * 128],
                   True, True, [KmT, qxT], [banks[b]], inc=(mch == 1))
            et = eTx[exi % 2]
            exi += 1
            fw.op("act", lambda e: e.activation(et[:], bk(b)[:, 0:256].rearrange("p (c t) -> p c t", c=2), AF.Exp), reads=[banks[b]], writes=[et])
            for mch in range(2):
                mm(ox_ps(h), et[:, mch, :], Vm[:, mch, h, :], mch == 0, mch == 1, [et, Vm], [banks[bA if h < 3 else bB]])
        vA = PS[:, bA, 0:387].rearrange("p (h c) -> p h c", c=129)
        fw.op("dve", lambda e: e.reciprocal(rinvx[:, 0:3], vA[:, :, 128]), reads=[banks[bA]], writes=[rinvx])
        fw.op("dve", lambda e: e.reciprocal(rinvx[:, 3:4], PS[:, bB, 128:129]), reads=[banks[bB]], writes=[rinvx])
        for h in range(4):
            fw.op("dve", lambda e: e.tensor_scalar(oxb[:, h * 128:(h + 1) * 128], ox_ps(h, 0, 128), rinvx[:, h:h + 1], None, ALU.mult),
                  reads=[banks[bA if h < 3 else bB], rinvx], writes=[oxb])
        b = nextbank()
        for h in range(4):
            fw.op("pe", lambda e: e.transpose(bkbf(b)[:, h * 128:(h + 1) * 128], oxb[:, h * 128:(h + 1) * 128], ident[:]),
                  reads=[oxb, ident], writes=[banks[b]], inc=(h == 3))
        fw.op("act", lambda e: e.copy(oxT[:], bkbf(b)[:, 0:512].rearrange("p (k t) -> p k t", k=4)), reads=[banks[b]], writes=[oxT])
        for cg in range(4):
            b = nextbank()
            for kc in range(4):
                mm(bk(b), oxT[:, kc, :], wxo[:, kc, cg * 512:(cg + 1) * 512], kc == 0, kc == 3, [oxT, wxo], [banks[b]])
            sl = x1[:, blk, cg * 512:(cg + 1) * 512]
            fw.op("dve", lambda e: e.scalar_tensor_tensor(sl, sl, ALPHA, bk(b), ALU.mult, ALU.add), reads=[x1, banks[b]], writes=[x1])
    layer_norm(x1, ln2_g, ln2_b, None)
    if "F" in dbg:
        dbg_out["x2"] = fw.dram("dbg_x2", [128, NB, D], F32, kind="ExternalOutput")
        fw.dma("sp", dbg_out["x2"].t.ap(), x1[:], reads=[x1], writes=[dbg_out["x2"]])
        fw.finish(list(dbg_out.values()))
        return nc, fw

    fw.barrier()
    acc = x1
    x2b = at(166, [128, NB, D], BF16, "x2b")
    posm_f = at(96, [128, NB, 64], F32, "posm_f")
    GWb = at(98, [128, NB, 64], BF16, "GWb")
    posmT = at(99, [64, NB * 128], BF16, "posmT")
    iota_c = at(101, [128, 64], F32, "iota_c")
    iota_pb = at(101.25, [64, 64], F32, "iota_pb")
    iota_p1 = at(101.5, [64, 1], F32, "iota_p1")
    x2hT = at(32, [128, 16, 128], BF16, "x2hT")
    x2lT = at(36, [128, 16, 128], BF16, "x2lT")
    wr = at(40, [128, 16, 68], F32, "wr")
    wrh = at(51, [128, 16, 68], BF16, "wrh")
    wrl = at(54, [128, 16, 68], BF16, "wrl")
    wtmp = at(57, [128, 16, 68], F32, "wtmp")
    x2l = at(62, [128, D], BF16, "x2l")
    bias_b = at(66, [128, 68], F32, "bias_b")
    logit = at(68, [128, NB, 68], F32, "logit")
    asg = at(46, [128, NB, 64], BF16, "asg")
    ustr = at(47, [128, 128], BF16, "ustr")
    ones_b = at(47.25, [128, 128], BF16, "ones_b")
    msk = at(71, [128, NB, 64], F32, "msk")
    msk2 = at(73, [128, NB, 64], F32, "msk2")
    oh1 = at(75, [128, NB, 64], F32, "oh1")
    oh2 = at(77, [128, NB, 64], F32, "oh2")
    d4 = at(79, [128, NB, 4], F32, "d4")
    e4 = at(79.125, [128, NB, 4], F32, "e4")
    gm = at(79.25, [128, NB, 4], F32, "gm")
    pen = at(79.375, [128, NB, 4], F32, "pen")
    sA_ = at(79.5, [128, NB], F32, "sA_")
    sB_ = at(79.53125, [128, NB], F32, "sB_")
    ggrp = at(79.5625, [128, NB], F32, "ggrp")
    m1_ = at(79.59375, [128, NB], F32, "m1_")
    m2_ = at(79.625, [128, NB], F32, "m2_")
    e2_ = at(79.65625, [128, NB], F32, "e2_")
    g1_ = at(79.6875, [128, NB], F32, "g1_")
    g2_ = at(79.71875, [128, NB], F32, "g2_")
    posb = at(50, [128, 64], BF16, "posb")
    fw.dma("sp", wr[:], r3(w_rt), reads=[w_rt], writes=[wr])
    fw.dma("sp", bias_b[:], b_rt.t.ap().to_broadcast([128, 68]), reads=[b_rt], writes=[bias_b])
    fw.dma("sp", iota_c[:], C["iota_c"][:], reads=[C["iota_c"]], writes=[iota_c])
    fw.dma("sp", iota_pb[:], C["iota_pb"][:], reads=[C["iota_pb"]], writes=[iota_pb])
    fw.op("dve", lambda e: e.tensor_copy(iota_p1[:], iota_pb[:, 0:1]), reads=[iota_pb], writes=[iota_p1])
    fw.dma("pool", ustr[:], C["ustrict"][:], reads=[C["ustrict"]], writes=[ustr])
    fw.op("dve", lambda e: e.memset(ones_b[:], 1.0), writes=[ones_b])
    fw.op("dve", lambda e: e.tensor_copy(wrh[:], wr[:]), reads=[wr], writes=[wrh])
    fw.op("dve", lambda e: e.tensor_copy(wtmp[:], wrh[:]), reads=[wrh], writes=[wtmp])
    fw.op("dve", lambda e: e.tensor_tensor(wrl[:], wr[:], wtmp[:], ALU.subtract), reads=[wr, wtmp], writes=[wrl])
    for blk in range(NB):
        fw.op("act", lambda e: e.copy(x2b[:, blk, :], acc[:, blk, :]), reads=[acc], writes=[x2b])
        fw.op("dve", lambda e: e.tensor_tensor(x2l[:], acc[:, blk, :], x2b[:, blk, :], ALU.subtract), reads=[acc, x2b], writes=[x2l])
        for (srcb, src_ap, dstT) in ((x2b, x2b[:, blk, :], x2hT), (x2l, x2l[:], x2lT)):
            for half in range(2):
                b = nextbank()
                for k8 in range(8):
                    kc = half * 8 + k8
                    fw.op("pe", lambda e: e.transpose(bkbf(b)[:, k8 * 128:(k8 + 1) * 128], src_ap[:, kc * 128:(kc + 1) * 128], ident[:]),
                          reads=[srcb, ident], writes=[banks[b]], inc=(k8 == 7))
                if half == 0:
                    fw.op("act", lambda e: e.copy(dstT[:, half * 8:(half + 1) * 8, :], bkbf(b).rearrange("p (k t) -> p k t", k=8)),
                          reads=[banks[b]], writes=[dstT])
                else:
                    fw.op("dve", lambda e: e.tensor_copy(dstT[:, half * 8:(half + 1) * 8, :], bkbf(b).rearrange("p (k t) -> p k t", k=8)),
                          reads=[banks[b]], writes=[dstT])
        b = nextbank()
        trip = [(x2hT, wrh), (x2hT, wrl), (x2lT, wrh)]
        for ti, (xt_, w_) in enumerate(trip):
            for kc in range(16):
                mm(bk(b)[:, 0:68], xt_[:, kc, :], w_[:, kc, :], ti == 0 and kc == 0, ti == 2 and kc == 15, [xt_, w_], [banks[b]])
        fw.op("dve", lambda e: e.tensor_tensor(logit[:, blk, :], bk(b)[:, 0:68], bias_b[:], ALU.add), reads=[banks[b], bias_b], writes=[logit])
        fw.op("act", lambda e: e.mul(acc[:, blk, :], acc[:, blk, :], ALPHA), reads=[acc], writes=[acc])
    if "R" in dbg:
        dbg_out["logit"] = fw.dram("dbg_logit", [128, NB, 68], F32, kind="ExternalOutput")
        fw.dma("sp", dbg_out["logit"].t.ap(), logit[:], reads=[logit], writes=[dbg_out["logit"]])
    dv = lambda fn, R, W: fw.op("dve", fn, reads=R, writes=W)
    L4 = logit[:, :, 0:4]
    LE = logit[:, :, 4:68].rearrange("p b (g x) -> p b g x", g=4)
    bc4 = lambda t: t[:].unsqueeze(2).to_broadcast([128, NB, 4])
    bc64 = lambda t: t[:].unsqueeze(2).to_broadcast([128, NB, 64])
    m4 = lambda t: t[:].rearrange("p b (g x) -> p b g x", g=4)
    dv(lambda e: e.reduce_max(sA_[:], L4, AX.X), [logit], [sA_])
    dv(lambda e: e.tensor_tensor(d4[:], L4, bc4(sA_), ALU.subtract), [logit, sA_], [d4])
    fw.op("act", lambda e: e.activation(e4[:], d4[:], AF.Exp), reads=[d4], writes=[e4])
    dv(lambda e: e.reduce_sum(sB_[:], e4[:], AX.X), [e4], [sB_])
    dv(lambda e: e.reciprocal(ggrp[:], sB_[:]), [sB_], [ggrp])
    dv(lambda e: e.tensor_scalar(gm[:], d4[:], 0.0, None, ALU.is_equal), [d4], [gm])
    dv(lambda e: e.tensor_scalar(pen[:], gm[:], 1e9, -1e9, ALU.mult, ALU.add), [gm], [pen])
    dv(lambda e: e.tensor_tensor(m4(msk), LE, gm[:].unsqueeze(3).to_broadcast([128, NB, 4, 16]), ALU.mult), [logit, gm], [msk])
    dv(lambda e: e.tensor_tensor(m4(msk), m4(msk), pen[:].unsqueeze(3).to_broadcast([128, NB, 4, 16]), ALU.add), [msk, pen], [msk])
    dv(lambda e: e.reduce_max(m1_[:], msk[:], AX.X), [msk], [m1_])
    dv(lambda e: e.tensor_tensor(oh1[:], msk[:], bc64(m1_), ALU.is_equal), [msk, m1_], [oh1])
    dv(lambda e: e.scalar_tensor_tensor(msk2[:], oh1[:], -1e9, msk[:], ALU.mult, ALU.add), [oh1, msk], [msk2])
    dv(lambda e: e.reduce_max(m2_[:], msk2[:], AX.X), [msk2], [m2_])
    dv(lambda e: e.tensor_tensor(oh2[:], msk2[:], bc64(m2_), ALU.is_equal), [msk2, m2_], [oh2])
    dv(lambda e: e.tensor_tensor(sA_[:], m2_[:], m1_[:], ALU.subtract), [m1_, m2_], [sA_])
    fw.op("act", lambda e: e.activation(e2_[:], sA_[:], AF.Exp), reads=[sA_], writes=[e2_])
    dv(lambda e: e.tensor_scalar(sB_[:], e2_[:], 1.0, None, ALU.add), [e2_], [sB_])
    dv(lambda e: e.reciprocal(sB_[:], sB_[:]), [sB_], [sB_])
    dv(lambda e: e.tensor_tensor(g1_[:], sB_[:], ggrp[:], ALU.mult), [sB_, ggrp], [g1_])
    dv(lambda e: e.tensor_tensor(g2_[:], g1_[:], e2_[:], ALU.mult), [g1_, e2_], [g2_])
    dv(lambda e: e.tensor_tensor(msk[:], oh1[:], bc64(g1_), ALU.mult), [oh1, g1_], [msk])
    dv(lambda e: e.tensor_tensor(msk2[:], oh2[:], bc64(g2_), ALU.mult), [oh2, g2_], [msk2])
    dv(lambda e: e.tensor_tensor(GWb[:], msk[:], msk2[:], ALU.add), [msk, msk2], [GWb])
    dv(lambda e: e.tensor_tensor(asg[:], oh1[:], oh2[:], ALU.add), [oh1, oh2], [asg])
    for blk in range(NB):
        b = nextbank()
        mm(bk(b)[:, 0:64], ustr[:], asg[:, blk, :], True, blk == 0, [ustr, asg], [banks[b]])
        for b2 in range(blk):
            mm(bk(b)[:, 0:64], ones_b[:], asg[:, b2, :], False, b2 == blk - 1, [ones_b, asg], [banks[b]])
        fw.op("dve", lambda e: e.scalar_tensor_tensor(posm_f[:, blk, :], bk(b)[:, 0:64], 1.0, asg[:, blk, :], ALU.add, ALU.mult),
              reads=[banks[b], asg], writes=[posm_f])
        fw.op("dve", lambda e: e.tensor_scalar(posm_f[:, blk, :], posm_f[:, blk, :], -1.0, 200.0, ALU.add, ALU.min), reads=[posm_f], writes=[posm_f])
        fw.op("dve", lambda e: e.tensor_copy(posb[:], posm_f[:, blk, :]), reads=[posm_f], writes=[posb])
        b3 = nextbank()
        fw.op("pe", lambda e: e.transpose(bkbf(b3)[0:64, 0:128], posb[:], ident[:]), reads=[posb, ident], writes=[banks[b3]])
        fw.op("act", lambda e: e.copy(posmT[:, blk * 128:(blk + 1) * 128], bkbf(b3)[0:64, 0:128]), reads=[banks[b3]], writes=[posmT])
    if "R" in dbg:
        dbg_out["posm"] = fw.dram("dbg_posm", [128, NB, 64], F32, kind="ExternalOutput")
        fw.dma("sp", dbg_out["posm"].t.ap(), posm_f[:], reads=[posm_f], writes=[dbg_out["posm"]])
        dbg_out["GW"] = fw.dram("dbg_GW", [128, NB, 64], BF16, kind="ExternalOutput")
        fw.dma("sp", dbg_out["GW"].t.ap(), GWb[:], reads=[GWb], writes=[dbg_out["GW"]])
        fw.finish(list(dbg_out.values()))
        return nc, fw
    fw.barrier()
    GE = 4
    Ygrp = at(0, [64, GE, D], BF16, "Ygrp")
    SelTg = at(16, [64, GE, NB * 128], BF16, "SelTg")
    XgT = [at(24, [128, 16, CAP], BF16, "XgT0"), at(26, [128, 16, CAP], BF16, "XgT1")]
    Sel = [at(28, [128, NB, CAP], BF16, "Sel0"), at(29, [128, NB, CAP], BF16, "Sel1")]
    hb = at(30, [64, 512], BF16, "hb")
    hT = at(31, [128, 4, CAP], BF16, "hT")
    gslot = at(31.5, [64, 1], F32, "gslot")
    rowsel = at(31.75, [64, 64], BF16, "rowsel")
    sgt = at(200, [64, 512], F32, "sgt")
    wsl = [at(32 + 16 * i4, [128, 16, 512], BF16, f"wsl{i4}") for i4 in range(4)]
    wcnt = [0]

    def wload(src_ap, srcbuf, shape3):
        t = wsl[wcnt[0] % 4]
        wcnt[0] += 1
        view = t[:] if shape3 == 16 else t[:].rearrange("p a (b c) -> p (a b) c", b=4)[:, 0:4, :] if False else None
        return t

    for eg in range(64 // GE):
        for el in range(GE):
            ex = eg * GE + el
            wg_, wu_, wd_ = wsl[(3 * ex) % 4], wsl[(3 * ex + 1) % 4], wsl[(3 * ex + 2) % 4]
            fw.dma("pool", wg_[:], w_eg.t.ap()[ex].rearrange("(kc p) n -> p kc n", p=128), reads=[w_eg], writes=[wg_])
            fw.dma("pool", wu_[:], w_eu.t.ap()[ex].rearrange("(kc p) n -> p kc n", p=128), reads=[w_eu], writes=[wu_])
            wdv = wd_[:].rearrange("p a n -> p (a n)").rearrange("p (f n) -> p f n", f=4)
            fw.dma("pool", wdv, w_ed.t.ap()[ex].rearrange("(fc p) n -> p fc n", p=128), reads=[w_ed], writes=[wd_])
            sel = Sel[ex % 2]
            for blk in range(NB):
                fw.op("dve", lambda e: e.tensor_scalar(sel[:, blk, :], iota_c[:], posm_f[:, blk, ex:ex + 1], None, ALU.is_equal),
                      reads=[iota_c, posm_f], writes=[sel])
            xg = XgT[ex % 2]
            for hf in range(2):
                b = nextbank()
                for k8 in range(8):
                    kc = hf * 8 + k8
                    for blk in range(NB):
                        mm(bk(b)[:, k8 * CAP:(k8 + 1) * CAP], x2b[:, blk, kc * 128:(kc + 1) * 128], sel[:, blk, :], blk == 0, blk == NB - 1,
                           [x2b, sel], [banks[b]], inc=(blk == NB - 1 and k8 == 7))
                cp = (lambda e: e.copy(xg[:, hf * 8:(hf + 1) * 8, :], bk(b).rearrange("p (k c) -> p k c", k=8)))
                fw.op("act", cp, reads=[banks[b]], writes=[xg])
            b = nextbank()
            for blk in range(NB):
                mm(bk(b)[0:CAP, 0:1], sel[:, blk, :], GWb[:, blk, ex:ex + 1], blk == 0, blk == NB - 1, [sel, GWb], [banks[b]])
            fw.op("dve", lambda e: e.tensor_copy(gslot[:], bk(b)[0:CAP, 0:1]), reads=[banks[b]], writes=[gslot])
            bg, bu = nextbank(), nextbank()
            for kc in range(16):
                mm(bk(bg)[0:CAP, :], xg[:, kc, :], wg_[:, kc, :], kc == 0, kc == 15, [xg, wg_], [banks[bg]])
            for kc in range(16):
                mm(bk(bu)[0:CAP, :], xg[:, kc, :], wu_[:, kc, :], kc == 0, kc == 15, [xg, wu_], [banks[bu]])
            fw.op("act", lambda e: e.activation(sgt[:], bk(bg)[0:CAP, :], AF.Silu), reads=[banks[bg]], writes=[sgt])
            fw.op("dve", lambda e: e.scalar_tensor_tensor(hb[:], bk(bu)[0:CAP, :], gslot[:, 0:1], sgt[:], ALU.mult, ALU.mult),
                  reads=[banks[bu], gslot, sgt], writes=[hb])
            b = nextbank()
            for fc in range(4):
                fw.op("pe", lambda e: e.transpose(bkbf(b)[:, fc * CAP:(fc + 1) * CAP], hb[:, fc * 128:(fc + 1) * 128], ident[0:CAP, 0:CAP]),
                      reads=[hb, ident], writes=[banks[b]], inc=(fc == 3))
            fw.op("act", lambda e: e.copy(hT[:], bkbf(b)[:, 0:4 * CAP].rearrange("p (k c) -> p k c", k=4)), reads=[banks[b]], writes=[hT])
            for cg in range(4):
                b = nextbank()
                for fc in range(4):
                    mm(bk(b)[0:CAP, :], hT[:, fc, :], wdv[:, fc, cg * 512:(cg + 1) * 512], fc == 0, fc == 3, [hT, wd_], [banks[b]])
                if cg % 2 == 0:
                    fw.op("act", lambda e: e.copy(Ygrp[:, el, cg * 512:(cg + 1) * 512], bk(b)[0:CAP, :]), reads=[banks[b]], writes=[Ygrp])
                else:
                    fw.op("dve", lambda e: e.tensor_copy(Ygrp[:, el, cg * 512:(cg + 1) * 512], bk(b)[0:CAP, :]), reads=[banks[b]], writes=[Ygrp])
            fw.op("dve", lambda e: e.tensor_scalar(rowsel[:], iota_pb[:], float(ex), None, ALU.is_equal), reads=[iota_pb], writes=[rowsel])
            for hf in range(2):
                b = nextbank()
                mm(bk(b)[0:CAP, :], rowsel[:], posmT[:, hf * 512:(hf + 1) * 512], True, True, [rowsel, posmT], [banks[b]])
                fw.op("dve", lambda e: e.tensor_scalar(SelTg[:, el, hf * 512:(hf + 1) * 512], bk(b)[0:CAP, :], iota_p1[:, 0:1], None, ALU.is_equal),
                      reads=[banks[b], iota_p1], writes=[SelTg])
        for blk in range(NB):
            bs4 = [nextbank() for _ in range(4)]
            for el in range(GE):
                for cg in range(4):
                    mm(bk(bs4[cg]), SelTg[:, el, blk * 128:(blk + 1) * 128], Ygrp[:, el, cg * 512:(cg + 1) * 512], el == 0, el == GE - 1,
                       [SelTg, Ygrp], [banks[bs4[cg]]])
            for cg in range(4):
                sl = acc[:, blk, cg * 512:(cg + 1) * 512]
                fw.op("dve", lambda e: e.tensor_tensor(sl, sl, bk(bs4[cg]), ALU.add), reads=[acc, banks[bs4[cg]]], writes=[acc])
    fw.barrier()
    layer_norm(acc, ln3_g, ln3_b, None, out_dram=out_d)
    fw.finish([out_d])
    return nc, fw


def _prep_inputs(inputs, core):
    b, j = core // 4, core % 4
    x = np.asarray(inputs["x"])
    w_in = np.asarray(inputs["w_in"])[0]
    m = {}
    m["x"] = np.ascontiguousarray(x[b])
    own = np.concatenate([np.arange(128 * (4 * i + j), 128 * (4 * i + j) + 128) for i in range(NB)])
    m["x_own"] = np.ascontiguousarray(x[b][own])
    m["w_in"] = w_in

    def swap_halves(w):
        sh = w.shape
        return np.ascontiguousarray(w.reshape(sh[0], -1, 2, 64)[:, :, ::-1, :].reshape(sh))

    m["wq_rot"] = swap_halves(w_in[:, OFF_Q:OFF_Q + 2048])
    kcols = np.concatenate([w_in[:, OFF_KV + 512:OFF_KV + 768], w_in[:, OFF_KV + 1024:OFF_KV + 1280]], axis=1)
    m["wk_rot"] = swap_halves(kcols)
    m["pe_kT"] = np.ascontiguousarray(np.asarray(inputs["cmp_pe_k"])[0].T)
    m["pe_vT"] = np.ascontiguousarray(np.asarray(inputs["cmp_pe_v"])[0].T)
    m["cmp_w1_k"] = np.asarray(inputs["cmp_w1_k"])[0]
    m["cmp_w1_v"] = np.asarray(inputs["cmp_w1_v"])[0]
    m["cmp_w2_k"] = np.asarray(inputs["cmp_w2_k"])[0]
    m["cmp_w2_k_rot"] = swap_halves(np.asarray(inputs["cmp_w2_k"])[0])
    m["cmp_w2_v"] = np.asarray(inputs["cmp_w2_v"])[0]
    g = lambda k: np.asarray(inputs[k])[0]
    m["sgu_wsT"] = np.ascontiguousarray(g("sgu_w_s").transpose(0, 2, 1))
    m["sgu_g_fm"] = np.ascontiguousarray(g("sgu_ln_g").reshape(16, 128).T)
    m["sgu_b_fm"] = np.ascontiguousarray(g("sgu_ln_b").reshape(16, 128).T)
    m["sgu_bs"] = np.ascontiguousarray(g("sgu_b_s").reshape(1, 1024))
    m["w_branch_a"] = g("w_branch_a")
    m["w_branch_b"] = g("w_branch_b")
    m["w_o"] = g("w_o")
    m["ln1_g"] = g("ln1_g").reshape(1, D)
    m["ln1_b"] = g("ln1_b").reshape(1, D)
    m["mem"] = np.ascontiguousarray(np.asarray(inputs["mem"])[b])
    for k in ("w_xq", "w_xk", "w_xv", "w_xo", "w_exp_gate", "w_exp_up", "w_exp_down"):
        m[k] = g(k)
    for k in ("ln2_g", "ln2_b", "ln3_g", "ln3_b"):
        m[k] = g(k).reshape(1, D)
    m["w_router"] = np.ascontiguousarray(np.concatenate([g("w_router_grp"), g("w_router_exp")], axis=1))
    m["b_router"] = np.concatenate([g("b_router_grp"), g("b_router_exp")]).reshape(1, 68)
    for k, v in _consts(j).items():
        m["c_" + k] = v
    return m


def kernel(**inputs):
    nc, fw = build()
    in_maps = [_prep_inputs(inputs, c) for c in range(8)]
    res = run_bass_kernel_spmd(nc, in_maps, core_ids=list(range(8)))
    out = np.zeros((2, S, D), np.float32)
    for c in range(8):
        b, j = c // 4, c % 4
        o = res.results[c]["out"]
        for i in range(NB):
            blk = 4 * i + j
            out[b, blk * 128:(blk + 1) * 128] = o[i * 128:(i + 1) * 128]
    return out
```

```python
import numpy as np
import concourse.bass as bass
import concourse.mybir as mybir
from concourse.bass_utils import run_bass_kernel_spmd

F32 = mybir.dt.float32
BF16 = mybir.dt.bfloat16
I32 = mybir.dt.int32
AF = mybir.ActivationFunctionType
ALU = mybir.AluOpType
AX = mybir.AxisListType

class Buf:
    __slots__ = ("t", "w", "r", "name")

    def __init__(self, t, name=""):
        self.t = t
        self.w = None
        self.r = {}
        self.name = name

    def __getitem__(self, k):
        return self.t[k]


class _Eng:
    def __init__(self, name, eng, sem):
        self.name = name
        self.eng = eng
        self.sem = sem
        self.tick = 0
        self.seen = {}
        self.pool = []
        self.pool_i = 0


class FW:
    def __init__(self, nc, n_dma_sems=6):
        self.nc = nc
        self.E = {}
        for name, eng in (("pe", nc.tensor), ("act", nc.scalar), ("dve", nc.vector),
                          ("pool", nc.gpsimd), ("sp", nc.sync)):
            e = _Eng(name, eng, nc.alloc_semaphore("s_" + name))
            self.E[name] = e
        for q in ("sp", "pool", "act"):
            e = self.E[q]
            for i in range(n_dma_sems):
                e.pool.append([nc.alloc_semaphore(f"d_{q}{i}"), 0])
        self.nsb = 0
        self.n_inst = 0

    def sb(self, shape, dtype, name=None, side=None):
        self.nsb += 1
        name = (name or "sb") + f"_{self.nsb}"
        return Buf(self.nc.alloc_sbuf_tensor(name, list(shape), dtype, side=side), name)

    def ps(self, shape, dtype=F32, name=None):
        self.nsb += 1
        name = name or f"ps{self.nsb}"
        return Buf(self.nc.alloc_psum_tensor(name, list(shape), dtype), name)

    def dram(self, name, shape, dtype, kind="Internal"):
        return Buf(self.nc.dram_tensor(name, list(shape), dtype, kind=kind), name)

    def _deps(self, reads, writes):
        evs = []
        for b in reads:
            if b.w is not None:
                evs.append(b.w)
        for b in writes:
            if b.w is not None:
                evs.append(b.w)
            evs.extend(b.r.values())
        return evs

    def _wait(self, e, evs):
        need = {}
        for (sem, val) in evs:
            k = id(sem)
            if e.seen.get(k, 0) >= val:
                continue
            if k not in need or need[k][1] < val:
                need[k] = (sem, val)
        for k, (sem, val) in need.items():
            if e.name == "pe" and sem is e.sem:
                continue
            e.eng.wait_ge(sem, val)
            e.seen[k] = val

    def _record(self, ev, reads, writes):
        k = id(ev[0])
        for b in reads:
            b.r[k] = ev
        for b in writes:
            b.w = ev
            b.r = {}

    def op(self, ename, fn, reads=(), writes=(), inc=True):
        e = self.E[ename]
        self._wait(e, self._deps(reads, writes))
        ins = fn(e.eng)
        self.n_inst += 1
        if inc:
            e.tick += 1
            ins.then_inc(e.sem, 1)
            self._record((e.sem, e.tick), reads, writes)
        else:
            self._record((e.sem, e.tick + 1), reads, writes)

    def barrier(self):
        evs = []
        for en in self.E.values():
            for s in en.pool:
                if s[1] > 0:
                    evs.append((s[0], s[1]))
            if en.tick > 0:
                evs.append((en.sem, en.tick))
        for en in self.E.values():
            for (sem, val) in evs:
                if en.seen.get(id(sem), 0) < val:
                    en.eng.wait_ge(sem, val)
                    en.seen[id(sem)] = val

    def dma(self, q, out_ap, in_ap, reads=(), writes=(), **kw):
        e = self.E[q]
        slot = e.pool[e.pool_i % len(e.pool)]
        e.pool_i += 1
        evs = self._deps(reads, writes)
        if slot[1] > 0:
            evs.append((slot[0], slot[1]))
        self._wait(e, evs)
        slot[1] += 16
        e.eng.dma_start(out=out_ap, in_=in_ap, **kw).then_inc(slot[0], 16)
        self.n_inst += 1
        self._record((slot[0], slot[1]), reads, writes)

    def finish(self, bufs):
        e = self.E["sp"]
        evs = []
        for b in bufs:
            if b.w is not None:
                evs.append(b.w)
        for en in self.E.values():
            for s in en.pool:
                if s[1] > 0:
                    evs.append((s[0], s[1]))
            if en.tick > 0 and en is not e:
                evs.append((en.sem, en.tick))
        self._wait(e, evs)


S = 4096
D = 2048
NB = 8
OFF_Q, OFF_KV, OFF_G, OFF_U, OFF_V, OFF_GA, OFF_GB = 0, 2048, 3584, 3632, 5680, 7728, 9776
ALPHA = 2.0 ** 0.25
EPS = 1e-5
CAP = 64


def _consts(j):
    c = {}
    c["ident"] = np.eye(128, dtype=np.float32)
    c["perm"] = np.roll(np.eye(128, dtype=np.float32), 64, axis=0)
    inv = (10000.0 ** (-np.arange(0, 128, 2, dtype=np.float32) / 128)).astype(np.float32)
    inv2 = np.concatenate([inv, inv])
    sgn = np.concatenate([-np.ones(64, np.float32), np.ones(64, np.float32)])

    def tab(pos, scale=1.0):
        ang = pos.astype(np.float32)[None, :] * inv2[:, None]
        return ((np.cos(ang) * scale).astype(np.float32),
                (np.sin(ang) * sgn[:, None] * scale).astype(np.float32))

    c["cosK"], c["sinK"] = tab(np.arange(S))
    own_pos = np.concatenate([128 * (4 * i + j) + np.arange(128) for i in range(NB)])
    c["cosQ"], c["sinQ"] = tab(own_pos, 128.0 ** -0.5)
    c["cosC"], c["sinC"] = tab(16 * np.arange(256) + 31)
    p = np.arange(128)[:, None]
    tl = np.arange(128)[None, :]
    caus = (p <= tl).astype(np.float32)
    dm = np.zeros((128, 4, 128), np.float32)
    for r in range(4):
        dm[:, r, :] = 1.0 if r < j else (caus if r == j else 0.0)
    c["dmask"] = dm
    wm = np.zeros((128, 8, 128), np.float32)
    for r in range(8):
        rel = r - 4 - j
        if rel == 0:
            wm[:, r, :] = caus
        elif rel in (-1, -2, -3):
            wm[:, r, :] = 1.0
        elif rel == -4:
            wm[:, r, :] = 1.0 - caus
    c["wmask"] = wm
    cm = np.zeros((128, NB, 2, 128), np.float32)
    sA = np.zeros((128, NB, 64), np.float32)
    sB = np.zeros((128, NB, 64), np.float32)
    jj = np.arange(64)[None, :]
    for i in range(NB):
        cb = 4 * i + j
        t = 128 * cb + np.arange(128)
        for ch in range(2):
            n = ch * 128 + np.arange(128)
            cm[:, i, ch, :] = ((16 * n[:, None] + 31 <= t[None, :]) & (n[:, None] < 255)).astype(np.float32)
        cur = (t // 64)[:, None]
        vis = jj <= cur
        f0 = jj == 0
        f1 = jj == cur
        f2 = jj == cur - 1
        forced = f0 | f1 | f2
        sA[:, i, :] = (vis & ~forced).astype(np.float32)
        b = np.where(vis, 0.0, -1e9)
        b = np.where(f2, 1e9, b)
        b = np.where(f1, 2e9, b)
        b = np.where(f0, 3e9, b)
        sB[:, i, :] = b
    c["cmask"], c["selA"], c["selB"] = cm, sA, sB
    n = np.arange(256)[:, None]
    ov = np.clip(np.minimum(16 * n + 32, 64 * jj + 64) - np.maximum(16 * n, 64 * jj), 0, None) / 32.0
    ov[255] = 0.0
    c["selmap"] = np.ascontiguousarray(ov.reshape(2, 128, 64).transpose(1, 0, 2)).astype(np.float32)
    ex = np.zeros((64, 32, 128), np.float32)
    for kt in range(32):
        ex[2 * kt, kt, :64] = 1.0
        ex[2 * kt + 1, kt, 64:] = 1.0
    c["exall"] = ex
    c["tri"] = caus
    c["ustrict"] = (p < tl).astype(np.float32)
    c["iota_c"] = np.tile(np.arange(64, dtype=np.float32)[None, :], (128, 1))
    c["iota_pb"] = np.tile(np.arange(64, dtype=np.float32)[:, None], (1, 64))
    return c


CONST_SHAPES = {k: v.shape for k, v in _consts(0).items()}


def build(dbg=()):
    nc = bass.Bass("TRN2", target_bir_lowering=False)
    fw = FW(nc)
    dbg_out = {}

    def din(name, shape):
        return fw.dram(name, shape, F32, kind="ExternalInput")

    x_d = din("x", [S, D])
    xo_d = din("x_own", [NB * 128, D])
    w_in = din("w_in", [D, 11824])
    wq_rot = din("wq_rot", [D, 2048])
    wk_rot = din("wk_rot", [D, 512])
    pe_kT = din("pe_kT", [128, 32])
    pe_vT = din("pe_vT", [128, 32])
    w1k_d = din("cmp_w1_k", [4096, 256])
    w1v_d = din("cmp_w1_v", [4096, 256])
    w2k_d = din("cmp_w2_k", [256, 128])
    w2kr_d = din("cmp_w2_k_rot", [256, 128])
    w2v_d = din("cmp_w2_v", [256, 128])
    sgu_wsT = din("sgu_wsT", [8, 128, 128])
    sgu_g_fm = din("sgu_g_fm", [128, 16])
    sgu_b_fm = din("sgu_b_fm", [128, 16])
    sgu_bs = din("sgu_bs", [1, 1024])
    w_br_a = din("w_branch_a", [D, D])
    w_br_b = din("w_branch_b", [D, D])
    w_o_d = din("w_o", [D, D])
    ln1_g = din("ln1_g", [1, D])
    ln1_b = din("ln1_b", [1, D])
    mem_d = din("mem", [256, D])
    w_xq = din("w_xq", [D, 512])
    w_xk = din("w_xk", [D, 512])
    w_xv = din("w_xv", [D, 512])
    w_xo = din("w_xo", [512, D])
    ln2_g = din("ln2_g", [1, D])
    ln2_b = din("ln2_b", [1, D])
    w_rt = din("w_router", [D, 68])
    b_rt = din("b_router", [1, 68])
    w_eg = din("w_exp_gate", [64, D, 512])
    w_eu = din("w_exp_up", [64, D, 512])
    w_ed = din("w_exp_down", [64, 512, D])
    ln3_g = din("ln3_g", [1, D])
    ln3_b = din("ln3_b", [1, D])
    C = {k: din("c_" + k, list(s)) for k, s in CONST_SHAPES.items()}
    out_d = fw.dram("out", [NB * 128, D], F32, kind="ExternalOutput")

    kT_d = fw.dram("kT_scr", [4, 128, S], BF16)
    v_d = fw.dram("v_scr", [S, 512], BF16)

    def r3(buf):
        return buf.t.ap().rearrange("(kc p) n -> p kc n", p=128)

    PS = fw.nc.alloc_psum_tensor("psum", [128, 8, 512], F32)
    banks = [Buf(PS, f"bank{i}") for i in range(8)]

    def bk(i):
        return PS[:, i, :]

    def bkbf(i):
        return PS[:, i, :].bitcast(BF16)

    rr = [0]

    def nextbank():
        i = rr[0] % 8
        rr[0] += 1
        return i

    epsc = fw.sb([128, 1], F32, "epsc")
    fw.op("dve", lambda e: e.memset(epsc[:], EPS), writes=[epsc])
    ident = fw.sb([128, 128], BF16, "ident")
    fw.dma("pool", ident[:], C["ident"][:], reads=[C["ident"]], writes=[ident])

    def mm(ps_ap, lhsT, rhs, start, stop, R, W, inc=None, sgc=False):
        if inc is None:
            inc = stop
        fw.op("pe", lambda e: e.matmul(ps_ap, lhsT, rhs, start=start, stop=stop, skip_group_check=sgc),
              reads=R, writes=W, inc=inc)

    def load_xT(src_d, row0, xb, dstT, col0, evac):
        fw.dma("pool", xb[:], src_d[row0:row0 + 128, :], reads=[src_d], writes=[xb])
        for half in range(2):
            b = nextbank()
            for k8 in range(8):
                kc = half * 8 + k8
                fw.op("pe", lambda e: e.transpose(bkbf(b)[:, k8 * 128:(k8 + 1) * 128],
                                                  xb[:, kc * 128:(kc + 1) * 128], ident[:]),
                      reads=[xb, ident], writes=[banks[b]], inc=(k8 == 7))
            src = bkbf(b).rearrange("p (k t) -> p k t", k=8)
            dst = dstT[:, half * 8:(half + 1) * 8, col0:col0 + 128]
            if evac == "act":
                fw.op("act", lambda e: e.copy(dst, src), reads=[banks[b]], writes=[dstT])
            else:
                fw.op("dve", lambda e: e.tensor_copy(dst, src), reads=[banks[b]], writes=[dstT])

    mark0 = nc.sbuf_base
    cmpraw = fw.sb([128, 4, S], BF16, "cmpraw")
    mark_dbg = nc.sbuf_base
    wkv = fw.sb([128, 16, 1536], BF16, "wkv")
    wkr = fw.sb([128, 16, 512], BF16, "wkr")
    w_in_r = r3(w_in)
    for c3 in range(3):
        fw.dma("pool", wkv[:, :, c3 * 512:(c3 + 1) * 512], w_in_r[:, :, OFF_KV + c3 * 512:OFF_KV + (c3 + 1) * 512],
               reads=[w_in], writes=[wkv])
    fw.dma("pool", wkr[:], r3(wk_rot), reads=[wk_rot], writes=[wkr])
    xbs = [fw.sb([128, D], BF16, f"xb{i}") for i in range(3)]
    xTs = [fw.sb([128, 16, 512], BF16, f"xT{i}") for i in range(2)]
    cos_t = [fw.sb([128, 512], F32, f"cos{i}") for i in range(2)]
    sin_t = [fw.sb([128, 512], F32, f"sin{i}") for i in range(2)]
    kst = [fw.sb([128, 4, 512], BF16, f"kst{i}") for i in range(2)]
    vst = [fw.sb([128, 4, 512], BF16, f"vst{i}") for i in range(2)]
    ropa = [fw.sb([128, 512], F32, f"ropa{i}") for i in range(2)]
    ropb = [fw.sb([128, 512], F32, f"ropb{i}") for i in range(2)]
    kT_r = kT_d.t.ap().rearrange("k d t -> d k t")

    def rope(ps_t, ps_r, cosb, sinb, cos_ap, sin_ap, out_ap, outbuf, n, idx):
        a, b2 = ropa[idx % 2], ropb[idx % 2]
        fw.op("dve", lambda e: e.tensor_tensor(a[:, 0:n], bk(ps_t)[:, 0:n], cos_ap, ALU.mult),
              reads=[banks[ps_t], cosb], writes=[a])
        fw.op("dve", lambda e: e.tensor_tensor(b2[:, 0:n], bk(ps_r)[:, 0:n], sin_ap, ALU.mult),
              reads=[banks[ps_r], sinb], writes=[b2])
        fw.op("dve", lambda e: e.tensor_tensor(out_ap, a[:, 0:n], b2[:, 0:n], ALU.add),
              reads=[a, b2], writes=[outbuf])

    ridx = [0]
    for tile in range(8):
        xT = xTs[tile % 2]
        ct, st = cos_t[tile % 2], sin_t[tile % 2]
        fw.dma("sp", ct[:], C["cosK"][:, tile * 512:(tile + 1) * 512], reads=[C["cosK"]], writes=[ct])
        fw.dma("sp", st[:], C["sinK"][:, tile * 512:(tile + 1) * 512], reads=[C["sinK"]], writes=[st])
        for blk in range(4):
            g = tile * 4 + blk
            load_xT(x_d, g * 128, xbs[g % 3], xT, blk * 128, "act" if blk % 2 == 0 else "dve")
        for f in range(4):
            b = nextbank()
            for kc in range(16):
                mm(bk(b), wkv[:, kc, f * 128:(f + 1) * 128], xT[:, kc, :], kc == 0, kc == 15, [wkv, xT], [banks[b]])
            fw.op("act", lambda e: e.copy(cmpraw[:, f, tile * 512:(tile + 1) * 512], bk(b)),
                  reads=[banks[b]], writes=[cmpraw])
        ks = kst[tile % 2]
        for kk in range(4):
            col = (512 if kk < 2 else 1024) + (kk % 2) * 128
            b1 = nextbank()
            for kc in range(16):
                mm(bk(b1), wkv[:, kc, col:col + 128], xT[:, kc, :], kc == 0, kc == 15, [wkv, xT], [banks[b1]])
            b2 = nextbank()
            for kc in range(16):
                mm(bk(b2), wkr[:, kc, kk * 128:(kk + 1) * 128], xT[:, kc, :], kc == 0, kc == 15, [wkr, xT], [banks[b2]])
            rope(b1, b2, ct, st, ct[:], st[:], ks[:, kk, :], ks, 512, ridx[0])
            ridx[0] += 1
        fw.dma("sp", kT_r[:, :, tile * 512:(tile + 1) * 512], ks[:], reads=[ks], writes=[kT_d])
        vs = vst[tile % 2]
        for blk in range(4):
            b = nextbank()
            for half, col in enumerate((768, 1280)):
                for kc in range(16):
                    mm(bk(b)[:, half * 256:(half + 1) * 256], xT[:, kc, blk * 128:(blk + 1) * 128],
                       wkv[:, kc, col:col + 256], kc == 0, kc == 15, [wkv, xT], [banks[b]],
                       inc=(kc == 15 and half == 1))
            fw.op("dve", lambda e: e.tensor_copy(vs[:, blk, :], bk(b)), reads=[banks[b]], writes=[vs])
        fw.dma("sp", v_d.t.ap()[tile * 512:(tile + 1) * 512, :].rearrange("(b p) f -> p b f", p=128), vs[:],
               reads=[vs], writes=[v_d])

    if "A" in dbg:
        dbg_out["cmpraw"] = fw.dram("dbg_cmpraw", [128, 4, S], BF16, kind="ExternalOutput")
        fw.dma("sp", dbg_out["cmpraw"].t.ap(), cmpraw[:], reads=[cmpraw], writes=[dbg_out["cmpraw"]])
        dbg_out["kT"] = fw.dram("dbg_kT", [4, 128, S], BF16, kind="ExternalOutput")
        dbg_out["v"] = fw.dram("dbg_v", [S, 512], BF16, kind="ExternalOutput")
        fw.barrier()
        nc.sbuf_base = mark_dbg
        tmpk = fw.sb([128, 4, S], BF16, "tmpk")
        fw.dma("sp", tmpk[:], kT_r, reads=[kT_d], writes=[tmpk])
        fw.dma("sp", dbg_out["kT"].t.ap().rearrange("k d t -> d k t"), tmpk[:], reads=[tmpk], writes=[dbg_out["kT"]])
        tmpv = fw.sb([128, 32, 512], BF16, "tmpv")
        fw.dma("sp", tmpv[:], v_d.t.ap().rearrange("(b p) f -> p b f", p=128), reads=[v_d], writes=[tmpv])
        fw.dma("sp", dbg_out["v"].t.ap().rearrange("(b p) f -> p b f", p=128), tmpv[:], reads=[tmpv], writes=[dbg_out["v"]])
        fw.finish(list(dbg_out.values()))
        return nc, fw


    fw.barrier()
    nc.sbuf_base = mark_dbg
    qT = fw.sb([128, NB, 16, 128], BF16, "qT")
    gates = fw.sb([128, NB, 48], F32, "gates")
    kcmpT = fw.sb([128, 2, 256], BF16, "kcmpT")
    vcmp = fw.sb([128, 2, 2, 129], BF16, "vcmp")
    mark2 = nc.sbuf_base
    xTo = fw.sb([128, 16, NB * 128], BF16, "xTo")
    xbo = [fw.sb([128, D], BF16, f"xbo{i}") for i in range(2)]
    for blk in range(NB):
        load_xT(xo_d, blk * 128, xbo[blk % 2], xTo, blk * 128, "act" if blk % 2 == 0 else "dve")
    wg = fw.sb([128, 16, 48], BF16, "wg")
    fw.dma("pool", wg[:], w_in_r[:, :, OFF_G:OFF_G + 48], reads=[w_in], writes=[wg])
    for blk in range(NB):
        b = nextbank()
        for kc in range(16):
            mm(bk(b)[:, 0:48], xTo[:, kc, blk * 128:(blk + 1) * 128], wg[:, kc, :], kc == 0, kc == 15, [xTo, wg], [banks[b]])
        fw.op("act", lambda e: e.activation(gates[:, blk, :], bk(b)[:, 0:48], AF.Sigmoid), reads=[banks[b]], writes=[gates])
    cosq = fw.sb([128, NB * 128], F32, "cosq")
    sinq = fw.sb([128, NB * 128], F32, "sinq")
    fw.dma("sp", cosq[:], C["cosQ"][:], reads=[C["cosQ"]], writes=[cosq])
    fw.dma("sp", sinq[:], C["sinQ"][:], reads=[C["sinQ"]], writes=[sinq])
    wqb = [fw.sb([128, 16, 512], BF16, f"wq{i}") for i in range(2)]
    wqrb = [fw.sb([128, 16, 512], BF16, f"wqr{i}") for i in range(2)]
    ropa2 = [fw.sb([128, 512], F32, f"ropa2{i}") for i in range(2)]
    ropb2 = [fw.sb([128, 512], F32, f"ropb2{i}") for i in range(2)]
    wqr_r = r3(wq_rot)
    ri = 0
    for hg in range(4):
        wq, wqr = wqb[hg % 2], wqrb[hg % 2]
        fw.dma("pool", wq[:], w_in_r[:, :, OFF_Q + hg * 512:OFF_Q + (hg + 1) * 512], reads=[w_in], writes=[wq])
        fw.dma("pool", wqr[:], wqr_r[:, :, hg * 512:(hg + 1) * 512], reads=[wq_rot], writes=[wqr])
        for half in range(2):
            for hh in range(4):
                b1 = nextbank()
                for kc in range(16):
                    mm(bk(b1), wq[:, kc, hh * 128:(hh + 1) * 128], xTo[:, kc, half * 512:(half + 1) * 512], kc == 0, kc == 15, [wq, xTo], [banks[b1]])
                b2 = nextbank()
                for kc in range(16):
                    mm(bk(b2), wqr[:, kc, hh * 128:(hh + 1) * 128], xTo[:, kc, half * 512:(half + 1) * 512], kc == 0, kc == 15, [wqr, xTo], [banks[b2]])
                a, bb = ropa2[ri % 2], ropb2[ri % 2]
                ri += 1
                fw.op("dve", lambda e: e.tensor_tensor(a[:], bk(b1), cosq[:, half * 512:(half + 1) * 512], ALU.mult), reads=[banks[b1], cosq], writes=[a])
                fw.op("dve", lambda e: e.tensor_tensor(bb[:], bk(b2), sinq[:, half * 512:(half + 1) * 512], ALU.mult), reads=[banks[b2], sinq], writes=[bb])
                fw.op("dve", lambda e: e.tensor_tensor(qT[:, half * 4:(half + 1) * 4, hg * 4 + hh, :],
                                                        a[:].rearrange("p (b t) -> p b t", b=4),
                                                        bb[:].rearrange("p (b t) -> p b t", b=4), ALU.add),
                      reads=[a, bb], writes=[qT])

    fw.barrier()
    nc.sbuf_base = mark2
    w1 = [fw.sb([128, 32, 256], BF16, f"w1{i}") for i in range(2)]
    peT = [fw.sb([128, 32], BF16, f"peT{i}") for i in range(2)]
    for kv, (wd, pd) in enumerate(((w1k_d, pe_kT), (w1v_d, pe_vT))):
        fw.dma("pool", w1[kv][:], wd.t.ap().rearrange("(j d) h -> d j h", d=128), reads=[wd], writes=[w1[kv]])
        fw.dma("pool", peT[kv][:], pd[:], reads=[pd], writes=[peT[kv]])
    w2s = []
    for wd in (w2k_d, w2kr_d, w2v_d):
        t = fw.sb([128, 2, 128], BF16, "w2")
        fw.dma("pool", t[:], wd.t.ap().rearrange("(hc h) d -> h hc d", h=128), reads=[wd], writes=[t])
        w2s.append(t)
    w2k, w2kr, w2v = w2s
    cosc = fw.sb([128, 256], F32, "cosc")
    sinc = fw.sb([128, 256], F32, "sinc")
    fw.dma("sp", cosc[:], C["cosC"][:], reads=[C["cosC"]], writes=[cosc])
    fw.dma("sp", sinc[:], C["sinC"][:], reads=[C["sinC"]], writes=[sinc])
    biasS = fw.sb([128, 4], F32, "biasS")
    for kv in range(2):
        for hc in range(2):
            b = nextbank()
            for jx in range(32):
                mm(bk(b)[:, 0:1], w1[kv][:, jx, hc * 128:(hc + 1) * 128], peT[kv][:, jx:jx + 1], jx == 0, jx == 31, [w1[kv], peT[kv]], [banks[b]])
            fw.op("act", lambda e: e.copy(biasS[:, kv * 2 + hc:kv * 2 + hc + 1], bk(b)[:, 0:1]), reads=[banks[b]], writes=[biasS])
    fw.op("dve", lambda e: e.memset(kcmpT[:], 0.0), writes=[kcmpT])
    fw.op("dve", lambda e: e.memset(vcmp[:], 0.0), writes=[vcmp])
    fw.op("dve", lambda e: e.memset(vcmp[:, :, :, 128:129], 1.0), writes=[vcmp])
    hidTs = [fw.sb([128, 2, 256], BF16, f"hidT{i}") for i in range(2)]
    ropc = [fw.sb([128, 256], F32, f"ropc{i}") for i in range(2)]
    for kv in range(2):
        for g in range(2):
            hidT = hidTs[(kv * 2 + g) % 2]
            for hc in range(2):
                b = nextbank()
                for jx in range(32):
                    mm(bk(b)[:, 0:255], w1[kv][:, jx, hc * 128:(hc + 1) * 128], cmpraw[:, kv * 2 + g, jx:jx + 16 * 254 + 1:16],
                       jx == 0, jx == 31, [w1[kv], cmpraw], [banks[b]])
                fw.op("act", lambda e: e.activation(hidT[:, hc, 0:255], bk(b)[:, 0:255], AF.Gelu_apprx_tanh,
                                                    bias=biasS[:, kv * 2 + hc:kv * 2 + hc + 1]),
                      reads=[banks[b], biasS], writes=[hidT])
            if kv == 0:
                b1 = nextbank()
                for hc in range(2):
                    mm(bk(b1)[:, 0:255], w2k[:, hc, :], hidT[:, hc, 0:255], hc == 0, hc == 1, [w2k, hidT], [banks[b1]])
                b2 = nextbank()
                for hc in range(2):
                    mm(bk(b2)[:, 0:255], w2kr[:, hc, :], hidT[:, hc, 0:255], hc == 0, hc == 1, [w2kr, hidT], [banks[b2]])
                a, bb = ropc[0], ropc[1]
                fw.op("dve", lambda e: e.tensor_tensor(a[:, 0:255], bk(b1)[:, 0:255], cosc[:, 0:255], ALU.mult), reads=[banks[b1], cosc], writes=[a])
                fw.op("dve", lambda e: e.tensor_tensor(bb[:, 0:255], bk(b2)[:, 0:255], sinc[:, 0:255], ALU.mult), reads=[banks[b2], sinc], writes=[bb])
                fw.op("dve", lambda e: e.tensor_tensor(kcmpT[:, g, 0:255], a[:, 0:255], bb[:, 0:255], ALU.add), reads=[a, bb], writes=[kcmpT])
            else:
                for ch in range(2):
                    nn = 128 if ch == 0 else 127
                    b = nextbank()
                    for hc in range(2):
                        mm(bk(b)[0:nn, 0:128], hidT[:, hc, ch * 128:ch * 128 + nn], w2v[:, hc, :], hc == 0, hc == 1, [hidT, w2v], [banks[b]])
                    fw.op("act", lambda e: e.copy(vcmp[0:nn, ch, g, 0:128], bk(b)[0:nn, 0:128]), reads=[banks[b]], writes=[vcmp])

    if "B" in dbg:
        for nm, buf, shp, dt_ in (("qT", qT, [128, NB, 16, 128], BF16), ("gates", gates, [128, NB, 48], F32),
                                  ("kcmpT", kcmpT, [128, 2, 256], BF16), ("vcmp", vcmp, [128, 2, 2, 129], BF16)):
            dbg_out[nm] = fw.dram("dbg_" + nm, shp, dt_, kind="ExternalOutput")
            fw.dma("sp", dbg_out[nm].t.ap(), buf[:], reads=[buf], writes=[dbg_out[nm]])
        fw.finish(list(dbg_out.values()))
        return nc, fw


    fw.barrier()
    nc.sbuf_base = mark2
    o_nsaT = fw.sb([128, 16, NB * 128], BF16, "o_nsaT", side="right")

    def cload(name, shape, dt_, q="pool"):
        t = fw.sb(shape, dt_, name)
        fw.dma(q, t[:], C[name].t.ap(), reads=[C[name]], writes=[t])
        return t

    exall = cload("exall", [64, 32, 128], BF16)
    dmask = cload("dmask", [128, 4, 128], BF16)
    wmask = cload("wmask", [128, 8, 128], BF16)
    cmask = cload("cmask", [128, NB, 2, 128], BF16)
    selmap = cload("selmap", [128, 2, 64], BF16)
    selA = cload("selA", [128, NB, 64], F32, "sp")
    selB = cload("selB", [128, NB, 64], F32, "sp")
    ksl = [fw.sb([128, S], BF16, f"ksl{i}") for i in range(2)]
    vsl = [fw.sb([128, 32, 129], BF16, f"vsl{i}") for i in range(2)]
    kwn = [fw.sb([128, 1024], BF16, f"kwn{i}") for i in range(2)]
    vwn = [fw.sb([128, 8, 129], BF16, f"vwn{i}") for i in range(2)]
    for t in vsl + vwn:
        fw.op("dve", lambda e: e.memset(t[:, :, 128:129], 1.0), writes=[t])
    eTs = [fw.sb([128, 512], BF16, f"eT{i}") for i in range(4)]
    pTs = [fw.sb([128, 4, 128], BF16, f"pT{i}") for i in range(8)]
    mks = [fw.sb([128, 128], BF16, f"mk{i}") for i in range(2)]
    o_out = fw.sb([128, D], F32, "o_out")
    o_bf = fw.sb([128, D], BF16, "o_bf")
    rinv = fw.sb([128, 8], F32, "rinv")
    wgt = fw.sb([128, 8], F32, "wgt")
    imp = fw.sb([128, 64], F32, "imp")
    score = fw.sb([128, 64], F32, "score")
    tmpm = fw.sb([128, 64], F32, "tmpm")
    mx = fw.sb([128, 16], F32, "mx")
    sel_bf = fw.sb([128, 64], BF16, "sel_bf")
    selT = fw.sb([64, 128], BF16, "selT")
    MB = 4
    OB = (5, 6, 7)
    obufs = [banks[5], banks[6], banks[7]]
    only_br = None
    for d_ in dbg:
        if d_.startswith("C") and len(d_) == 2:
            only_br = int(d_[1])

    def oacc(h, lo=0, hi=129):
        return PS[:, 5 + h // 3, (h % 3) * 129 + lo:(h % 3) * 129 + hi]

    cnt = {"e": 0, "p": 0, "s": 0, "m": 0}

    NS = 4

    def scores(kT_ap, qblk, Rk, hf):
        b = cnt["s"] % NS
        cnt["s"] += 1
        mm(bk(b), kT_ap, qblk[:, hf * 4:(hf + 1) * 4, :], True, True, Rk + [qT], [banks[b]])
        eT = eTs[cnt["e"] % len(eTs)]
        cnt["e"] += 1
        fw.op("act", lambda e: e.activation(eT[:], bk(b), AF.Exp), reads=[banks[b]], writes=[eT])
        return eT

    def masked(eT, mask_ap, Rm):
        pT = pTs[cnt["p"] % len(pTs)]
        cnt["p"] += 1
        fw.op("dve", lambda e: e.tensor_tensor(pT[:], eT[:].rearrange("p (h t) -> p h t", h=4),
                                               mask_ap.unsqueeze(1).to_broadcast([128, 4, 128]), ALU.mult),
              reads=[eT] + Rm, writes=[pT])
        return pT

    def pv(pT, v_ap, Rv, first, last, hf):
        for h4 in range(4):
            h = hf * 4 + h4
            mm(oacc(h), pT[:, h4, :], v_ap, first and h % 3 == 0, last, [pT] + Rv, [obufs[h // 3]],
               inc=(h4 == 3), sgc=True)

    ocp = [fw.sb([128, 3, 387], F32, f"ocp{i}") for i in range(2)]
    pimp_sb = fw.sb([128, 512], F32, "pimp_sb")
    maskT = fw.sb([128, 32, 128], BF16, "maskT")
    ptmp = [fw.sb([128, 128], F32, f"ptmp{i}") for i in range(2)]
    ocnt = [0]

    def grab():
        oc = ocp[ocnt[0] % 2]
        ocnt[0] += 1
        for bnk in range(3):
            nh = 3 if bnk < 2 else 2
            if bnk == 1:
                fw.op("act", lambda e: e.copy(oc[:, bnk, 0:nh * 129], PS[:, 5 + bnk, 0:nh * 129]), reads=[obufs[bnk]], writes=[oc])
            else:
                fw.op("dve", lambda e: e.tensor_copy(oc[:, bnk, 0:nh * 129], PS[:, 5 + bnk, 0:nh * 129]), reads=[obufs[bnk]], writes=[oc])
        return oc

    def fin_ops(i, g, br, oc):
        ops = []
        ocv = oc[:].rearrange("p b (h c) -> p b h c", c=129)

        def och(h, lo, hi):
            return oc[:, h // 3, (h % 3) * 129 + lo:(h % 3) * 129 + hi]

        def f_rs():
            for bnk in range(3):
                nh = 3 if bnk < 2 else 2
                fw.op("dve", lambda e: e.tensor_scalar(rinv[:, bnk * 3:bnk * 3 + nh], ocv[:, bnk, 0:nh, 128], 1e-30, None, ALU.max),
                      reads=[oc], writes=[rinv])
            fw.op("dve", lambda e: e.reciprocal(rinv[:], rinv[:]), reads=[rinv], writes=[rinv])
            if only_br is None:
                gv = gates[:, i, g * 24 + br:g * 24 + 24:3]
                fw.op("dve", lambda e: e.tensor_tensor(wgt[:], rinv[:], gv, ALU.mult), reads=[rinv, gates], writes=[wgt])
            elif only_br == br:
                fw.op("dve", lambda e: e.tensor_copy(wgt[:], rinv[:]), reads=[rinv], writes=[wgt])
            else:
                fw.op("dve", lambda e: e.memset(wgt[:], 0.0), writes=[wgt])
        ops.append(f_rs)
        for h in range(8):
            def f_h(h=h):
                dst = o_out[:, (g * 8 + h) * 128:(g * 8 + h + 1) * 128]
                if br == 0:
                    fw.op("dve", lambda e: e.tensor_scalar(dst, och(h, 0, 128), wgt[:, h:h + 1], None, ALU.mult),
                          reads=[oc, wgt], writes=[o_out])
                else:
                    fw.op("dve", lambda e: e.scalar_tensor_tensor(dst, och(h, 0, 128), wgt[:, h:h + 1], dst, ALU.mult, ALU.add),
                          reads=[oc, wgt, o_out], writes=[o_out])
            ops.append(f_h)
        return ops

    def topk_ops(i):
        ops = []

        def f0():
            fw.op("act", lambda e: e.copy(pimp_sb[:], bk(MB)), reads=[banks[MB]], writes=[pimp_sb])
        ops.append(f0)
        for h in range(8):
            def f(h=h):
                src = pimp_sb[:, h * 64:(h + 1) * 64]
                if h == 0:
                    fw.op("dve", lambda e: e.tensor_scalar(imp[:], src, rinv[:, 0:1], None, ALU.mult), reads=[pimp_sb, rinv], writes=[imp])
                else:
                    fw.op("dve", lambda e: e.scalar_tensor_tensor(imp[:], src, rinv[:, h:h + 1], imp[:], ALU.mult, ALU.add),
                          reads=[pimp_sb, rinv, imp], writes=[imp])
            ops.append(f)

        def f1():
            fw.op("dve", lambda e: e.tensor_tensor(score[:], imp[:], selA[:, i, :], ALU.mult), reads=[imp, selA], writes=[score])
            fw.op("dve", lambda e: e.tensor_tensor(score[:], score[:], selB[:, i, :], ALU.add), reads=[score, selB], writes=[score])
            fw.op("dve", lambda e: e.max(out=mx[:, 0:8], in_=score[:]), reads=[score], writes=[mx])
        ops.append(f1)

        def f2():
            fw.op("dve", lambda e: e.match_replace(out=tmpm[:], in_to_replace=mx[:, 0:8], in_values=score[:], imm_value=-1e30),
                  reads=[score, mx], writes=[tmpm])
            fw.op("dve", lambda e: e.max(out=mx[:, 8:16], in_=tmpm[:]), reads=[tmpm], writes=[mx])
            fw.op("dve", lambda e: e.tensor_scalar(sel_bf[:], score[:], mx[:, 15:16], None, ALU.is_ge), reads=[score, mx], writes=[sel_bf])
        ops.append(f2)

        def f3():
            fw.op("pe", lambda e: e.transpose(bkbf(MB)[0:64, 0:128], sel_bf[:], ident[:]), reads=[sel_bf, ident], writes=[banks[MB]])
            fw.op("act", lambda e: e.copy(selT[:], bkbf(MB)[0:64, 0:128]), reads=[banks[MB]], writes=[selT])
        ops.append(f3)
        return ops

    pending = []

    def drain(n):
        for _ in range(min(n, len(pending))):
            pending.pop(0)()

    SKEW = 3

    def run_tiles(fronts, backs, per_tile=2):
        live = []
        for k in range(len(fronts)):
            live.append(fronts[k]())
            if k >= SKEW:
                backs[k - SKEW](live[k - SKEW])
            drain(per_tile)
        for k in range(max(len(fronts) - SKEW, 0), len(fronts)):
            backs[k](live[k])

    kT_all = kT_d.t.ap()
    v_all = v_d.t.ap()

    def kv_loads(i, g):
        it = i * 2 + g
        nsl = 4 * i + 4
        w0 = max(4 * i - 4, 0)
        nw = 4 * i + 4 - w0
        ks, vs, kw, vw = ksl[it % 2], vsl[it % 2], kwn[it % 2], vwn[it % 2]
        fw.dma("sp", ks[:, 0:nsl * 128], kT_all[g, :, 0:nsl * 128], reads=[kT_d], writes=[ks])
        fw.dma("sp", vs[:, 0:nsl, 0:128], v_all[0:nsl * 128, g * 128:(g + 1) * 128].rearrange("(t p) d -> p t d", p=128),
               reads=[v_d], writes=[vs])
        fw.dma("sp", kw[:, 0:nw * 128], kT_all[2 + g, :, w0 * 128:(w0 + nw) * 128], reads=[kT_d], writes=[kw])
        fw.dma("sp", vw[:, 0:nw, 0:128],
               v_all[w0 * 128:(w0 + nw) * 128, 256 + g * 128:256 + (g + 1) * 128].rearrange("(t p) d -> p t d", p=128),
               reads=[v_d], writes=[vw])

    def block_out(i):
        def f():
            if "C" in dbg or only_br is not None:
                if "o" not in dbg_out:
                    dbg_out["o"] = fw.dram("dbg_o", [NB, 128, D], F32, kind="ExternalOutput")
                fw.dma("sp", dbg_out["o"].t.ap()[i], o_out[:], reads=[o_out], writes=[dbg_out["o"]])
            fw.op("act", lambda e: e.copy(o_bf[:], o_out[:]), reads=[o_out], writes=[o_bf])
            for half in range(2):
                b = MB
                for k8 in range(8):
                    kc = half * 8 + k8
                    fw.op("pe", lambda e: e.transpose(bkbf(b)[:, k8 * 128:(k8 + 1) * 128], o_bf[:, kc * 128:(kc + 1) * 128], ident[:]),
                          reads=[o_bf, ident], writes=[banks[b]], inc=(k8 == 7))
                fw.op("act", lambda e: e.copy(o_nsaT[:, half * 8:(half + 1) * 8, i * 128:(i + 1) * 128],
                                              bkbf(b).rearrange("p (k t) -> p k t", k=8)),
                      reads=[banks[b]], writes=[o_nsaT])
        return f

    units = []

    def add_iter(i, g):
        it = i * 2 + g
        nsl = 4 * i + 4
        w0 = max(4 * i - 4, 0)
        nw = 4 * i + 4 - w0
        ks, vs, kw, vw = ksl[it % 2], vsl[it % 2], kwn[it % 2], vwn[it % 2]
        qblk = qT[:, i, g * 8:(g + 1) * 8, :]
        nch = 1 if i < 4 else 2
        pcs = {}
        first = len(units)

        def c_front(ch, hf):
            eT = scores(kcmpT[:, g, ch * 128:(ch + 1) * 128], qblk, [kcmpT], hf)
            p_ = masked(eT, cmask[:, i, ch, :], [cmask])
            pcs[(ch, hf)] = p_
            return p_

        def c_post():
            drain(len(pending))
            for h in range(8):
                for ch in range(nch):
                    mm(bk(MB)[:, h * 64:(h + 1) * 64], pcs[(ch, h // 4)][:, h % 4, :], selmap[:, ch, :], ch == 0 and h == 0, ch == nch - 1,
                       [pcs[(ch, h // 4)], selmap], [banks[MB]], inc=(h == 7 and ch == nch - 1) or None, sgc=True)
            drain(len(pending))
            oc = grab()
            pending.extend(fin_ops(i, g, 0, oc))
            pending.extend(topk_ops(i))
            for q4 in range(i + 1):
                def f_mask(q4=q4):
                    for k4 in range(4):
                        mm(bk(MB)[:, k4 * 128:(k4 + 1) * 128], exall[:, q4 * 4 + k4, :], selT[:], True, True, [exall, selT], [banks[MB]],
                           inc=(k4 == 3))
                    src = bk(MB).rearrange("p (k t) -> p k t", k=4)
                    if q4 == i:
                        fw.op("dve", lambda e: e.tensor_tensor(maskT[:, q4 * 4:(q4 + 1) * 4, :], src, dmask[:], ALU.mult),
                              reads=[banks[MB], dmask], writes=[maskT])
                    else:
                        fw.op("act", lambda e: e.copy(maskT[:, q4 * 4:(q4 + 1) * 4, :], src), reads=[banks[MB]], writes=[maskT])
                pending.append(f_mask)

        cu = [(ch, hf) for ch in range(nch) for hf in range(2)]
        for n_, (ch, hf) in enumerate(cu):
            units.append(dict(pre=None, front=(lambda ch=ch, hf=hf: c_front(ch, hf)),
                              back=(lambda p_, ch=ch, hf=hf: pv(p_, vcmp[:, ch, g, :], [vcmp], ch == 0, ch == nch - 1, hf)),
                              post=c_post if n_ == len(cu) - 1 else None))

        def w_front(r_, hf):
            eT = scores(kw[:, r_ * 128:(r_ + 1) * 128], qblk, [kw], hf)
            return masked(eT, wmask[:, w0 + r_ - (4 * i - 4), :], [wmask])

        def w_post():
            drain(len(pending))
            oc = grab()
            pending.extend(fin_ops(i, g, 2, oc))

        wu = [(r_, hf) for r_ in range(nw) for hf in range(2)]
        for n_, (r_, hf) in enumerate(wu):
            units.append(dict(pre=None, front=(lambda r_=r_, hf=hf: w_front(r_, hf)),
                              back=(lambda p_, r_=r_, hf=hf: pv(p_, vw[:, r_, :], [vw], r_ == 0, r_ == nw - 1, hf)),
                              post=w_post if n_ == len(wu) - 1 else None))

        def s_front(kt, hf):
            eT = scores(ks[:, kt * 128:(kt + 1) * 128], qblk, [ks], hf)
            return masked(eT, maskT[:, kt, :], [maskT])

        def s_post():
            drain(len(pending))
            oc = grab()
            pending.extend(fin_ops(i, g, 1, oc))
            if g == 1:
                pending.append(block_out(i))

        su = [(kt, hf) for kt in range(nsl) for hf in range(2)]
        for n_, (kt, hf) in enumerate(su):
            units.append(dict(pre=(lambda: drain(len(pending))) if n_ == 0 else None,
                              front=(lambda kt=kt, hf=hf: s_front(kt, hf)),
                              back=(lambda p_, kt=kt, hf=hf: pv(p_, vs[:, kt, :], [vs], kt == 0, kt == nsl - 1, hf)),
                              post=s_post if n_ == len(su) - 1 else None))
        if it + 1 < 2 * NB:
            ni, ng = (it + 1) // 2, (it + 1) % 2
            old_pre = units[first + 6]["pre"]

            def pre6(old_pre=old_pre, ni=ni, ng=ng):
                if old_pre is not None:
                    old_pre()
                kv_loads(ni, ng)
            units[first + 6]["pre"] = pre6

    for i in range(NB):
        for g in range(2):
            add_iter(i, g)
    kv_loads(0, 0)
    live = {}

    def do_back(k):
        units[k]["back"](live.pop(k))
        if units[k]["post"] is not None:
            units[k]["post"]()

    for k, u in enumerate(units):
        if u["pre"] is not None:
            u["pre"]()
        live[k] = u["front"]()
        if k >= SKEW:
            do_back(k - SKEW)
        drain(2)
    for k in range(len(units) - SKEW, len(units)):
        do_back(k)
    drain(len(pending))

    if "C" in dbg or only_br is not None:
        fw.finish(list(dbg_out.values()))
        return nc, fw


    BASE = 16512 + 1024

    def at(kib, shape, dt_, name):
        assert (kib * 1024) % 32 == 0, (name, kib)
        fw.nsb += 1
        return Buf(nc.alloc_sbuf_tensor_at(f"{name}_{fw.nsb}", list(shape), dt_, offset=BASE + int(kib * 1024)), name)

    fw.barrier()
    xTo = at(0, [128, 16, NB * 128], BF16, "xTo2")
    uT = at(32, [128, 16, NB * 128], BF16, "uT")
    vtm = at(64, [128, NB, D], BF16, "vtm")
    wst = [at(96, [128, 16, 512], BF16, "wst0"), at(112, [128, 16, 512], BF16, "wst1")]
    xbo2 = [at(128, [128, D], BF16, "xbo2a"), at(132, [128, D], BF16, "xbo2b")]
    wsT_f = at(136, [128, 8, 128], F32, "wsT_f")
    tri = at(140, [128, 128], F32, "tri")
    wcT = at(140.5, [128, 8, 128], BF16, "wcT")
    addt = at(142.5, [128, 16, 128], F32, "addt")
    gcol = at(150.5, [128, 16], F32, "gcol")
    bcol = at(150.75, [128, 16], F32, "bcol")
    bsrow = at(151, [1, 8 * 128], BF16, "bsrow")
    onesr = at(153, [128, 128], BF16, "onesr")
    stats = at(153.5, [128, 4, 6], F32, "stats")
    mv = at(154, [128, 2], F32, "mv")
    rstd = at(154.25, [128, 1], F32, "rstd")
    tmpz = [at(155, [128, 128], F32, "tmpz0"), at(155.5, [128, 128], F32, "tmpz1")]
    for blk in range(NB):
        load_xT(xo_d, blk * 128, xbo2[blk % 2], xTo, blk * 128, "act" if blk % 2 == 0 else "dve")
    fw.dma("sp", wsT_f[:], sgu_wsT.t.ap().rearrange("g s t -> s g t"), reads=[sgu_wsT], writes=[wsT_f])
    fw.dma("sp", tri[:], C["tri"][:], reads=[C["tri"]], writes=[tri])
    fw.dma("sp", gcol[:], sgu_g_fm[:], reads=[sgu_g_fm], writes=[gcol])
    fw.dma("sp", bcol[:], sgu_b_fm[:], reads=[sgu_b_fm], writes=[bcol])
    fw.dma("pool", bsrow[:], sgu_bs[:], reads=[sgu_bs], writes=[bsrow])
    fw.op("dve", lambda e: e.memset(onesr[:], 1.0), writes=[onesr])
    fw.op("dve", lambda e: e.tensor_tensor(wcT[:], wsT_f[:], tri[:].unsqueeze(1).to_broadcast([128, 8, 128]), ALU.mult),
          reads=[wsT_f, tri], writes=[wcT])
    for g8 in range(8):
        b = nextbank()
        mm(bk(b)[:, 0:128], onesr[:], wcT[:, g8, :], True, True, [onesr, wcT], [banks[b]])
        b2 = nextbank()
        mm(bk(b2)[:, 0:128], onesr[0:1, :], bsrow[0:1, g8 * 128:(g8 + 1) * 128], True, True, [onesr, bsrow], [banks[b2]])
        for f2 in range(2):
            fc = g8 * 2 + f2
            fw.op("dve", lambda e: e.tensor_scalar(addt[:, fc, :], bk(b)[:, 0:128], bcol[:, fc:fc + 1], None, ALU.mult),
                  reads=[banks[b], bcol], writes=[addt])
            fw.op("dve", lambda e: e.tensor_tensor(addt[:, fc, :], addt[:, fc, :], bk(b2)[:, 0:128], ALU.add),
                  reads=[banks[b2], addt], writes=[addt])
    for cg in range(4):
        w = wst[cg % 2]
        fw.dma("pool", w[:], w_in_r[:, :, OFF_V + cg * 512:OFF_V + (cg + 1) * 512], reads=[w_in], writes=[w])
        for blk in range(NB):
            b = nextbank()
            for kc in range(16):
                mm(bk(b), xTo[:, kc, blk * 128:(blk + 1) * 128], w[:, kc, :], kc == 0, kc == 15, [xTo, w], [banks[b]])
            fw.op("act", lambda e: e.activation(vtm[:, blk, cg * 512:(cg + 1) * 512], bk(b), AF.Gelu_apprx_tanh),
                  reads=[banks[b]], writes=[vtm])
    for blk in range(NB):
        for c4 in range(4):
            fw.op("dve", lambda e: e.bn_stats(stats[:, c4, :], vtm[:, blk, c4 * 512:(c4 + 1) * 512]), reads=[vtm], writes=[stats])
        fw.op("dve", lambda e: e.bn_aggr(mv[:], stats[:]), reads=[stats], writes=[mv])
        fw.op("act", lambda e: e.activation(rstd[:], mv[:, 1:2], AF.Sqrt, bias=epsc[:]), reads=[mv, epsc], writes=[rstd])
        fw.op("dve", lambda e: e.reciprocal(rstd[:], rstd[:]), reads=[rstd], writes=[rstd])
        fw.op("dve", lambda e: e.tensor_scalar(vtm[:, blk, :], vtm[:, blk, :], mv[:, 0:1], rstd[:, 0:1], ALU.subtract, ALU.mult),
              reads=[vtm, mv, rstd], writes=[vtm])
    for cg in range(4):
        w = wst[cg % 2]
        fw.dma("pool", w[:], w_in_r[:, :, OFF_U + cg * 512:OFF_U + (cg + 1) * 512], reads=[w_in], writes=[w])
        for half in range(2):
            for f4 in range(4):
                b = nextbank()
                for kc in range(16):
                    mm(bk(b), w[:, kc, f4 * 128:(f4 + 1) * 128], xTo[:, kc, half * 512:(half + 1) * 512], kc == 0, kc == 15, [w, xTo], [banks[b]])
                fw.op("act", lambda e: e.activation(uT[:, cg * 4 + f4, half * 512:(half + 1) * 512], bk(b), AF.Gelu_apprx_tanh),
                      reads=[banks[b]], writes=[uT])
    zi = 0
    for blk in range(NB):
        for q4 in range(4):
            b = nextbank()
            for f4 in range(4):
                fc = q4 * 4 + f4
                mm(bk(b)[:, f4 * 128:(f4 + 1) * 128], vtm[:, blk, fc * 128:(fc + 1) * 128], wcT[:, fc // 2, :], True, True,
                   [vtm, wcT], [banks[b]], inc=(f4 == 3))
            for f4 in range(4):
                fc = q4 * 4 + f4
                tz = tmpz[zi % 2]
                zi += 1
                fw.op("dve", lambda e: e.scalar_tensor_tensor(tz[:], bk(b)[:, f4 * 128:(f4 + 1) * 128], gcol[:, fc:fc + 1], addt[:, fc, :],
                                                              ALU.mult, ALU.add), reads=[banks[b], gcol, addt], writes=[tz])
                fw.op("pool", lambda e: e.tensor_tensor(uT[:, fc, blk * 128:(blk + 1) * 128], uT[:, fc, blk * 128:(blk + 1) * 128], tz[:], ALU.mult),
                      reads=[uT, tz], writes=[uT])
    if "D" in dbg:
        dbg_out["osguT"] = fw.dram("dbg_osguT", [128, 16, NB * 128], BF16, kind="ExternalOutput")
        fw.dma("sp", dbg_out["osguT"].t.ap(), uT[:], reads=[uT], writes=[dbg_out["osguT"]])
        fw.finish(list(dbg_out.values()))
        return nc, fw

    fw.barrier()
    mergedT = at(70, [128, 16, NB * 128], BF16, "mergedT")
    wm_ = [[at(102 + 8 * (2 * k + i2), [128, 16, 256], BF16, f"wm{k}{i2}") for i2 in range(2)] for k in range(2)]
    sg = [at(134, [128, 512], F32, "sg0"), at(136, [128, 512], F32, "sg1")]
    m1 = [at(138, [128, 512], F32, "m10"), at(140, [128, 512], F32, "m11")]
    wa_r, wb_r = r3(w_br_a), r3(w_br_b)
    ei = 0
    for ps_ in range(2):
        srcT = o_nsaT if ps_ == 0 else uT
        wr = wa_r if ps_ == 0 else wb_r
        goff = OFF_GA if ps_ == 0 else OFF_GB
        for cg in range(8):
            wA, wG = wm_[0][cg % 2], wm_[1][cg % 2]
            fw.dma("pool", wA[:], wr[:, :, cg * 256:(cg + 1) * 256], reads=[w_br_a, w_br_b], writes=[wA])
            fw.dma("pool", wG[:], w_in_r[:, :, goff + cg * 256:goff + (cg + 1) * 256], reads=[w_in], writes=[wG])
            for fc in range(2):
                for half in range(2):
                    ba = nextbank()
                    for kc in range(16):
                        mm(bk(ba), wA[:, kc, fc * 128:(fc + 1) * 128], srcT[:, kc, half * 512:(half + 1) * 512], kc == 0, kc == 15, [wA, srcT], [banks[ba]])
                    bg = nextbank()
                    for kc in range(16):
                        mm(bk(bg), wG[:, kc, fc * 128:(fc + 1) * 128], xTo[:, kc, half * 512:(half + 1) * 512], kc == 0, kc == 15, [wG, xTo], [banks[bg]])
                    s_, m_ = sg[ei % 2], m1[ei % 2]
                    ei += 1
                    dst = mergedT[:, cg * 2 + fc, half * 512:(half + 1) * 512]
                    fw.op("act", lambda e: e.activation(s_[:], bk(bg), AF.Sigmoid), reads=[banks[bg]], writes=[s_])
                    if ps_ == 0:
                        fw.op("dve", lambda e: e.tensor_tensor(dst, s_[:], bk(ba), ALU.mult), reads=[s_, banks[ba]], writes=[mergedT])
                    else:
                        fw.op("dve", lambda e: e.tensor_tensor(m_[:], s_[:], bk(ba), ALU.mult), reads=[s_, banks[ba]], writes=[m_])
                        fw.op("dve", lambda e: e.tensor_tensor(dst, dst, m_[:], ALU.add), reads=[m_, mergedT], writes=[mergedT])
    fw.barrier()
    x1 = at(102, [128, NB, D], F32, "x1")
    xT1 = at(166, [128, 16, NB * 128], BF16, "xT1")
    wo_ = [at(32, [128, 16, 512], BF16, "wo0"), at(48, [128, 16, 512], BF16, "wo1")]
    xst = [at(64 + 2 * i3, [128, 512], F32, f"xst{i3}") for i3 in range(3)]
    lnt = [at(0, [128, D], F32, "lnt0"), at(8, [128, D], F32, "lnt1")]
    lng = at(16, [128, D], F32, "lng")
    lnb = at(24, [128, D], F32, "lnb")
    stats2 = at(198, [128, 4, 6], F32, "stats2")
    mv2 = at(198.125, [128, 2], F32, "mv2")
    rstd2 = at(198.25, [128, 1], F32, "rstd2")
    xbf = [at(198.5, [128, D], BF16, "xbf0"), at(202.5, [128, D], BF16, "xbf1")]
    wo_r = r3(w_o_d)

    def layer_norm(acc, g_d, b_d, xT_out, out_dram=None):
        fw.dma("sp", lng[:], g_d.t.ap().to_broadcast([128, D]), reads=[g_d], writes=[lng])
        fw.dma("sp", lnb[:], b_d.t.ap().to_broadcast([128, D]), reads=[b_d], writes=[lnb])
        for blk in range(NB):
            t_ = lnt[blk % 2]
            for c4 in range(4):
                fw.op("dve", lambda e: e.bn_stats(stats2[:, c4, :], acc[:, blk, c4 * 512:(c4 + 1) * 512]), reads=[acc], writes=[stats2])
            fw.op("dve", lambda e: e.bn_aggr(mv2[:], stats2[:]), reads=[stats2], writes=[mv2])
            fw.op("act", lambda e: e.activation(rstd2[:], mv2[:, 1:2], AF.Sqrt, bias=epsc[:]), reads=[mv2, epsc], writes=[rstd2])
            fw.op("dve", lambda e: e.reciprocal(rstd2[:], rstd2[:]), reads=[rstd2], writes=[rstd2])
            fw.op("dve", lambda e: e.tensor_scalar(t_[:], acc[:, blk, :], mv2[:, 0:1], rstd2[:, 0:1], ALU.subtract, ALU.mult),
                  reads=[acc, mv2, rstd2], writes=[t_])
            fw.op("dve", lambda e: e.tensor_tensor(t_[:], t_[:], lng[:], ALU.mult), reads=[t_, lng], writes=[t_])
            if out_dram is None:
                fw.op("pool", lambda e: e.tensor_tensor(acc[:, blk, :], t_[:], lnb[:], ALU.add), reads=[t_, lnb], writes=[acc])
            else:
                fw.op("pool", lambda e: e.tensor_tensor(t_[:], t_[:], lnb[:], ALU.add), reads=[t_, lnb], writes=[t_])
                fw.dma("sp", out_dram.t.ap()[blk * 128:(blk + 1) * 128, :], t_[:], reads=[t_], writes=[out_dram])
            if xT_out is not None:
                xb_ = xbf[blk % 2]
                fw.op("act", lambda e: e.copy(xb_[:], acc[:, blk, :]), reads=[acc], writes=[xb_])
                for half in range(2):
                    b = nextbank()
                    for k8 in range(8):
                        kc = half * 8 + k8
                        fw.op("pe", lambda e: e.transpose(bkbf(b)[:, k8 * 128:(k8 + 1) * 128], xb_[:, kc * 128:(kc + 1) * 128], ident[:]),
                              reads=[xb_, ident], writes=[banks[b]], inc=(k8 == 7))
                    fw.op("act", lambda e: e.copy(xT_out[:, half * 8:(half + 1) * 8, blk * 128:(blk + 1) * 128],
                                                  bkbf(b).rearrange("p (k t) -> p k t", k=8)), reads=[banks[b]], writes=[xT_out])

    xi = 0
    for cg in range(4):
        w = wo_[cg % 2]
        fw.dma("pool", w[:], wo_r[:, :, cg * 512:(cg + 1) * 512], reads=[w_o_d], writes=[w])
        for blk in range(NB):
            xs_ = xst[xi % 3]
            xi += 1
            fw.dma("sp", xs_[:], xo_d[blk * 128:(blk + 1) * 128, cg * 512:(cg + 1) * 512], reads=[xo_d], writes=[xs_])
            b = nextbank()
            for kc in range(16):
                mm(bk(b), mergedT[:, kc, blk * 128:(blk + 1) * 128], w[:, kc, :], kc == 0, kc == 15, [mergedT, w], [banks[b]])
            fw.op("dve", lambda e: e.scalar_tensor_tensor(x1[:, blk, cg * 512:(cg + 1) * 512], xs_[:], ALPHA, bk(b), ALU.mult, ALU.add),
                  reads=[xs_, banks[b]], writes=[x1])
    wxk = at(32, [128, 16, 512], BF16, "wxk")
    wxv = at(48, [128, 16, 512], BF16, "wxv")
    fw.dma("pool", wxk[:], r3(w_xk), reads=[w_xk], writes=[wxk, wo_[0]])
    fw.dma("pool", wxv[:], r3(w_xv), reads=[w_xv], writes=[wxv, wo_[1]])
    layer_norm(x1, ln1_g, ln1_b, xT1)
    if "E" in dbg:
        dbg_out["x1"] = fw.dram("dbg_x1", [128, NB, D], F32, kind="ExternalOutput")
        fw.dma("sp", dbg_out["x1"].t.ap(), x1[:], reads=[x1], writes=[dbg_out["x1"]])
        fw.finish(list(dbg_out.values()))
        return nc, fw


    fw.barrier()
    memb = [at(64, [128, D], BF16, "memb0"), at(68, [128, D], BF16, "memb1")]
    memT = at(72, [128, 16, 256], BF16, "memT")
    KmT = at(80, [128, 4, 256], BF16, "KmT")
    Vm = at(82, [128, 2, 4, 129], BF16, "Vm")
    for mb in range(2):
        load_xT(mem_d, mb * 128, memb[mb], memT, mb * 128, "act" if mb == 0 else "dve")
    fw.op("dve", lambda e: e.memset(Vm[:, :, :, 128:129], 1.0), writes=[Vm])
    for h in range(4):
        b = nextbank()
        for kc in range(16):
            mm(bk(b)[:, 0:256], wxk[:, kc, h * 128:(h + 1) * 128], memT[:, kc, :], kc == 0, kc == 15, [wxk, memT], [banks[b]])
        fw.op("act", lambda e: e.copy(KmT[:, h, :], bk(b)[:, 0:256]), reads=[banks[b]], writes=[KmT])
    for mch in range(2):
        b = nextbank()
        for kc in range(16):
            mm(bk(b), memT[:, kc, mch * 128:(mch + 1) * 128], wxv[:, kc, :], kc == 0, kc == 15, [memT, wxv], [banks[b]])
        fw.op("dve", lambda e: e.tensor_copy(Vm[:, mch, :, 0:128], bk(b).rearrange("p (h d) -> p h d", h=4)), reads=[banks[b]], writes=[Vm])
    fw.barrier()
    wxq = at(32, [128, 16, 512], BF16, "wxq")
    wxo = at(48, [128, 4, D], BF16, "wxo")
    qxT = at(64, [128, 4, NB * 128], BF16, "qxT")
    eTx = [at(72, [128, 2, 128], BF16, "eTx0"), at(72.5, [128, 2, 128], BF16, "eTx1")]
    oxb = at(75, [128, 512], BF16, "oxb")
    oxT = at(76, [128, 4, 128], BF16, "oxT")
    rinvx = at(77, [128, 4], F32, "rinvx")
    fw.dma("pool", wxq[:], r3(w_xq), reads=[w_xq], writes=[wxq])
    fw.dma("pool", wxo[:], r3(w_xo), reads=[w_xo], writes=[wxo])
    for h in range(4):
        for half in range(2):
            b = nextbank()
            for kc in range(16):
                mm(bk(b), wxq[:, kc, h * 128:(h + 1) * 128], xT1[:, kc, half * 512:(half + 1) * 512], kc == 0, kc == 15, [wxq, xT1], [banks[b]])
            fw.op("act", lambda e: e.mul(qxT[:, h, half * 512:(half + 1) * 512], bk(b), 128.0 ** -0.5), reads=[banks[b]], writes=[qxT])
    exi = 0
    for blk in range(NB):
        bA, bB = nextbank(), nextbank()

        def ox_ps(h, lo=0, hi=129):
            return PS[:, bA if h < 3 else bB, (h % 3) * 129 + lo:(h % 3) * 129 + hi]

        for h in range(4):
            b = nextbank()
            for mch in range(2):
                mm(bk(b)[:, mch * 128:(mch + 1) * 128], KmT[:, h, mch * 128:(mch + 1) * 128], qxT[:, h, blk * 128:(blk + 1) * 128],
                   True, True, [KmT, qxT], [banks[b]], inc=(mch == 1))
            et = eTx[exi % 2]
            exi += 1
            fw.op("act", lambda e: e.activation(et[:], bk(b)[:, 0:256].rearrange("p (c t) -> p c t", c=2), AF.Exp), reads=[banks[b]], writes=[et])
            for mch in range(2):
                mm(ox_ps(h), et[:, mch, :], Vm[:, mch, h, :], mch == 0, mch == 1, [et, Vm], [banks[bA if h < 3 else bB]])
        vA = PS[:, bA, 0:387].rearrange("p (h c) -> p h c", c=129)
        fw.op("dve", lambda e: e.reciprocal(rinvx[:, 0:3], vA[:, :, 128]), reads=[banks[bA]], writes=[rinvx])
        fw.op("dve", lambda e: e.reciprocal(rinvx[:, 3:4], PS[:, bB, 128:129]), reads=[banks[bB]], writes=[rinvx])
        for h in range(4):
            fw.op("dve", lambda e: e.tensor_scalar(oxb[:, h * 128:(h + 1) * 128], ox_ps(h, 0, 128), rinvx[:, h:h + 1], None, ALU.mult),
                  reads=[banks[bA if h < 3 else bB], rinvx], writes=[oxb])
        b = nextbank()
        for h in range(4):
            fw.op("pe", lambda e: e.transpose(bkbf(b)[:, h * 128:(h + 1) * 128], oxb[:, h * 128:(h + 1) * 128], ident[:]),
                  reads=[oxb, ident], writes=[banks[b]], inc=(h == 3))
        fw.op("act", lambda e: e.copy(oxT[:], bkbf(b)[:, 0:512].rearrange("p (k t) -> p k t", k=4)), reads=[banks[b]], writes=[oxT])
        for cg in range(4):
            b = nextbank()
            for kc in range(4):
                mm(bk(b), oxT[:, kc, :], wxo[:, kc, cg * 512:(cg + 1) * 512], kc == 0, kc == 3, [oxT, wxo], [banks[b]])
            sl = x1[:, blk, cg * 512:(cg + 1) * 512]
            fw.op("dve", lambda e: e.scalar_tensor_tensor(sl, sl, ALPHA, bk(b), ALU.mult, ALU.add), reads=[x1, banks[b]], writes=[x1])
    layer_norm(x1, ln2_g, ln2_b, None)
    if "F" in dbg:
        dbg_out["x2"] = fw.dram("dbg_x2", [128, NB, D], F32, kind="ExternalOutput")
        fw.dma("sp", dbg_out["x2"].t.ap(), x1[:], reads=[x1], writes=[dbg_out["x2"]])
        fw.finish(list(dbg_out.values()))
        return nc, fw

    fw.barrier()
    acc = x1
    x2b = at(166, [128, NB, D], BF16, "x2b")
    posm_f = at(96, [128, NB, 64], F32, "posm_f")
    GWb = at(98, [128, NB, 64], BF16, "GWb")
    posmT = at(99, [64, NB * 128], BF16, "posmT")
    iota_c = at(101, [128, 64], F32, "iota_c")
    iota_pb = at(101.25, [64, 64], F32, "iota_pb")
    iota_p1 = at(101.5, [64, 1], F32, "iota_p1")
    x2hT = at(32, [128, 16, 128], BF16, "x2hT")
    x2lT = at(36, [128, 16, 128], BF16, "x2lT")
    wr = at(40, [128, 16, 68], F32, "wr")
    wrh = at(51, [128, 16, 68], BF16, "wrh")
    wrl = at(54, [128, 16, 68], BF16, "wrl")
    wtmp = at(57, [128, 16, 68], F32, "wtmp")
    x2l = at(62, [128, D], BF16, "x2l")
    bias_b = at(66, [128, 68], F32, "bias_b")
    logit = at(68, [128, NB, 68], F32, "logit")
    asg = at(46, [128, NB, 64], BF16, "asg")
    ustr = at(47, [128, 128], BF16, "ustr")
    ones_b = at(47.25, [128, 128], BF16, "ones_b")
    msk = at(71, [128, NB, 64], F32, "msk")
    msk2 = at(73, [128, NB, 64], F32, "msk2")
    oh1 = at(75, [128, NB, 64], F32, "oh1")
    oh2 = at(77, [128, NB, 64], F32, "oh2")
    d4 = at(79, [128, NB, 4], F32, "d4")
    e4 = at(79.125, [128, NB, 4], F32, "e4")
    gm = at(79.25, [128, NB, 4], F32, "gm")
    pen = at(79.375, [128, NB, 4], F32, "pen")
    sA_ = at(79.5, [128, NB], F32, "sA_")
    sB_ = at(79.53125, [128, NB], F32, "sB_")
    ggrp = at(79.5625, [128, NB], F32, "ggrp")
    m1_ = at(79.59375, [128, NB], F32, "m1_")
    m2_ = at(79.625, [128, NB], F32, "m2_")
    e2_ = at(79.65625, [128, NB], F32, "e2_")
    g1_ = at(79.6875, [128, NB], F32, "g1_")
    g2_ = at(79.71875, [128, NB], F32, "g2_")
    posb = at(50, [128, 64], BF16, "posb")
    fw.dma("sp", wr[:], r3(w_rt), reads=[w_rt], writes=[wr])
    fw.dma("sp", bias_b[:], b_rt.t.ap().to_broadcast([128, 68]), reads=[b_rt], writes=[bias_b])
    fw.dma("sp", iota_c[:], C["iota_c"][:], reads=[C["iota_c"]], writes=[iota_c])
    fw.dma("sp", iota_pb[:], C["iota_pb"][:], reads=[C["iota_pb"]], writes=[iota_pb])
    fw.op("dve", lambda e: e.tensor_copy(iota_p1[:], iota_pb[:, 0:1]), reads=[iota_pb], writes=[iota_p1])
    fw.dma("pool", ustr[:], C["ustrict"][:], reads=[C["ustrict"]], writes=[ustr])
    fw.op("dve", lambda e: e.memset(ones_b[:], 1.0), writes=[ones_b])
    fw.op("dve", lambda e: e.tensor_copy(wrh[:], wr[:]), reads=[wr], writes=[wrh])
    fw.op("dve", lambda e: e.tensor_copy(wtmp[:], wrh[:]), reads=[wrh], writes=[wtmp])
    fw.op("dve", lambda e: e.tensor_tensor(wrl[:], wr[:], wtmp[:], ALU.subtract), reads=[wr, wtmp], writes=[wrl])
    for blk in range(NB):
        fw.op("act", lambda e: e.copy(x2b[:, blk, :], acc[:, blk, :]), reads=[acc], writes=[x2b])
        fw.op("dve", lambda e: e.tensor_tensor(x2l[:], acc[:, blk, :], x2b[:, blk, :], ALU.subtract), reads=[acc, x2b], writes=[x2l])
        for (srcb, src_ap, dstT) in ((x2b, x2b[:, blk, :], x2hT), (x2l, x2l[:], x2lT)):
            for half in range(2):
                b = nextbank()
                for k8 in range(8):
                    kc = half * 8 + k8
                    fw.op("pe", lambda e: e.transpose(bkbf(b)[:, k8 * 128:(k8 + 1) * 128], src_ap[:, kc * 128:(kc + 1) * 128], ident[:]),
                          reads=[srcb, ident], writes=[banks[b]], inc=(k8 == 7))
                if half == 0:
                    fw.op("act", lambda e: e.copy(dstT[:, half * 8:(half + 1) * 8, :], bkbf(b).rearrange("p (k t) -> p k t", k=8)),
                          reads=[banks[b]], writes=[dstT])
                else:
                    fw.op("dve", lambda e: e.tensor_copy(dstT[:, half * 8:(half + 1) * 8, :], bkbf(b).rearrange("p (k t) -> p k t", k=8)),
                          reads=[banks[b]], writes=[dstT])
        b = nextbank()
        trip = [(x2hT, wrh), (x2hT, wrl), (x2lT, wrh)]
        for ti, (xt_, w_) in enumerate(trip):
            for kc in range(16):
                mm(bk(b)[:, 0:68], xt_[:, kc, :], w_[:, kc, :], ti == 0 and kc == 0, ti == 2 and kc == 15, [xt_, w_], [banks[b]])
        fw.op("dve", lambda e: e.tensor_tensor(logit[:, blk, :], bk(b)[:, 0:68], bias_b[:], ALU.add), reads=[banks[b], bias_b], writes=[logit])
        fw.op("act", lambda e: e.mul(acc[:, blk, :], acc[:, blk, :], ALPHA), reads=[acc], writes=[acc])
    if "R" in dbg:
        dbg_out["logit"] = fw.dram("dbg_logit", [128, NB, 68], F32, kind="ExternalOutput")
        fw.dma("sp", dbg_out["logit"].t.ap(), logit[:], reads=[logit], writes=[dbg_out["logit"]])
    dv = lambda fn, R, W: fw.op("dve", fn, reads=R, writes=W)
    L4 = logit[:, :, 0:4]
    LE = logit[:, :, 4:68].rearrange("p b (g x) -> p b g x", g=4)
    bc4 = lambda t: t[:].unsqueeze(2).to_broadcast([128, NB, 4])
    bc64 = lambda t: t[:].unsqueeze(2).to_broadcast([128, NB, 64])
    m4 = lambda t: t[:].rearrange("p b (g x) -> p b g x", g=4)
    dv(lambda e: e.reduce_max(sA_[:], L4, AX.X), [logit], [sA_])
    dv(lambda e: e.tensor_tensor(d4[:], L4, bc4(sA_), ALU.subtract), [logit, sA_], [d4])
    fw.op("act", lambda e: e.activation(e4[:], d4[:], AF.Exp), reads=[d4], writes=[e4])
    dv(lambda e: e.reduce_sum(sB_[:], e4[:], AX.X), [e4], [sB_])
    dv(lambda e: e.reciprocal(ggrp[:], sB_[:]), [sB_], [ggrp])
    dv(lambda e: e.tensor_scalar(gm[:], d4[:], 0.0, None, ALU.is_equal), [d4], [gm])
    dv(lambda e: e.tensor_scalar(pen[:], gm[:], 1e9, -1e9, ALU.mult, ALU.add), [gm], [pen])
    dv(lambda e: e.tensor_tensor(m4(msk), LE, gm[:].unsqueeze(3).to_broadcast([128, NB, 4, 16]), ALU.mult), [logit, gm], [msk])
    dv(lambda e: e.tensor_tensor(m4(msk), m4(msk), pen[:].unsqueeze(3).to_broadcast([128, NB, 4, 16]), ALU.add), [msk, pen], [msk])
    dv(lambda e: e.reduce_max(m1_[:], msk[:], AX.X), [msk], [m1_])
    dv(lambda e: e.tensor_tensor(oh1[:], msk[:], bc64(m1_), ALU.is_equal), [msk, m1_], [oh1])
    dv(lambda e: e.scalar_tensor_tensor(msk2[:], oh1[:], -1e9, msk[:], ALU.mult, ALU.add), [oh1, msk], [msk2])
    dv(lambda e: e.reduce_max(m2_[:], msk2[:], AX.X), [msk2], [m2_])
    dv(lambda e: e.tensor_tensor(oh2[:], msk2[:], bc64(m2_), ALU.is_equal), [msk2, m2_], [oh2])
    dv(lambda e: e.tensor_tensor(sA_[:], m2_[:], m1_[:], ALU.subtract), [m1_, m2_], [sA_])
    fw.op("act", lambda e: e.activation(e2_[:], sA_[:], AF.Exp), reads=[sA_], writes=[e2_])
    dv(lambda e: e.tensor_scalar(sB_[:], e2_[:], 1.0, None, ALU.add), [e2_], [sB_])
    dv(lambda e: e.reciprocal(sB_[:], sB_[:]), [sB_], [sB_])
    dv(lambda e: e.tensor_tensor(g1_[:], sB_[:], ggrp[:], ALU.mult), [sB_, ggrp], [g1_])
    dv(lambda e: e.tensor_tensor(g2_[:], g1_[:], e2_[:], ALU.mult), [g1_, e2_], [g2_])
    dv(lambda e: e.tensor_tensor(msk[:], oh1[:], bc64(g1_), ALU.mult), [oh1, g1_], [msk])
    dv(lambda e: e.tensor_tensor(msk2[:], oh2[:], bc64(g2_), ALU.mult), [oh2, g2_], [msk2])
    dv(lambda e: e.tensor_tensor(GWb[:], msk[:], msk2[:], ALU.add), [msk, msk2], [GWb])
    dv(lambda e: e.tensor_tensor(asg[:], oh1[:], oh2[:], ALU.add), [oh1, oh2], [asg])
    for blk in range(NB):
        b = nextbank()
        mm(bk(b)[:, 0:64], ustr[:], asg[:, blk, :], True, blk == 0, [ustr, asg], [banks[b]])
        for b2 in range(blk):
            mm(bk(b)[:, 0:64], ones_b[:], asg[:, b2, :], False, b2 == blk - 1, [ones_b, asg], [banks[b]])
        fw.op("dve", lambda e: e.scalar_tensor_tensor(posm_f[:, blk, :], bk(b)[:, 0:64], 1.0, asg[:, blk, :], ALU.add, ALU.mult),
              reads=[banks[b], asg], writes=[posm_f])
        fw.op("dve", lambda e: e.tensor_scalar(posm_f[:, blk, :], posm_f[:, blk, :], -1.0, 200.0, ALU.add, ALU.min), reads=[posm_f], writes=[posm_f])
        fw.op("dve", lambda e: e.tensor_copy(posb[:], posm_f[:, blk, :]), reads=[posm_f], writes=[posb])
        b3 = nextbank()
        fw.op("pe", lambda e: e.transpose(bkbf(b3)[0:64, 0:128], posb[:], ident[:]), reads=[posb, ident], writes=[banks[b3]])
        fw.op("act", lambda e: e.copy(posmT[:, blk * 128:(blk + 1) * 128], bkbf(b3)[0:64, 0:128]), reads=[banks[b3]], writes=[posmT])
    if "R" in dbg:
        dbg_out["posm"] = fw.dram("dbg_posm", [128, NB, 64], F32, kind="ExternalOutput")
        fw.dma("sp", dbg_out["posm"].t.ap(), posm_f[:], reads=[posm_f], writes=[dbg_out["posm"]])
        dbg_out["GW"] = fw.dram("dbg_GW", [128, NB, 64], BF16, kind="ExternalOutput")
        fw.dma("sp", dbg_out["GW"].t.ap(), GWb[:], reads=[GWb], writes=[dbg_out["GW"]])
        fw.finish(list(dbg_out.values()))
        return nc, fw
    fw.barrier()
    GE = 4
    Ygrp = at(0, [64, GE, D], BF16, "Ygrp")
    SelTg = at(16, [64, GE, NB * 128], BF16, "SelTg")
    XgT = [at(24, [128, 16, CAP], BF16, "XgT0"), at(26, [128, 16, CAP], BF16, "XgT1")]
    Sel = [at(28, [128, NB, CAP], BF16, "Sel0"), at(29, [128, NB, CAP], BF16, "Sel1")]
    hb = at(30, [64, 512], BF16, "hb")
    hT = at(31, [128, 4, CAP], BF16, "hT")
    gslot = at(31.5, [64, 1], F32, "gslot")
    rowsel = at(31.75, [64, 64], BF16, "rowsel")
    sgt = at(200, [64, 512], F32, "sgt")
    wsl = [at(32 + 16 * i4, [128, 16, 512], BF16, f"wsl{i4}") for i4 in range(4)]
    wcnt = [0]

    def wload(src_ap, srcbuf, shape3):
        t = wsl[wcnt[0] % 4]
        wcnt[0] += 1
        view = t[:] if shape3 == 16 else t[:].rearrange("p a (b c) -> p (a b) c", b=4)[:, 0:4, :] if False else None
        return t

    for eg in range(64 // GE):
        for el in range(GE):
            ex = eg * GE + el
            wg_, wu_, wd_ = wsl[(3 * ex) % 4], wsl[(3 * ex + 1) % 4], wsl[(3 * ex + 2) % 4]
            fw.dma("pool", wg_[:], w_eg.t.ap()[ex].rearrange("(kc p) n -> p kc n", p=128), reads=[w_eg], writes=[wg_])
            fw.dma("pool", wu_[:], w_eu.t.ap()[ex].rearrange("(kc p) n -> p kc n", p=128), reads=[w_eu], writes=[wu_])
            wdv = wd_[:].rearrange("p a n -> p (a n)").rearrange("p (f n) -> p f n", f=4)
            fw.dma("pool", wdv, w_ed.t.ap()[ex].rearrange("(fc p) n -> p fc n", p=128), reads=[w_ed], writes=[wd_])
            sel = Sel[ex % 2]
            for blk in range(NB):
                fw.op("dve", lambda e: e.tensor_scalar(sel[:, blk, :], iota_c[:], posm_f[:, blk, ex:ex + 1], None, ALU.is_equal),
                      reads=[iota_c, posm_f], writes=[sel])
            xg = XgT[ex % 2]
            for hf in range(2):
                b = nextbank()
                for k8 in range(8):
                    kc = hf * 8 + k8
                    for blk in range(NB):
                        mm(bk(b)[:, k8 * CAP:(k8 + 1) * CAP], x2b[:, blk, kc * 128:(kc + 1) * 128], sel[:, blk, :], blk == 0, blk == NB - 1,
                           [x2b, sel], [banks[b]], inc=(blk == NB - 1 and k8 == 7))
                cp = (lambda e: e.copy(xg[:, hf * 8:(hf + 1) * 8, :], bk(b).rearrange("p (k c) -> p k c", k=8)))
                fw.op("act", cp, reads=[banks[b]], writes=[xg])
            b = nextbank()
            for blk in range(NB):
                mm(bk(b)[0:CAP, 0:1], sel[:, blk, :], GWb[:, blk, ex:ex + 1], blk == 0, blk == NB - 1, [sel, GWb], [banks[b]])
            fw.op("dve", lambda e: e.tensor_copy(gslot[:], bk(b)[0:CAP, 0:1]), reads=[banks[b]], writes=[gslot])
            bg, bu = nextbank(), nextbank()
            for kc in range(16):
                mm(bk(bg)[0:CAP, :], xg[:, kc, :], wg_[:, kc, :], kc == 0, kc == 15, [xg, wg_], [banks[bg]])
            for kc in range(16):
                mm(bk(bu)[0:CAP, :], xg[:, kc, :], wu_[:, kc, :], kc == 0, kc == 15, [xg, wu_], [banks[bu]])
            fw.op("act", lambda e: e.activation(sgt[:], bk(bg)[0:CAP, :], AF.Silu), reads=[banks[bg]], writes=[sgt])
            fw.op("dve", lambda e: e.scalar_tensor_tensor(hb[:], bk(bu)[0:CAP, :], gslot[:, 0:1], sgt[:], ALU.mult, ALU.mult),
                  reads=[banks[bu], gslot, sgt], writes=[hb])
            b = nextbank()
            for fc in range(4):
                fw.op("pe", lambda e: e.transpose(bkbf(b)[:, fc * CAP:(fc + 1) * CAP], hb[:, fc * 128:(fc + 1) * 128], ident[0:CAP, 0:CAP]),
                      reads=[hb, ident], writes=[banks[b]], inc=(fc == 3))
            fw.op("act", lambda e: e.copy(hT[:], bkbf(b)[:, 0:4 * CAP].rearrange("p (k c) -> p k c", k=4)), reads=[banks[b]], writes=[hT])
            for cg in range(4):
                b = nextbank()
                for fc in range(4):
                    mm(bk(b)[0:CAP, :], hT[:, fc, :], wdv[:, fc, cg * 512:(cg + 1) * 512], fc == 0, fc == 3, [hT, wd_], [banks[b]])
                if cg % 2 == 0:
                    fw.op("act", lambda e: e.copy(Ygrp[:, el, cg * 512:(cg + 1) * 512], bk(b)[0:CAP, :]), reads=[banks[b]], writes=[Ygrp])
                else:
                    fw.op("dve", lambda e: e.tensor_copy(Ygrp[:, el, cg * 512:(cg + 1) * 512], bk(b)[0:CAP, :]), reads=[banks[b]], writes=[Ygrp])
            fw.op("dve", lambda e: e.tensor_scalar(rowsel[:], iota_pb[:], float(ex), None, ALU.is_equal), reads=[iota_pb], writes=[rowsel])
            for hf in range(2):
                b = nextbank()
                mm(bk(b)[0:CAP, :], rowsel[:], posmT[:, hf * 512:(hf + 1) * 512], True, True, [rowsel, posmT], [banks[b]])
                fw.op("dve", lambda e: e.tensor_scalar(SelTg[:, el, hf * 512:(hf + 1) * 512], bk(b)[0:CAP, :], iota_p1[:, 0:1], None, ALU.is_equal),
                      reads=[banks[b], iota_p1], writes=[SelTg])
        for blk in range(NB):
            bs4 = [nextbank() for _ in range(4)]
            for el in range(GE):
                for cg in range(4):
                    mm(bk(bs4[cg]), SelTg[:, el, blk * 128:(blk + 1) * 128], Ygrp[:, el, cg * 512:(cg + 1) * 512], el == 0, el == GE - 1,
                       [SelTg, Ygrp], [banks[bs4[cg]]])
            for cg in range(4):
                sl = acc[:, blk, cg * 512:(cg + 1) * 512]
                fw.op("dve", lambda e: e.tensor_tensor(sl, sl, bk(bs4[cg]), ALU.add), reads=[acc, banks[bs4[cg]]], writes=[acc])
    fw.barrier()
    layer_norm(acc, ln3_g, ln3_b, None, out_dram=out_d)
    fw.finish([out_d])
    return nc, fw


def _prep_inputs(inputs, core):
    b, j = core // 4, core % 4
    x = np.asarray(inputs["x"])
    w_in = np.asarray(inputs["w_in"])[0]
    m = {}
    m["x"] = np.ascontiguousarray(x[b])
    own = np.concatenate([np.arange(128 * (4 * i + j), 128 * (4 * i + j) + 128) for i in range(NB)])
    m["x_own"] = np.ascontiguousarray(x[b][own])
    m["w_in"] = w_in

    def swap_halves(w):
        sh = w.shape
        return np.ascontiguousarray(w.reshape(sh[0], -1, 2, 64)[:, :, ::-1, :].reshape(sh))

    m["wq_rot"] = swap_halves(w_in[:, OFF_Q:OFF_Q + 2048])
    kcols = np.concatenate([w_in[:, OFF_KV + 512:OFF_KV + 768], w_in[:, OFF_KV + 1024:OFF_KV + 1280]], axis=1)
    m["wk_rot"] = swap_halves(kcols)
    m["pe_kT"] = np.ascontiguousarray(np.asarray(inputs["cmp_pe_k"])[0].T)
    m["pe_vT"] = np.ascontiguousarray(np.asarray(inputs["cmp_pe_v"])[0].T)
    m["cmp_w1_k"] = np.asarray(inputs["cmp_w1_k"])[0]
    m["cmp_w1_v"] = np.asarray(inputs["cmp_w1_v"])[0]
    m["cmp_w2_k"] = np.asarray(inputs["cmp_w2_k"])[0]
    m["cmp_w2_k_rot"] = swap_halves(np.asarray(inputs["cmp_w2_k"])[0])
    m["cmp_w2_v"] = np.asarray(inputs["cmp_w2_v"])[0]
    g = lambda k: np.asarray(inputs[k])[0]
    m["sgu_wsT"] = np.ascontiguousarray(g("sgu_w_s").transpose(0, 2, 1))
    m["sgu_g_fm"] = np.ascontiguousarray(g("sgu_ln_g").reshape(16, 128).T)
    m["sgu_b_fm"] = np.ascontiguousarray(g("sgu_ln_b").reshape(16, 128).T)
    m["sgu_bs"] = np.ascontiguousarray(g("sgu_b_s").reshape(1, 1024))
    m["w_branch_a"] = g("w_branch_a")
    m["w_branch_b"] = g("w_branch_b")
    m["w_o"] = g("w_o")
    m["ln1_g"] = g("ln1_g").reshape(1, D)
    m["ln1_b"] = g("ln1_b").reshape(1, D)
    m["mem"] = np.ascontiguousarray(np.asarray(inputs["mem"])[b])
    for k in ("w_xq", "w_xk", "w_xv", "w_xo", "w_exp_gate", "w_exp_up", "w_exp_down"):
        m[k] = g(k)
    for k in ("ln2_g", "ln2_b", "ln3_g", "ln3_b"):
        m[k] = g(k).reshape(1, D)
    m["w_router"] = np.ascontiguousarray(np.concatenate([g("w_router_grp"), g("w_router_exp")], axis=1))
    m["b_router"] = np.concatenate([g("b_router_grp"), g("b_router_exp")]).reshape(1, 68)
    for k, v in _consts(j).items():
        m["c_" + k] = v
    return m


def kernel(**inputs):
    nc, fw = build()
    in_maps = [_prep_inputs(inputs, c) for c in range(8)]
    res = run_bass_kernel_spmd(nc, in_maps, core_ids=list(range(8)))
    out = np.zeros((2, S, D), np.float32)
    for c in range(8):
        b, j = c // 4, c % 4
        o = res.results[c]["out"]
        for i in range(NB):
            blk = 4 * i + j
            out[b, blk * 128:(blk + 1) * 128] = o[i * 128:(i + 1) * 128]
    return out
```
